# Optimizing a Trainium2 kernel written in Bass

```python
import jax, jax.numpy as jnp
from jax import lax
import numpy as np


D_MODEL = 2048
BATCH = 8
SEQ = 2048
DEPTH = 1

D_MIX = D_MODEL
NSA_HEADS = 8
NSA_KV_GROUPS = 2
NSA_GROUP_HEADS = NSA_HEADS // NSA_KV_GROUPS
NSA_HEAD_DIM = 128
RET_HEADS = 8
RET_HEAD_DIM = 128
CMP_BLOCK = 32
CMP_STRIDE = 16
SEL_BLOCK = 64
SEL_TOPK = 8
WINDOW = 512
Q_BLOCK = 128
SEL_Q_BLOCK = 64
RET_CHUNK = 128
ROPE_BASE = 10000.0
N_GROUPS = 4
EXPERTS_PER_GROUP = 8
N_EXPERTS = N_GROUPS * EXPERTS_PER_GROUP
TOPK_IN_GROUP = 2
D_EXPERT = 512
MOE_BLOCK = 256
RMS_EPS = 1e-6
GN_EPS = 1e-5
NEG_INF = -1e30
FORCED_SCORE = 1e6
NSA_Q_WIDTH = NSA_HEADS * NSA_HEAD_DIM
NSA_KV_WIDTH = NSA_KV_GROUPS * NSA_HEAD_DIM
NSA_GATE_WIDTH = 3 * NSA_HEADS
RET_WIDTH = RET_HEADS * RET_HEAD_DIM
IN_PROJ_WIDTH = NSA_Q_WIDTH + 6 * NSA_KV_WIDTH + NSA_GATE_WIDTH + 4 * RET_WIDTH

kernel_name = 'hymba_nsa_retnet_hmoe_block'


def rms_norm(x, g, eps=RMS_EPS):
    xf = x.astype(jnp.float32)
    y = xf * lax.rsqrt(jnp.mean(xf * xf, axis=-1, keepdims=True) + eps)
    return (y * g.astype(jnp.float32)).astype(x.dtype)


def masked_softmax(s, mask):
    return jax.nn.softmax(jnp.where(mask, s, NEG_INF), axis=-1)


def nsa_attention(q, k_cmp, v_cmp, k_slc, v_slc, k_win, v_win, gate_logits,
                  cmp_pos, cmp_w1, cmp_w2, q_norm_g, k_norm_g):
    B, S = q.shape[0], q.shape[1]
    G, HG, hd = NSA_KV_GROUPS, NSA_GROUP_HEADS, NSA_HEAD_DIM
    dt = q.dtype
    scale = hd ** -0.5
    pos = jnp.arange(S)
    qg = rms_norm(q, q_norm_g).reshape(B, S, G, HG, hd).transpose(0, 2, 3, 1, 4)

    n_cmp = (S - CMP_BLOCK) // CMP_STRIDE + 1
    cmp_start = jnp.arange(n_cmp) * CMP_STRIDE
    blk_idx = cmp_start[:, None] + jnp.arange(CMP_BLOCK)[None, :]

    def compress(t, i):
        tb = t[:, blk_idx] + cmp_pos[i][None, None, :, None, :]
        hid = jax.nn.silu(jnp.einsum('bnlgd,lde->bnge', tb, cmp_w1[i]))
        return jnp.einsum('bnge,ef->bgnf', hid, cmp_w2[i])

    kc = rms_norm(compress(k_cmp, 0), k_norm_g[0])
    vc = compress(v_cmp, 1)
    s_cmp = jnp.einsum('bghsd,bgnd->bghsn', qg, kc).astype(jnp.float32) * scale
    cmask = (cmp_start + CMP_BLOCK - 1)[None, :] <= pos[:, None]
    p_cmp = masked_softmax(s_cmp, cmask) * cmask
    o_cmp = jnp.einsum('bghsn,bgnd->bghsd', p_cmp.astype(dt), vc)

    n_sblk = S // SEL_BLOCK
    n_sel = min(SEL_TOPK, n_sblk)
    sel_start = jnp.arange(n_sblk) * SEL_BLOCK
    overlap = ((cmp_start[:, None] < (sel_start + SEL_BLOCK)[None, :]) &
               ((cmp_start + CMP_BLOCK)[:, None] > sel_start[None, :])).astype(jnp.float32)
    importance = jnp.einsum('bghsn,nj->bgsj', p_cmp, overlap)
    jb = jnp.arange(n_sblk)[None, :]
    cur = (pos // SEL_BLOCK)[:, None]
    sel_valid = sel_start[None, :] <= pos[:, None]
    forced = (jb == 0) | (jb == cur) | (jb == cur - 1)
    score = jnp.where(sel_valid, jnp.where(forced, FORCED_SCORE, importance), -jnp.inf)
    _, sel_idx = lax.top_k(score, n_sel)

    k_blocks = rms_norm(k_slc, k_norm_g[1]).transpose(0, 2, 1, 3).reshape(B, G, n_sblk, SEL_BLOCK, hd)
    v_blocks = v_slc.transpose(0, 2, 1, 3).reshape(B, G, n_sblk, SEL_BLOCK, hd)
    n_sqc = S // SEL_Q_BLOCK
    q_chunks = qg.reshape(B, G, HG, n_sqc, SEL_Q_BLOCK, hd).transpose(3, 0, 1, 2, 4, 5)
    idx_chunks = sel_idx.reshape(B, G, n_sqc, SEL_Q_BLOCK, n_sel).transpose(2, 0, 1, 3, 4)
    pos_chunks = pos.reshape(n_sqc, SEL_Q_BLOCK)
    bi = jnp.arange(B)[:, None, None, None]
    gi = jnp.arange(G)[None, :, None, None]

    def sel_chunk(args):
        qc, ic, tc = args
        kg = k_blocks[bi, gi, ic].reshape(B, G, SEL_Q_BLOCK, n_sel * SEL_BLOCK, hd)
        vg = v_blocks[bi, gi, ic].reshape(B, G, SEL_Q_BLOCK, n_sel * SEL_BLOCK, hd)
        kpos = (ic[..., None] * SEL_BLOCK + jnp.arange(SEL_BLOCK)).reshape(B, G, SEL_Q_BLOCK, -1)
        m = (kpos <= tc[None, None, :, None])[:, :, None]
        s = jnp.einsum('bghqd,bgqkd->bghqk', qc, kg).astype(jnp.float32) * scale
        p = masked_softmax(s, m)
        return jnp.einsum('bghqk,bgqkd->bghqd', p.astype(dt), vg)

    o_slc = lax.map(sel_chunk, (q_chunks, idx_chunks, pos_chunks))
    o_slc = o_slc.transpose(1, 2, 3, 0, 4, 5).reshape(B, G, HG, S, hd)

    n_qc = S // Q_BLOCK
    n_wb = WINDOW // Q_BLOCK + 1
    kw = rms_norm(k_win, k_norm_g[2]).transpose(0, 2, 1, 3)
    vw = v_win.transpose(0, 2, 1, 3)
    pad = ((0, 0), (0, 0), (WINDOW, 0), (0, 0))
    kp = jnp.pad(kw, pad).reshape(B, G, (S + WINDOW) // Q_BLOCK, Q_BLOCK, hd)
    vp = jnp.pad(vw, pad).reshape(B, G, (S + WINDOW) // Q_BLOCK, Q_BLOCK, hd)
    kband = jnp.concatenate([kp[:, :, i:i + n_qc] for i in range(n_wb)], axis=3)
    vband = jnp.concatenate([vp[:, :, i:i + n_qc] for i in range(n_wb)], axis=3)
    kpos = jnp.arange(n_qc)[:, None] * Q_BLOCK - WINDOW + jnp.arange(n_wb * Q_BLOCK)[None, :]
    qpos = pos.reshape(n_qc, Q_BLOCK)
    kpb, qpb = kpos[:, None, :], qpos[:, :, None]
    wmask = (kpb <= qpb) & (kpb > qpb - WINDOW) & (kpb >= 0)
    qb = qg.reshape(B, G, HG, n_qc, Q_BLOCK, hd)
    s_win = jnp.einsum('bghcqd,bgckd->bghcqk', qb, kband).astype(jnp.float32) * scale
    p_win = masked_softmax(s_win, wmask)
    o_win = jnp.einsum('bghcqk,bgckd->bghcqd', p_win.astype(dt), vband).reshape(B, G, HG, S, hd)

    g = jax.nn.sigmoid(gate_logits).reshape(B, S, G, HG, 3).transpose(0, 2, 3, 1, 4)
    o = o_cmp * g[..., 0:1] + o_slc * g[..., 1:2] + o_win * g[..., 2:3]
    return o.transpose(0, 3, 1, 2, 4).reshape(B, S, NSA_Q_WIDTH)


def retention(q, k, v, gate, gn_g, gn_b):
    B, S, H, hd = q.shape
    dt = gate.dtype
    half = hd // 2
    inv_freq = ROPE_BASE ** (-jnp.arange(half, dtype=jnp.float32) / half)
    ang = jnp.arange(S, dtype=jnp.float32)[:, None] * inv_freq[None, :]
    cos = jnp.cos(ang)[None, :, None, :]
    sin = jnp.sin(ang)[None, :, None, :]

    def rot(t):
        t = t.astype(jnp.float32)
        t1, t2 = t[..., :half], t[..., half:]
        return jnp.concatenate([t1 * cos - t2 * sin, t1 * sin + t2 * cos], axis=-1)

    qf = rot(q)
    kf = rot(k) * (hd ** -0.5)
    vf = v.astype(jnp.float32)
    C = RET_CHUNK
    n_ch = S // C
    log_g = jnp.log1p(-jnp.exp2(-5.0 - jnp.arange(H, dtype=jnp.float32)))
    n = jnp.arange(C, dtype=jnp.float32)
    diff = n[:, None] - n[None, :]
    inner_decay = jnp.where(diff >= 0, jnp.exp(log_g[:, None, None] * jnp.maximum(diff, 0.0)), 0.0)
    xi = jnp.exp(log_g[:, None] * (n + 1.0))[None, :, :, None]
    zeta = jnp.exp(log_g[:, None] * (C - 1.0 - n))[None, :, :, None]
    chunk_decay = jnp.exp(log_g * C)[None, :, None, None]

    def to_chunks(t):
        return t.reshape(B, n_ch, C, H, hd).transpose(1, 0, 3, 2, 4)

    def step(R, qkv):
        qc, kc, vc = qkv
        a = jnp.einsum('bhnd,bhmd->bhnm', qc, kc) * inner_decay
        o = jnp.einsum('bhnm,bhme->bhne', a, vc) + jnp.einsum('bhnd,bhde->bhne', qc, R) * xi
        R = R * chunk_decay + jnp.einsum('bhmd,bhme->bhde', kc * zeta, vc)
        return R, o

    R0 = jnp.zeros((B, H, hd, hd), jnp.float32)
    _, o = lax.scan(step, R0, (to_chunks(qf), to_chunks(kf), to_chunks(vf)))
    o = o.transpose(1, 0, 3, 2, 4)
    mu = jnp.mean(o, axis=-1, keepdims=True)
    var = jnp.mean(jnp.square(o - mu), axis=-1, keepdims=True)
    y = (o - mu) * lax.rsqrt(var + GN_EPS) * gn_g.astype(jnp.float32) + gn_b.astype(jnp.float32)
    y = y.reshape(B, S, H * hd).astype(dt)
    return jax.nn.silu(gate) * y


def hier_moe(h, w_rg, b_rg, w_re, b_re, w_gate, w_up, w_down):
    B, S, D = h.shape
    T = B * S
    xt = h.reshape(T, D)
    pg = jax.nn.softmax((xt @ w_rg + b_rg).astype(jnp.float32), axis=-1)
    g_top, g_idx = lax.top_k(pg, 1)
    le = (xt @ w_re + b_re).astype(jnp.float32).reshape(T, N_GROUPS, EXPERTS_PER_GROUP)
    le_sel = jnp.take_along_axis(le, g_idx[:, :, None], axis=1)[:, 0]
    pe = jax.nn.softmax(le_sel, axis=-1)
    e_top, e_loc = lax.top_k(pe, TOPK_IN_GROUP)
    w = g_top * e_top / jnp.sum(e_top, axis=-1, keepdims=True)
    e_glob = g_idx * EXPERTS_PER_GROUP + e_loc

    n_assign = T * TOPK_IN_GROUP
    e_flat = e_glob.reshape(-1)
    tok_flat = jnp.repeat(jnp.arange(T, dtype=jnp.int32), TOPK_IN_GROUP)
    w_flat = w.reshape(-1)
    order = jnp.argsort(e_flat)
    e_sorted = e_flat[order]
    counts = jnp.bincount(e_flat, length=N_EXPERTS)
    padded = (counts + MOE_BLOCK - 1) // MOE_BLOCK * MOE_BLOCK
    pad_end = jnp.cumsum(padded)
    pad_start = pad_end - padded
    start = jnp.cumsum(counts) - counts
    dest = pad_start[e_sorted] + (jnp.arange(n_assign) - start[e_sorted])
    n_rows = (n_assign + N_EXPERTS * (MOE_BLOCK - 1) + MOE_BLOCK - 1) // MOE_BLOCK * MOE_BLOCK
    n_blocks = n_rows // MOE_BLOCK
    row_tok = jnp.zeros((n_rows,), jnp.int32).at[dest].set(tok_flat[order])
    row_w = jnp.zeros((n_rows,), w_flat.dtype).at[dest].set(w_flat[order])
    block_expert = jnp.minimum(
        jnp.searchsorted(pad_end, jnp.arange(n_blocks) * MOE_BLOCK, side='right'), N_EXPERTS - 1)

    def expert_block(args):
        tok, wt, e = args
        xb = xt[tok]
        hb = jax.nn.silu(xb @ w_gate[e]) * (xb @ w_up[e])
        return (hb @ w_down[e]) * wt[:, None].astype(xb.dtype)

    y_rows = lax.map(expert_block, (row_tok.reshape(n_blocks, MOE_BLOCK),
                                    row_w.reshape(n_blocks, MOE_BLOCK), block_expert))
    out = jnp.zeros((T, D), h.dtype).at[row_tok].add(y_rows.reshape(n_rows, D))
    return out.reshape(B, S, D)


def setup_inputs(seed: int = 0) -> dict:
    key = jax.random.key(seed)
    ks = jax.random.split(key, 20)
    f32 = jnp.float32
    L = DEPTH
    hd = NSA_HEAD_DIM

    def nrm(k, shape, scale):
        return jax.random.normal(k, shape, f32) * scale

    return {
        'x': nrm(ks[0], (BATCH, SEQ, D_MODEL), 1.0),
        'norm1_g': 1.0 + nrm(ks[1], (L, D_MODEL), 0.02),
        'w_in': nrm(ks[2], (L, D_MODEL, IN_PROJ_WIDTH), D_MODEL ** -0.5),
        'cmp_pos': nrm(ks[3], (L, 2, CMP_BLOCK, hd), 0.02),
        'cmp_w1': nrm(ks[4], (L, 2, CMP_BLOCK, hd, hd), (CMP_BLOCK * hd) ** -0.5),
        'cmp_w2': nrm(ks[5], (L, 2, hd, hd), hd ** -0.5),
        'q_norm_g': 1.0 + nrm(ks[6], (L, hd), 0.02),
        'k_norm_g': 1.0 + nrm(ks[7], (L, 3, hd), 0.02),
        'ret_gn_g': 1.0 + nrm(ks[8], (L, RET_HEADS, RET_HEAD_DIM), 0.02),
        'ret_gn_b': nrm(ks[9], (L, RET_HEADS, RET_HEAD_DIM), 0.02),
        'w_out': nrm(ks[10], (L, D_MIX, D_MODEL), D_MIX ** -0.5),
        'norm2_g': 1.0 + nrm(ks[11], (L, D_MODEL), 0.02),
        'w_router_group': nrm(ks[12], (L, D_MODEL, N_GROUPS), D_MODEL ** -0.5),
        'b_router_group': nrm(ks[13], (L, N_GROUPS), 0.01),
        'w_router_expert': nrm(ks[14], (L, D_MODEL, N_EXPERTS), D_MODEL ** -0.5),
        'b_router_expert': nrm(ks[15], (L, N_EXPERTS), 0.01),
        'w_exp_gate': nrm(ks[16], (L, N_EXPERTS, D_MODEL, D_EXPERT), D_MODEL ** -0.5),
        'w_exp_up': nrm(ks[17], (L, N_EXPERTS, D_MODEL, D_EXPERT), D_MODEL ** -0.5),
        'w_exp_down': nrm(ks[18], (L, N_EXPERTS, D_EXPERT, D_MODEL), D_EXPERT ** -0.5),
    }


def reference(x, norm1_g, w_in, cmp_pos, cmp_w1, cmp_w2, q_norm_g, k_norm_g, ret_gn_g, ret_gn_b,
              w_out, norm2_g, w_router_group, b_router_group, w_router_expert, b_router_expert,
              w_exp_gate, w_exp_up, w_exp_down):
    B, S, _ = x.shape
    sizes = [NSA_Q_WIDTH] + [NSA_KV_WIDTH] * 6 + [NSA_GATE_WIDTH] + [RET_WIDTH] * 4
    offsets = np.cumsum(sizes)[:-1].tolist()
    for l in range(DEPTH):
        h = rms_norm(x, norm1_g[l])
        proj = h @ w_in[l]
        (nq, nkc, nvc, nks, nvs, nkw, nvw, ngate, rq, rk, rv, rg) = jnp.split(proj, offsets, axis=-1)
        kv = lambda t: t.reshape(B, S, NSA_KV_GROUPS, NSA_HEAD_DIM)
        o_nsa = nsa_attention(nq.reshape(B, S, NSA_HEADS, NSA_HEAD_DIM), kv(nkc), kv(nvc), kv(nks), kv(nvs),
                              kv(nkw), kv(nvw), ngate, cmp_pos[l], cmp_w1[l], cmp_w2[l],
                              q_norm_g[l], k_norm_g[l])
        rh = lambda t: t.reshape(B, S, RET_HEADS, RET_HEAD_DIM)
        o_ret = retention(rh(rq), rh(rk), rh(rv), rg, ret_gn_g[l], ret_gn_b[l])
        x = x + jnp.concatenate([o_nsa, o_ret], axis=-1) @ w_out[l]
        h2 = rms_norm(x, norm2_g[l])
        x = x + hier_moe(h2, w_router_group[l], b_router_group[l], w_router_expert[l], b_router_expert[l],
                         w_exp_gate[l], w_exp_up[l], w_exp_down[l])
    return x
```

```python
import contextlib
import numpy as np
import ml_dtypes
import concourse.bass as bass
import concourse.mybir as mybir
from concourse.bass_utils import run_bass_kernel_spmd

F32 = mybir.dt.float32
BF16 = mybir.dt.bfloat16
I32 = mybir.dt.int32
AF = mybir.ActivationFunctionType
ALU = mybir.AluOpType
AX = mybir.AxisListType

S = 2048
D = 2048
NT = 16
HD = 128
NE = 32
DE = 512
NBLK = 64
NROWS = NBLK * 128
RMS_EPS = 1e-6
GN_EPS = 1e-5
BIG = 30000.0


class K:
    def __init__(self, nc, es):
        self.nc = nc
        self.es = es
        self.E = dict(pe=nc.tensor, act=nc.scalar, dve=nc.vector, pool=nc.gpsimd, sp=nc.sync)
        self.semobj = {}
        self.ccnt = {}
        for n in ("pe", "act", "dve", "pool"):
            self.semobj[n] = es.enter_context(nc.semaphore("c_" + n))
            self.ccnt[n] = 0
        self.dq = {}
        for q, n in (("sp", 20), ("pool", 10), ("act", 4)):
            names = []
            for i in range(n):
                nm = "d_%s%d" % (q, i)
                self.semobj[nm] = es.enter_context(nc.semaphore(nm))
                self.ccnt[nm] = 0
                names.append(nm)
            self.dq[q] = [names, 0]
        self.waited = {}
        self.lastw = {}
        self.readers = {}
        self.n_ins = 0

    def sb(self, name, shape, dt):
        return self.es.enter_context(self.nc.sbuf_tensor(name, list(shape), dt))

    def psum(self, name, shape, dt):
        return self.es.enter_context(self.nc.psum_tensor(name, list(shape), dt))

    def _wait(self, eng, tok):
        s, v = tok
        if eng == "pe" and s == "pe":
            return
        if self.waited.get((eng, s), 0) >= v:
            return
        self.E[eng].wait_ge(self.semobj[s], v)
        self.waited[(eng, s)] = v

    def _deps(self, reads, writes):
        toks = {}
        def add(t):
            if t is None:
                return
            if toks.get(t[0], 0) < t[1]:
                toks[t[0]] = t[1]
        for r in reads:
            add(self.lastw.get(r))
        for w in writes:
            add(self.lastw.get(w))
            for s, v in self.readers.get(w, {}).items():
                add((s, v))
        return list(toks.items())

    def _record(self, reads, writes, tok):
        for r in reads:
            d = self.readers.setdefault(r, {})
            if d.get(tok[0], 0) < tok[1]:
                d[tok[0]] = tok[1]
        for w in writes:
            self.lastw[w] = tok
            self.readers[w] = {}

    def op(self, eng, reads, writes, fn):
        for t in self._deps(reads, writes):
            self._wait(eng, t)
        ins = fn(self.E[eng])
        self.ccnt[eng] += 1
        ins.then_inc(self.semobj[eng], 1)
        tok = (eng, self.ccnt[eng])
        self._record(reads, writes, tok)
        self.n_ins += 1
        return tok

    def dma(self, q, reads, writes, fn):
        names, nxt = self.dq[q]
        nm = names[nxt]
        self.dq[q][1] = (nxt + 1) % len(names)
        if self.ccnt[nm] > 0:
            self._wait(q, (nm, self.ccnt[nm]))
        for t in self._deps(reads, writes):
            self._wait(q, t)
        ins = fn(self.E[q])
        self.ccnt[nm] += 16
        ins.then_inc(self.semobj[nm], 16)
        tok = (nm, self.ccnt[nm])
        self._record(reads, writes, tok)
        self.n_ins += 1
        return tok

    def barrier(self):
        for eng in ("pe", "act", "dve", "pool", "sp"):
            for s, v in self.ccnt.items():
                if v > 0:
                    if eng == "pe" and s == "pe":
                        continue
                    self._wait(eng, (s, v))

    def wait_all(self, eng, keys):
        for t in self._deps(keys, keys):
            self._wait(eng, t)


def _consts():
    c = {}
    bf = ml_dtypes.bfloat16
    c["ident"] = np.eye(128, dtype=np.float32).astype(bf)
    pos = np.arange(S)
    half = HD // 2
    inv_freq = 10000.0 ** (-np.arange(half, dtype=np.float64) / half)
    ang = pos[:, None].astype(np.float64) * inv_freq[None, :]
    def tm(a):
        return np.ascontiguousarray(a.reshape(NT, 128, -1).transpose(1, 0, 2)).astype(np.float32)
    c["cos"] = tm(np.cos(ang.astype(np.float32).astype(np.float64)))
    c["sin"] = tm(np.sin(ang.astype(np.float32).astype(np.float64)))
    c["nsin"] = -c["sin"]
    log_g = np.log1p(-np.exp2(-5.0 - np.arange(8, dtype=np.float64)))
    n = np.arange(128, dtype=np.float64)
    scale = HD ** -0.5
    c["xi"] = np.exp(log_g[None, :] * (n[:, None] + 1.0)).astype(np.float32)
    c["zs"] = (np.exp(log_g[None, :] * (127.0 - n[:, None])) * scale).astype(np.float32)
    diff = n[None, :] - n[:, None]
    dt = np.where(diff[:, None, :] >= 0, np.exp(log_g[None, :, None] * np.maximum(diff[:, None, :], 0.0)), 0.0) * scale
    c["DT"] = np.ascontiguousarray(dt).astype(np.float32)
    c["gC"] = [float(np.float32(np.exp(log_g[h] * 128.0))) for h in range(8)]
    ncmp = 127
    cstart = np.arange(ncmp) * 16
    cm = np.zeros((128, S), np.float32)
    cm[:ncmp] = ((cstart + 31)[:, None] <= pos[None, :])
    c["cmaskT"] = cm.astype(bf)
    sel_start = np.arange(32) * 64
    ovl = ((cstart[:, None] < (sel_start + 64)[None, :]) & ((cstart + 32)[:, None] > sel_start[None, :])).astype(np.float32)
    ri = np.zeros((128, 33), np.float32)
    ri[:ncmp, 0] = 1.0
    ri[:ncmp, 1:] = ovl
    c["rhs_imp"] = ri.astype(bf)
    k = np.arange(128)[:, None]
    cc = np.arange(1536)[None, :] - 512
    c["Mc"] = ((cc - k) >= 0).astype(np.float32).astype(bf)
    c["Mw"] = (((cc - k) >= 0) & ((cc - k) < 512)).astype(np.float32).astype(bf)
    c["Efull"] = (np.arange(S)[None, :] // 64 == np.arange(32)[:, None]).astype(np.float32).astype(bf)
    jb = np.arange(32)[None, :]
    cur = (pos // 64)[:, None]
    valid = sel_start[None, :] <= pos[:, None]
    forced = (jb == 0) | (jb == cur) | (jb == cur - 1)
    c["valid_nf"] = tm((valid & ~forced).astype(np.float32)).astype(bf)
    c["forcedbig"] = tm(np.where(valid, np.where(forced, 1e6, 0.0), -1e30)).astype(bf)
    c["Ustrict"] = (np.arange(128)[:, None] < np.arange(128)[None, :]).astype(np.float32).astype(bf)
    c["ones_bf"] = np.ones((128, 128), np.float32).astype(bf)
    c["bvals"] = np.tile((np.arange(NBLK, dtype=np.float32) * 128.0)[None, :], (128, 1))
    return c


CONST_DT = dict(ident=BF16, cos=F32, sin=F32, nsin=F32, xi=F32, zs=F32, DT=F32, cmaskT=BF16, rhs_imp=BF16,
                Mc=BF16, Mw=BF16, Efull=BF16, valid_nf=BF16, forcedbig=BF16, Ustrict=BF16, ones_bf=BF16, bvals=F32)


def build(consts, stage="full"):
    nc = bass.Bass("TRN2", target_bir_lowering=False)
    es = contextlib.ExitStack()
    dbg = {}
    with es:
        k = K(nc, es)

        k.inputs = []

        def din(name, shape, dt=F32):
            k.inputs.append(name)
            return nc.dram_tensor(name, list(shape), dt, kind="ExternalInput").ap()

        x = din("x", [S, D])
        zrow = din("zrow", [128, D], BF16)
        g1 = din("g1", [1, D])
        g2 = din("g2", [1, D])
        wq = din("wq", [2, 128, 16 * 512])
        wkv = din("wkv", [2, 128, 16 * 512])
        ww = din("ww", [2, 128, 16 * 268])
        wret = din("wret", [8, 128, 16 * 512])
        w1 = din("w1", [2, 128, 32 * 128])
        posT = din("posT", [2, 128, 32])
        w2 = din("w2", [2, 128, 128])
        gq = din("gq", [1, 128])
        gk = din("gk", [1, 3 * 128])
        gng = din("gng", [1, 1024])
        gnb = din("gnb", [1, 1024])
        wout = din("wout", [128, 16 * 2048])
        wr = din("wr", [128, 16 * 36])
        br = din("br", [1, 36])
        if stage in ("full", "E", "E4"):
            wg_d = din("w_gate", [NE, 128, 16 * DE])
            wu_d = din("w_up", [NE, 128, 16 * DE])
            wd_d = din("w_down", [NE, 128, 4 * D])
        cd = {n: din("c_" + n, list(consts[n].shape), CONST_DT[n]) for n in CONST_DT}
        y = nc.dram_tensor("y", [S, D], F32, kind="ExternalOutput").ap()
        h2d = nc.dram_tensor("h2d", [S, D], BF16, kind="Internal").ap()
        xs = nc.dram_tensor("xs", [NROWS, D], BF16, kind="Internal").ap()
        ysd = [nc.dram_tensor("ysd%d" % i, [NROWS, D // 2], F32, kind="Internal").ap() for i in range(2)]
        if stage != "full":
            dbgo = nc.dram_tensor("dbg", [128, 16 * 2048], F32, kind="ExternalOutput").ap()

        scopes = [es]

        class scope:
            def __enter__(self_):
                st = contextlib.ExitStack()
                st.__enter__()
                scopes.append(st)
                return st
            def __exit__(self_, *a):
                k.barrier()
                st = scopes.pop()
                return st.__exit__(*a)

        uid = [0]

        def sb(name, shape, dt):
            uid[0] += 1
            return scopes[-1].enter_context(nc.sbuf_tensor("%s_u%d" % (name, uid[0]), list(shape), dt))

        def cload(n):
            shp = list(consts[n].shape)
            t = sb("s_" + n, [shp[0], int(np.prod(shp[1:]))], CONST_DT[n])
            src = cd[n]
            if len(shp) == 3:
                src = src.rearrange("p a b -> p (a b)")
            k.dma("sp", [], ["s_" + n], lambda e: e.dma_start(out=t[:], in_=src))
            return t

        def bload(name, src, w):
            t = sb(name, [128, w], F32)
            k.dma("sp", [], [name], lambda e: e.dma_start(out=t[:], in_=src.partition_broadcast(128)))
            return t

        def wload(name, t, src):
            k.dma("pool", [], [name], lambda e: e.dma_start(out=t, in_=src))

        ident = cload("ident")
        PB = [k.psum("pb%d" % i, [128, 512], F32) for i in range(8)]
        PBb = [p[:].bitcast(BF16) for p in PB]

        def pkey(i):
            return "pb%d" % i

        hT = sb("hT", [128, 16 * S], BF16)
        oT = sb("oTn", [128, 8 * S], BF16)
        hT3 = hT[:].rearrange("p (c t) -> p c t", c=16)
        oT3 = oT[:].rearrange("p (c t) -> p c t", c=8)
        small = sb("small", [128, 64], F32)

        def rstd_from(ssk, ss_ap, out_ap, outk, n, eps):
            k.op("act", [ssk], [outk], lambda e: e.activation(out_ap, ss_ap, AF.Sqrt, bias=float(eps), scale=1.0 / n))
            k.op("dve", [outk], [outk], lambda e: e.reciprocal(out_ap, out_ap))

        def early():
            while len(scopes) > 1:
                scopes.pop().__exit__(None, None, None)

        def dump(keys, src_ap, width):
            dt_ = sb("dbgt", [128, width], F32)
            k.op("act", keys, ["dbgt"], lambda e: e.copy(dt_[:], src_ap))
            k.dma("sp", ["dbgt"], ["dbgo"], lambda e: e.dma_start(out=dbgo[:, 0:width], in_=dt_[:]))
            k.wait_all("sp", ["dbgo"])

        with scope():
            g1b = bload("g1b", g1, D)
            sqs = sb("sqs", [128, 2048], BF16)
            xt = [sb("xt%d" % i, [128, D], F32) for i in range(2)]
            hb = [sb("hb%d" % i, [128, D], BF16) for i in range(2)]

            def a_front(i):
                b = i % 2
                k.dma("sp", [], ["xt%d" % b], lambda e: e.dma_start(out=xt[b][:], in_=x[i * 128:(i + 1) * 128, :]))
                k.op("act", ["xt%d" % b], ["sqs", "ss%d" % b], lambda e: e.activation(sqs[:], xt[b][:], AF.Square, accum_out=small[:, b:b + 1]))
                rstd_from("ss%d" % b, small[:, b:b + 1], small[:, 2 + b:3 + b], "rs%d" % b, D, RMS_EPS)
                k.op("dve", ["rs%d" % b, "xt%d" % b, "g1b"], ["hb%d" % b],
                     lambda e: e.scalar_tensor_tensor(hb[b][:], xt[b][:], small[:, 2 + b:3 + b], g1b[:], ALU.mult, ALU.mult))

            def a_back(i):
                b = i % 2
                for half in range(2):
                    pbv = PBb[2 * b + half]
                    for c in range(8):
                        cc = half * 8 + c
                        k.op("pe", ["hb%d" % b, "s_ident"], [pkey(2 * b + half)],
                             lambda e: e.transpose(pbv[:, c * 128:(c + 1) * 128], hb[b][:, cc * 128:(cc + 1) * 128], ident[:]))
                    k.op("act", [pkey(2 * b + half)], ["hT"],
                         lambda e: e.copy(hT3[:, half * 8:(half + 1) * 8, i * 128:(i + 1) * 128],
                                          pbv.rearrange("p (c t) -> p c t", c=8)))
            a_front(0)
            for i in range(NT):
                if i + 1 < NT:
                    a_front(i + 1)
                a_back(i)

        wbufs = {"wbA": sb("wbA", [128, 16 * 512], BF16)}
        wloaded = {}

        def wprefetch(bk, src, ncols, tag):
            wload(bk, wbufs[bk][:, 0:16 * ncols], src)
            wloaded[bk] = tag
        IPB = [0, 1, 5, 6]

        def inproj(bk, tag, src, ncols, post_a, post_b):
            if wloaded.get(bk) != tag:
                wprefetch(bk, src, ncols, tag)
            w3 = wbufs[bk][:, 0:16 * ncols].rearrange("p (c n) -> p c n", c=16)
            pend = None
            for i in range(NT):
                bank = IPB[i % 4]
                for c in range(16):
                    k.op("pe", ["hT", bk], [pkey(bank)],
                         lambda e: e.matmul(PB[bank][:, 0:ncols], hT3[:, c, i * 128:(i + 1) * 128], w3[:, c, :],
                                            start=(c == 0), stop=(c == 15)))
                if pend is not None:
                    post_b(pend)
                post_a(i, bank)
                pend = i
            post_b(pend)

        def tr_to(src_key, src_ap, n128, tbank, dst_key, dst_ap):
            for j in range(n128):
                k.op("pe", [src_key, "s_ident"], [pkey(tbank)],
                     lambda e: e.transpose(PBb[tbank][:, j * 128:(j + 1) * 128], src_ap[:, j * 128:(j + 1) * 128], ident[:]))
            k.op("act", [pkey(tbank)], [dst_key],
                 lambda e: e.copy(dst_ap, PBb[tbank][:, 0:n128 * 128].rearrange("p (c t) -> p c t", c=n128)))

        def pipeline(blocks):
            n = len(blocks)
            if n == 0:
                return
            slot = lambda i: i % 3
            deferred = []
            for j in range(min(2, n)):
                blocks[j][0](slot(j))
            for i in range(n):
                if i + 2 < n:
                    blocks[i + 2][0](slot(i + 2))
                for d in deferred:
                    d()
                deferred = []
                blocks[i][1](slot(i))
                blocks[i][2](slot(i))
                if blocks[i][3] is not None:
                    blocks[i][3]()
                if blocks[i][4] is not None:
                    deferred.append(blocks[i][4])
            for d in deferred:
                d()

        with scope():
            wbufs["wbB"] = sb("wbB", [128, 16 * 512], BF16)
            gqb = bload("gqb", gq, 128)
            gkb = bload("gkb", gk, 384)
            k.op("dve", ["gqb"], ["gqb"], lambda e: e.tensor_scalar(gqb[:], gqb[:], float(HD ** -0.5), None, ALU.mult))
            sqs = sb("sqsB", [128, 512], F32)
            qn = [sb("qn%d" % i, [128, 512], BF16) for i in range(2)]
            accb = [sb("accb%d" % i, [128, 512], BF16) for i in range(2)]
            qT = sb("qT", [128, 4 * S], BF16); qT3 = qT[:].rearrange("p (h t) -> p h t", h=4)
            ksT = sb("ksT", [128, S], BF16)
            kwT = sb("kwT", [128, S], BF16)
            vs1 = sb("vs1", [128, NT * 129], BF16); vs13 = vs1[:].rearrange("p (i c) -> p i c", i=NT)
            vw1 = sb("vw1", [128, NT * 129], BF16); vw13 = vw1[:].rearrange("p (i c) -> p i c", i=NT)
            vc1 = sb("vc1", [128, 129], BF16)
            gates = sb("gates", [128, NT * 12], F32); gates3 = gates[:].rearrange("p (i c) -> p i c", i=NT)
            kcnT = sb("kcnT", [128, 128], BF16)
            k.op("pool", [], ["vs1"], lambda e: e.memset(vs1[:], 1.0))
            k.op("pool", [], ["vw1"], lambda e: e.memset(vw1[:], 1.0))
            k.op("pool", [], ["vc1"], lambda e: e.memset(vc1[:], 1.0))

            def norm_heads(bank, c0, nh, gtab, outt, outk):
                P = PB[bank]
                k.op("act", [pkey(bank)], ["sqsB"], lambda e: e.activation(sqs[:, 0:nh * 128], P[:, c0:c0 + nh * 128], AF.Square))
                k.op("dve", ["sqsB"], ["ssB"], lambda e: e.tensor_reduce(small[:, 8:8 + nh], sqs[:, 0:nh * 128].rearrange("p (h d) -> p h d", h=nh), AX.X, ALU.add))
                rstd_from("ssB", small[:, 8:8 + nh], small[:, 16:16 + nh], "rsB", HD, RMS_EPS)
                for h in range(nh):
                    k.op("dve", ["rsB", pkey(bank)], [outk],
                         lambda e: e.scalar_tensor_tensor(outt[:, h * 128:(h + 1) * 128], P[:, c0 + h * 128:c0 + (h + 1) * 128],
                                                          small[:, 16 + h:17 + h], gtab, ALU.mult, ALU.mult))

            for g in range(2):
              sc1 = scope(); sc1.__enter__()
              if True:
                cvT = sb("cvT", [128, 2 * S], BF16); cvT3 = cvT[:].rearrange("p (h t) -> p h t", h=2)
                w1s = sb("w1s", [128, 2 * 32 * 128], BF16)
                wload("w1s", w1s[:].rearrange("p (i r) -> p i r", i=2), w1.rearrange("i p r -> p i r"))
                w1v = w1s[:].rearrange("p (i l e) -> p i l e", i=2, l=32)
                w2s = sb("w2s", [128, 2 * 128], BF16)
                wload("w2s", w2s[:].rearrange("p (i r) -> p i r", i=2), w2.rearrange("i p r -> p i r"))
                posTs = sb("posTs", [128, 2 * 32], BF16)
                wload("posTs", posTs[:].rearrange("p (i r) -> p i r", i=2), posT.rearrange("i p r -> p i r"))
                hidT = sb("hidT", [128, 2 * 128], BF16)
                kcn = sb("kcn", [128, 128], BF16)
                cbias = sb("cbias", [128, 2], F32)
                k.op("pool", [], ["kcn"], lambda e: e.memset(kcn[:], 0.0))
                k.op("pool", [], ["hidT"], lambda e: e.memset(hidT[:], 0.0))

                def q_a(i, bank):
                    norm_heads(bank, 0, 4, gqb[:], qn[i % 2], "qn%d" % (i % 2))

                def q_b(i):
                    tr_to("qn%d" % (i % 2), qn[i % 2], 4, 2, "qT", qT3[:, :, i * 128:(i + 1) * 128])
                bq, bkv = ("wbA", "wbB") if g == 0 else ("wbB", "wbA")
                if g == 0:
                    wprefetch(bq, wq[g], 512, "q0")
                wprefetch(bkv, wkv[g], 512, "kv%d" % g)
                inproj(bq, "q%d" % g, wq[g], 512, q_a, q_b)
                wprefetch(bq, ww[g], 268, "w%d" % g)

                def kv_a(i, bank):
                    P = PB[bank]
                    k.op("act", [pkey(bank)], ["qn%d" % (i % 2)], lambda e: e.copy(qn[i % 2][:, 0:256], P[:, 0:256]))
                    norm_heads(bank, 256, 1, gkb[:, 128:256], accb[i % 2], "accb%d" % (i % 2))
                    k.op("act", [pkey(bank)], ["vs1"], lambda e: e.copy(vs13[:, i, 0:128], P[:, 384:512]))

                def kv_b(i):
                    tr_to("qn%d" % (i % 2), qn[i % 2], 2, 2, "cvT", cvT3[:, :, i * 128:(i + 1) * 128])
                    tr_to("accb%d" % (i % 2), accb[i % 2], 1, 3, "ksT", ksT[:, i * 128:(i + 1) * 128].rearrange("p (c t) -> p c t", c=1))
                inproj(bkv, "kv%d" % g, wkv[g], 512, kv_a, kv_b)
                if g == 0:
                    wprefetch(bkv, wq[1], 512, "q1")

                def w_a(i, bank):
                    P = PB[bank]
                    norm_heads(bank, 0, 1, gkb[:, 256:384], accb[i % 2], "accb%d" % (i % 2))
                    k.op("act", [pkey(bank)], ["vw1"], lambda e: e.copy(vw13[:, i, 0:128], P[:, 128:256]))
                    k.op("act", [pkey(bank)], ["gates"], lambda e: e.activation(gates3[:, i, :], P[:, 256:268], AF.Sigmoid))

                def w_b(i):
                    tr_to("accb%d" % (i % 2), accb[i % 2], 1, 3, "kwT", kwT[:, i * 128:(i + 1) * 128].rearrange("p (c t) -> p c t", c=1))
                inproj(bq, "w%d" % g, ww[g], 268, w_a, w_b)
                if g == 1:
                    wprefetch("wbA", wret[0], 512, "r0")

                for ci in range(2):
                    src = cvT3[:, ci, :]
                    for l in range(32):
                        k.op("pe", ["w1s", "posTs"], [pkey(4)],
                             lambda e: e.matmul(PB[4][:, 0:1], w1v[:, ci, l, :], posTs[:, ci * 32 + l:ci * 32 + l + 1], start=(l == 0), stop=(l == 31)))
                    k.op("act", [pkey(4)], ["cbias"], lambda e: e.copy(cbias[:, ci:ci + 1], PB[4][:, 0:1]))
                    for l in range(32):
                        k.op("pe", ["w1s", "cvT"], [pkey(5)],
                             lambda e: e.matmul(PB[5][:, 0:127], w1v[:, ci, l, :], src[:, l:l + 16 * 126 + 1:16], start=(l == 0), stop=(l == 31)))
                    k.op("act", [pkey(5), "cbias"], ["hidT"],
                         lambda e: e.activation(hidT[:, ci * 128:ci * 128 + 127], PB[5][:, 0:127], AF.Silu, bias=cbias[:, ci:ci + 1]))
                    k.op("pe", ["hidT", "w2s"], [pkey(4)],
                         lambda e: e.matmul(PB[4][0:127, 0:128], hidT[:, ci * 128:ci * 128 + 127], w2s[:, ci * 128:(ci + 1) * 128], start=True, stop=True))
                    if ci == 0:
                        P = PB[4]
                        k.op("act", [pkey(4)], ["sqsB"], lambda e: e.activation(sqs[0:127, 0:128], P[0:127, 0:128], AF.Square, accum_out=small[0:127, 24:25]))
                        rstd_from("sqsB", small[0:127, 24:25], small[0:127, 25:26], "rsC", HD, RMS_EPS)
                        k.op("dve", ["rsC", pkey(4)], ["kcn"],
                             lambda e: e.scalar_tensor_tensor(kcn[0:127, :], P[0:127, 0:128], small[0:127, 25:26], gkb[0:127, 0:128], ALU.mult, ALU.mult))
                        tr_to("kcn", kcn, 1, 3, "kcnT", kcnT[:].rearrange("p (c t) -> p c t", c=1))
                    else:
                        k.op("act", [pkey(4)], ["vc1"], lambda e: e.copy(vc1[0:127, 0:128], PB[4][0:127, 0:128]))
              sc1.__exit__(None, None, None)
              sc2 = scope(); sc2.__enter__()
              if True:
                cmaskT = cload("cmaskT"); rhs_imp = cload("rhs_imp"); Mc = cload("Mc"); Mw = cload("Mw")
                Efull = cload("Efull"); valid_nf = cload("valid_nf"); forcedbig = cload("forcedbig")
                imp = sb("imp", [128, NT * 32], F32); imp3 = imp[:].rearrange("p (i c) -> p i c", i=NT)
                score = sb("score", [128, NT * 32], F32); score3 = score[:].rearrange("p (i c) -> p i c", i=NT)
                selneg = sb("selneg", [128, NT * 32], BF16); selneg3 = selneg[:].rearrange("p (i c) -> p i c", i=NT)
                selnegT = sb("selnegT", [32, S], BF16)
                m8 = sb("m8", [128, NT * 8], F32); m83 = m8[:].rearrange("p (i c) -> p i c", i=NT)
                Et = [sb("Et%d" % i, [128, 512], BF16) for i in range(3)]
                acc = sb("acc", [128, 512], F32)
                accq = [sb("accq%d" % i, [128, 512], BF16) for i in range(2)]
                Osb = [sb("Osb%d" % i, [128, 4 * 129], F32) for i in range(2)]
                rrp = [sb("rrp%d" % i, [128, 4], F32) for i in range(2)]
                rr1 = [sb("rr1_%d" % i, [128, 4], F32) for i in range(2)]

                def S_cmp(hh, qb):
                    def f(sl):
                        k.op("pe", ["kcnT", "qT"], [pkey(sl)],
                             lambda e: e.matmul(PB[sl][:, :], kcnT[:], qT3[:, hh, qb * 512:(qb + 1) * 512], start=True, stop=True))
                    return f

                def post_cmp(qb):
                    def f(sl):
                        k.op("act", [pkey(sl)], ["Et%d" % sl], lambda e: e.activation(Et[sl][:], PB[sl][:, :], AF.Exp))
                        k.op("dve", ["Et%d" % sl, "s_cmaskT"], ["Et%d" % sl],
                             lambda e: e.tensor_tensor(Et[sl][:], Et[sl][:], cmaskT[:, qb * 512:(qb + 1) * 512], ALU.mult))
                    return f

                blocks = []
                cnt_ = [0]
                for hh in range(4):
                    for qb in range(4):
                        def mk(hh=hh, qb=qb, idx=cnt_[0]):
                            rb = 5 + idx % 2
                            R3 = PB[rb][:, 0:4 * 64].rearrange("p (s c) -> p s c", s=4)[:, :, 0:33]
                            r1 = rr1[idx % 2]

                            def pvf(sl):
                                for st in range(4):
                                    k.op("pe", ["Et%d" % sl, "s_rhs_imp"], [pkey(rb)],
                                         lambda e: e.matmul(R3[:, st, :], Et[sl][:, st * 128:(st + 1) * 128], rhs_imp[:], start=True, stop=True))

                            def tail():
                                kr = "rr1_%d" % (idx % 2)
                                k.op("dve", [pkey(rb)], [kr], lambda e: e.tensor_scalar(r1[:, 0:4], R3[:, :, 0], 1e-30, None, ALU.max))
                                k.op("dve", [kr], [kr], lambda e: e.reciprocal(r1[:, 0:4], r1[:, 0:4]))
                                for st in range(4):
                                    ti = qb * 4 + st
                                    if hh == 0:
                                        k.op("dve", [kr, pkey(rb)], ["imp"],
                                             lambda e: e.tensor_scalar(imp3[:, ti, :], R3[:, st, 1:33], r1[:, st:st + 1], None, ALU.mult))
                                    else:
                                        k.op("dve", [kr, pkey(rb), "imp"], ["imp"],
                                             lambda e: e.scalar_tensor_tensor(imp3[:, ti, :], R3[:, st, 1:33], r1[:, st:st + 1], imp3[:, ti, :], ALU.mult, ALU.add))
                            return (S_cmp(hh, qb), post_cmp(qb), pvf, tail, None)
                        blocks.append(mk())
                        cnt_[0] += 1
                pipeline(blocks)

                k.op("dve", ["imp", "s_valid_nf"], ["score"], lambda e: e.tensor_tensor(score[:], imp[:], valid_nf[:], ALU.mult))
                k.op("dve", ["score", "s_forcedbig"], ["score"], lambda e: e.tensor_tensor(score[:], score[:], forcedbig[:], ALU.add))
                for ti in range(NT):
                    k.op("dve", ["score"], ["m8"], lambda e: e.max(m83[:, ti, :], score3[:, ti, :]))
                k.op("dve", ["score", "m8"], ["score"],
                     lambda e: e.tensor_tensor(score3, score3, m83[:, :, 7:8].broadcast_to([128, NT, 32]), ALU.is_ge))
                k.op("dve", ["score"], ["selneg"], lambda e: e.tensor_scalar(selneg[:], score[:], BIG, -BIG, ALU.mult, ALU.add))
                for half in range(2):
                    for j in range(8):
                        ti = half * 8 + j
                        k.op("pe", ["selneg", "s_ident"], [pkey(3)],
                             lambda e: e.transpose(PBb[3][0:32, j * 128:(j + 1) * 128], selneg3[:, ti, :], ident[:]))
                    k.op("act", [pkey(3)], ["selnegT"], lambda e: e.copy(selnegT[:, half * 1024:(half + 1) * 1024], PBb[3][0:32, :]))

                fbn = [0]

                def finish_branch(qb, col, first):
                    j = fbn[0] % 2
                    fbn[0] += 1
                    ok_ = "Osb%d" % j
                    O_ = Osb[j][:].rearrange("p (s c) -> p s c", s=4)
                    for st in range(4):
                        k.op("dve", [pkey(4 + st)], [ok_], lambda e: e.tensor_copy(O_[:, st, :], PB[4 + st][:, 0:129]))
                    rk = "rrp%d" % j
                    k.op("dve", [ok_], [rk], lambda e: e.tensor_scalar(rrp[j][:, 0:4], O_[:, :, 128], 1e-30, None, ALU.max))
                    k.op("dve", [rk], [rk], lambda e: e.reciprocal(rrp[j][:, 0:4], rrp[j][:, 0:4]))
                    k.op("pool", [rk, "gates"], [rk], lambda e: e.tensor_tensor(rrp[j][:, 0:4], gates3[:, qb * 4:qb * 4 + 4, col], rrp[j][:, 0:4], ALU.mult))
                    rb_ = rrp[j][:, 0:4].unsqueeze(2).broadcast_to([128, 4, 128])
                    acc3 = acc[:].rearrange("p (s c) -> p s c", s=4)
                    if first:
                        k.op("pool", [rk, ok_], ["acc"], lambda e: e.tensor_tensor(acc3, O_[:, :, 0:128], rb_, ALU.mult))
                    else:
                        k.op("pool", [rk, ok_], [ok_], lambda e: e.tensor_tensor(O_[:, :, 0:128], O_[:, :, 0:128], rb_, ALU.mult))
                        k.op("pool", [ok_, "acc"], ["acc"], lambda e: e.tensor_tensor(acc3, acc3, O_[:, :, 0:128], ALU.add))

                def pv(sl, vkey, vap, first_of, last_of, sts):
                    for st in sts:
                        k.op("pe", ["Et%d" % sl, vkey], [pkey(4 + st)],
                             lambda e: e.matmul(PB[4 + st][:, 0:129], Et[sl][:, st * 128:(st + 1) * 128], vap, start=first_of(st), stop=last_of(st)))

                blocks = []
                it_ = [0]
                for hh in range(4):
                    for qb in range(4):
                        def mk_c(hh=hh, qb=qb):
                            def pvf(sl):
                                pv(sl, "vc1", vc1[:], lambda st: True, lambda st: True, range(4))
                            return (S_cmp(hh, qb), post_cmp(qb), pvf, lambda: finish_branch(qb, hh * 3 + 0, True), None)
                        blocks.append(mk_c())
                        nkb = 4 * qb + 4
                        for kb in range(nkb):
                            def mk_s(hh=hh, qb=qb, kb=kb, nkb=nkb):
                                def sf(sl):
                                    k.op("pe", ["ksT", "qT"], [pkey(sl)],
                                         lambda e: e.matmul(PB[sl][:, :], ksT[:, kb * 128:(kb + 1) * 128], qT3[:, hh, qb * 512:(qb + 1) * 512], start=True, stop=False))
                                    k.op("pe", ["s_Efull", "selnegT"], [pkey(sl)],
                                         lambda e: e.matmul(PB[sl][:, :], Efull[0:32, kb * 128:(kb + 1) * 128], selnegT[:, qb * 512:(qb + 1) * 512], start=False, stop=True))

                                def pf(sl):
                                    k.op("act", [pkey(sl)], ["Et%d" % sl], lambda e: e.activation(Et[sl][:], PB[sl][:, :], AF.Exp))
                                    o = kb * 128 - qb * 512
                                    if o >= 0:
                                        k.op("dve", ["Et%d" % sl, "s_Mc"], ["Et%d" % sl],
                                             lambda e: e.tensor_tensor(Et[sl][:], Et[sl][:], Mc[:, 512 - o:1024 - o], ALU.mult))

                                def pvf(sl):
                                    sts = [st for st in range(4) if kb * 128 <= qb * 512 + st * 128 + 127]
                                    pv(sl, "vs1", vs13[:, kb, :], lambda st: kb == 0, lambda st: kb == 4 * qb + st, sts)
                                tail = (lambda: finish_branch(qb, hh * 3 + 1, False)) if kb == nkb - 1 else None
                                return (sf, pf, pvf, tail, None)
                            blocks.append(mk_s())
                        kb0 = max(0, 4 * qb - 4)
                        kbl = 4 * qb + 3
                        for kb in range(kb0, kbl + 1):
                            def mk_w(hh=hh, qb=qb, kb=kb, kbl=kbl, itn=it_[0]):
                                def sf(sl):
                                    k.op("pe", ["kwT", "qT"], [pkey(sl)],
                                         lambda e: e.matmul(PB[sl][:, :], kwT[:, kb * 128:(kb + 1) * 128], qT3[:, hh, qb * 512:(qb + 1) * 512], start=True, stop=True))

                                def pf(sl):
                                    k.op("act", [pkey(sl)], ["Et%d" % sl], lambda e: e.activation(Et[sl][:], PB[sl][:, :], AF.Exp))
                                    o = kb * 128 - qb * 512
                                    k.op("dve", ["Et%d" % sl, "s_Mw"], ["Et%d" % sl],
                                         lambda e: e.tensor_tensor(Et[sl][:], Et[sl][:], Mw[:, 512 - o:1024 - o], ALU.mult))

                                def pvf(sl):
                                    sts = [st for st in range(4) if 4 * qb + st - 4 <= kb <= 4 * qb + st]
                                    pv(sl, "vw1", vw13[:, kb, :], lambda st: kb == max(0, 4 * qb + st - 4), lambda st: kb == 4 * qb + st, sts)
                                tail = None
                                tail_pe = None
                                if kb == kbl:
                                    aq = accq[itn % 2]
                                    kq = "accq%d" % (itn % 2)

                                    def tail():
                                        finish_branch(qb, hh * 3 + 2, False)
                                        k.op("act", ["acc"], [kq], lambda e: e.copy(aq[:], acc[:]))

                                    def tail_pe():
                                        tr_to(kq, aq, 4, 3, "oT", oT3[:, 4 * g + hh, qb * 512:(qb + 1) * 512].rearrange("p (c t) -> p c t", c=4))
                                return (sf, pf, pvf, tail, tail_pe)
                            blocks.append(mk_w())
                        it_[0] += 1
                pipeline(blocks)
              sc2.__exit__(None, None, None)
        if stage == "C":
            dump(["oT"], oT[:, 0:2 * S], 2 * S)
            return nc, k

        oTr = sb("oTr", [128, 8 * S], BF16)
        oTr3 = oTr[:].rearrange("p (c t) -> p c t", c=8)
        with scope():
            cos = cload("cos"); sin = cload("sin"); xi = cload("xi"); zs = cload("zs")
            cos3 = cos[:].rearrange("p (i c) -> p i c", i=NT)
            sin3 = sin[:].rearrange("p (i c) -> p i c", i=NT)
            DTh = sb("DTh", [128, 128], F32)
            gngh = sb("gngh", [128, 128], F32)
            gnbh = sb("gnbh", [128, 128], F32)
            rot32 = sb("rot32", [128, 256], F32)
            ta = sb("ta", [128, 128], F32)
            tb = sb("tb", [128, 128], F32)
            rotb = [sb("rotb%d" % i, [128, 256], BF16) for i in range(2)]
            qkT = sb("qkT", [128, 2 * S], BF16); qkT3 = qkT[:].rearrange("p (a t) -> p a t", a=2)
            kz = sb("kz", [128, NT * 128], BF16); kz3 = kz[:].rearrange("p (i c) -> p i c", i=NT)
            rvb = sb("rvb", [128, NT * 128], BF16); rvb3 = rvb[:].rearrange("p (i c) -> p i c", i=NT)
            rgs = sb("rgs", [128, NT * 128], BF16); rgs3 = rgs[:].rearrange("p (i c) -> p i c", i=NT)
            At16 = sb("At16", [128, NT * 128], BF16); At3 = At16[:].rearrange("p (i c) -> p i c", i=NT)
            o32a = sb("o32a", [128, NT * 128], F32); o3 = o32a[:].rearrange("p (i c) -> p i c", i=NT)
            Rball = sb("Rball", [128, NT * 128], BF16); Rb3 = Rball[:].rearrange("p (i c) -> p i c", i=NT)
            ob16a = sb("ob16a", [128, NT * 128], BF16)
            R32 = [sb("R32_%d" % i, [128, 128], F32) for i in range(2)]
            stt_ = sb("stt", [128, 96], F32)
            for h in range(8):
                k.dma("sp", [], ["DTh"], lambda e: e.dma_start(out=DTh[:], in_=cd["DT"][:, h, :]))
                k.dma("sp", [], ["gngh"], lambda e: e.dma_start(out=gngh[:], in_=gng[:, h * 128:(h + 1) * 128].partition_broadcast(128)))
                k.dma("sp", [], ["gnbh"], lambda e: e.dma_start(out=gnbh[:], in_=gnb[:, h * 128:(h + 1) * 128].partition_broadcast(128)))

                def r_a(i, bank):
                    P = PB[bank]
                    P4 = P[:, 0:256].rearrange("p (a b c) -> p a b c", a=2, b=2)
                    t1 = P4[:, :, 0, :]
                    t2 = P4[:, :, 1, :]
                    r4 = rot32[:].rearrange("p (a b c) -> p a b c", a=2, b=2)
                    ta3 = ta[:].rearrange("p (a c) -> p a c", a=2)
                    tb3 = tb[:].rearrange("p (a c) -> p a c", a=2)
                    cb = cos3[:, i, :].unsqueeze(1).broadcast_to([128, 2, 64])
                    sbb = sin3[:, i, :].unsqueeze(1).broadcast_to([128, 2, 64])
                    k.op("dve", [pkey(bank), "s_cos"], ["ta"], lambda e: e.tensor_tensor(ta3, t1, cb, ALU.mult))
                    k.op("dve", [pkey(bank), "s_sin"], ["tb"], lambda e: e.tensor_tensor(tb3, t2, sbb, ALU.mult))
                    k.op("dve", ["ta", "tb"], ["rot32"], lambda e: e.tensor_tensor(r4[:, :, 0, :], ta3, tb3, ALU.subtract))
                    k.op("dve", [pkey(bank), "s_sin"], ["ta"], lambda e: e.tensor_tensor(ta3, t1, sbb, ALU.mult))
                    k.op("dve", [pkey(bank), "s_cos"], ["tb"], lambda e: e.tensor_tensor(tb3, t2, cb, ALU.mult))
                    k.op("dve", ["ta", "tb"], ["rot32"], lambda e: e.tensor_tensor(r4[:, :, 1, :], ta3, tb3, ALU.add))
                    k.op("act", ["rot32"], ["rotb%d" % (i % 2)], lambda e: e.copy(rotb[i % 2][:], rot32[:]))
                    k.op("pool", ["rot32", "s_zs"], ["kz"], lambda e: e.tensor_scalar(kz3[:, i, :], rot32[:, 128:256], zs[:, h:h + 1], None, ALU.mult))
                    k.op("act", [pkey(bank)], ["rvb"], lambda e: e.copy(rvb3[:, i, :], P[:, 256:384]))
                    k.op("act", [pkey(bank)], ["rgs"], lambda e: e.activation(rgs3[:, i, :], P[:, 384:512], AF.Silu))

                def r_b(i):
                    tr_to("rotb%d" % (i % 2), rotb[i % 2], 2, 2, "qkT", qkT3[:, :, i * 128:(i + 1) * 128])
                inproj("wbA", "r%d" % h, wret[h], 512, r_a, r_b)
                if h == 0:
                    for b in range(NBLK):
                        k.dma("sp", [], ["xs_z%d" % b], lambda e: e.dma_start(out=xs[b * 128:(b + 1) * 128, :], in_=zrow[:, :]))
                if h + 1 < 8:
                    wprefetch("wbA", wret[h + 1], 512, "r%d" % (h + 1))
                else:
                    for c in range(16):
                        wload("hT", hT3[:, c, :], wout[:, c * D:(c + 1) * D])

                cs_ = lambda c: slice(c * 128, (c + 1) * 128)
                AB = [3, 4]
                OB = [5, 6]

                def emitA(c):
                    k.op("pe", ["qkT"], [pkey(AB[c % 2])], lambda e: e.matmul(PB[AB[c % 2]][:, 0:128], qkT3[:, 1, cs_(c)], qkT3[:, 0, cs_(c)], start=True, stop=True))
                    k.op("dve", [pkey(AB[c % 2]), "DTh"], ["At16"], lambda e: e.tensor_tensor(At3[:, c, :], PB[AB[c % 2]][:, 0:128], DTh[:], ALU.mult))
                emitA(0)
                for c in range(NT):
                    if c + 1 < NT:
                        emitA(c + 1)
                    k.op("pe", ["At16", "rvb"], [pkey(OB[c % 2])], lambda e: e.matmul(PB[OB[c % 2]][:, 0:128], At3[:, c, :], rvb3[:, c, :], start=True, stop=True))
                    k.op("act", [pkey(OB[c % 2])], ["o32a"], lambda e: e.copy(o3[:, c, :], PB[OB[c % 2]][:, 0:128]))
                for c in range(NT - 1):
                    bd = AB[c % 2]
                    k.op("pe", ["kz", "rvb"], [pkey(bd)], lambda e: e.matmul(PB[bd][:, 0:128], kz3[:, c, :], rvb3[:, c, :], start=True, stop=True))
                    if c == 0:
                        k.op("dve", [pkey(bd)], ["R32_0"], lambda e: e.tensor_copy(R32[0][:], PB[bd][:, 0:128]))
                    else:
                        k.op("dve", [pkey(bd), "R32_%d" % ((c - 1) % 2)], ["R32_%d" % (c % 2)],
                             lambda e: e.scalar_tensor_tensor(R32[c % 2][:], R32[(c - 1) % 2][:], float(consts["gC"][h]), PB[bd][:, 0:128], ALU.mult, ALU.add))
                    k.op("act", ["R32_%d" % (c % 2)], ["Rball"], lambda e: e.copy(Rb3[:, c + 1, :], R32[c % 2][:]))
                for c in range(1, NT):
                    bo = OB[c % 2]
                    k.op("pe", ["qkT", "Rball"], [pkey(bo)], lambda e: e.matmul(PB[bo][:, 0:128], qkT3[:, 0, cs_(c)], Rb3[:, c, :], start=True, stop=True))
                    k.op("dve", [pkey(bo), "o32a", "s_xi"], ["o32a"],
                         lambda e: e.scalar_tensor_tensor(o3[:, c, :], PB[bo][:, 0:128], xi[:, h:h + 1], o3[:, c, :], ALU.mult, ALU.add))
                k.op("dve", ["o32a"], ["gsum"], lambda e: e.tensor_reduce(stt_[:, 0:16], o3, AX.X, ALU.add))
                for c in range(NT):
                    k.op("act", ["o32a"], ["ta", "gsq"], lambda e: e.activation(ta[:], o3[:, c, :], AF.Square, accum_out=stt_[:, 16 + c:17 + c]))
                k.op("dve", ["gsum"], ["gsum"], lambda e: e.tensor_scalar(stt_[:, 0:16], stt_[:, 0:16], 1.0 / 128, None, ALU.mult))
                k.op("dve", ["gsum"], ["gmsq"], lambda e: e.tensor_tensor(stt_[:, 32:48], stt_[:, 0:16], stt_[:, 0:16], ALU.mult))
                k.op("dve", ["gsq", "gmsq"], ["gvar"], lambda e: e.scalar_tensor_tensor(stt_[:, 48:64], stt_[:, 16:32], 1.0 / 128, stt_[:, 32:48], ALU.mult, ALU.subtract))
                k.op("act", ["gvar"], ["grs"], lambda e: e.activation(stt_[:, 64:80], stt_[:, 48:64], AF.Sqrt, bias=float(GN_EPS), scale=1.0))
                k.op("dve", ["grs"], ["grs"], lambda e: e.reciprocal(stt_[:, 64:80], stt_[:, 64:80]))
                k.op("dve", ["o32a", "gsum"], ["o32a"], lambda e: e.tensor_tensor(o3, o3, stt_[:, 0:16].unsqueeze(2).broadcast_to([128, NT, 128]), ALU.subtract))
                k.op("dve", ["o32a", "grs"], ["o32a"], lambda e: e.tensor_tensor(o3, o3, stt_[:, 64:80].unsqueeze(2).broadcast_to([128, NT, 128]), ALU.mult))
                k.op("dve", ["o32a", "gngh"], ["o32a"], lambda e: e.tensor_tensor(o3, o3, gngh[:].unsqueeze(1).broadcast_to([128, NT, 128]), ALU.mult))
                k.op("dve", ["o32a", "gnbh"], ["o32a"], lambda e: e.tensor_tensor(o3, o3, gnbh[:].unsqueeze(1).broadcast_to([128, NT, 128]), ALU.add))
                k.op("dve", ["o32a", "rgs"], ["ob16a"], lambda e: e.tensor_tensor(ob16a[:], o32a[:], rgs[:], ALU.mult))
                for half in range(2):
                    tr_to("ob16a", ob16a[:, half * 1024:(half + 1) * 1024], 8, 2 + half, "oTr",
                          oTr3[:, h, half * 1024:(half + 1) * 1024].rearrange("p (c t) -> p c t", c=8))
        if stage == "D":
            dump(["oTr"], oTr[:, 0:2 * S], 2 * S)
            return nc, k

        wo3 = hT3
        lgt = sb("lgt", [128, NT * 36], F32)
        lg3 = lgt[:].rearrange("p (i c) -> p i c", i=NT)
        with scope():
            g2b = bload("g2b", g2, D)
            brb = bload("brb", br, 36)
            wrs = sb("wrs", [128, 16 * 36], BF16)
            wload("wrs", wrs[:], wr)
            wrs3 = wrs[:].rearrange("p (c n) -> p c n", c=16)
            xt2 = [sb("xt2_%d" % i, [128, D], F32) for i in range(2)]
            x1t = [sb("x1t_%d" % i, [128, D], F32) for i in range(2)]
            sq2 = sb("sq2", [128, D], BF16)
            h2t = [sb("h2t_%d" % i, [128, D], BF16) for i in range(2)]
            h2T = sb("h2T", [128, 16 * 128], BF16)
            h2T3 = h2T[:].rearrange("p (c t) -> p c t", c=16)

            def e1_front(i):
                b = i % 2
                ts_ = slice(i * 128, (i + 1) * 128)
                k.dma("sp", [], ["xt2_%d" % b], lambda e: e.dma_start(out=xt2[b][:], in_=x[ts_, :]))
                for nb in range(4):
                    for c in range(16):
                        src = oT3[:, c, ts_] if c < 8 else oTr3[:, c - 8, ts_]
                        k.op("pe", ["oT", "oTr", "hT"], [pkey(nb)],
                             lambda e: e.matmul(PB[nb][:, :], src, wo3[:, c, nb * 512:(nb + 1) * 512], start=(c == 0), stop=(c == 15)))
                    k.op("dve", [pkey(nb), "xt2_%d" % b], ["x1t_%d" % b],
                         lambda e: e.tensor_tensor(x1t[b][:, nb * 512:(nb + 1) * 512], PB[nb][:, :], xt2[b][:, nb * 512:(nb + 1) * 512], ALU.add))
                k.dma("sp", ["x1t_%d" % b], ["y"], lambda e: e.dma_start(out=y[ts_, :], in_=x1t[b][:]))
                k.op("act", ["x1t_%d" % b], ["sq2", "ss2_%d" % b], lambda e: e.activation(sq2[:], x1t[b][:], AF.Square, accum_out=small[:, 32 + b:33 + b]))
                rstd_from("ss2_%d" % b, small[:, 32 + b:33 + b], small[:, 34 + b:35 + b], "rs2_%d" % b, D, RMS_EPS)
                k.op("dve", ["rs2_%d" % b, "x1t_%d" % b, "g2b"], ["h2t_%d" % b],
                     lambda e: e.scalar_tensor_tensor(h2t[b][:], x1t[b][:], small[:, 34 + b:35 + b], g2b[:], ALU.mult, ALU.mult))
                k.dma("sp", ["h2t_%d" % b], ["h2d"], lambda e: e.dma_start(out=h2d[ts_, :], in_=h2t[b][:]))

            def e1_back(i):
                b = i % 2
                for half in range(2):
                    tr_to("h2t_%d" % b, h2t[b][:, half * 1024:(half + 1) * 1024], 8, 4 + half, "h2T", h2T3[:, half * 8:(half + 1) * 8, :])
                for c in range(16):
                    k.op("pe", ["h2T", "wrs"], [pkey(6)],
                         lambda e: e.matmul(PB[6][:, 0:36], h2T3[:, c, :], wrs3[:, c, :], start=(c == 0), stop=(c == 15)))
                k.op("dve", [pkey(6), "brb"], ["lgt"], lambda e: e.tensor_tensor(lg3[:, i, :], PB[6][:, 0:36], brb[:], ALU.add))
            e1_front(0)
            for i in range(NT):
                if i + 1 < NT:
                    e1_front(i + 1)
                e1_back(i)
        if stage == "E1":
            dump(["lgt"], lgt[:], NT * 36)
            k.wait_all("sp", ["y", "h2d"])
            return nc, k

        wsets = [
            (("wgA", hT[:, 0:8192]), ("wuA", hT[:, 8192:16384]), ("wdA", hT[:, 16384:24576])),
            (("wgB", hT[:, 24576:32768]), ("wuB", oT[:, 0:8192]), ("wdB", oT[:, 8192:16384])),
        ]

        def load_w(ex, extra=()):
            (kg, wgt), (ku, wut), (kd, wdt) = wsets[ex % 2]
            k.dma("pool", [], [kg] + list(extra), lambda e: e.dma_start(out=wgt, in_=wg_d[ex]))
            k.dma("pool", [], [ku] + list(extra), lambda e: e.dma_start(out=wut, in_=wu_d[ex]))
            k.dma("pool", [], [kd] + list(extra), lambda e: e.dma_start(out=wdt, in_=wd_d[ex]))

        rt = scope(); rt.__enter__()
        ones_bf = cload("ones_bf"); Ust = cload("Ustrict"); bvals = cload("bvals")

        def Rr(name, w, dt=F32):
            return sb(name, [128, w], dt)
        gmax = Rr("gmax", 16); gsh = Rr("gsh", 64); gsum = Rr("gsum", 16); gtop = Rr("gtop", 16)
        ohg = Rr("ohg", 64); msk = Rr("msk", 512); m8e = Rr("m8e", 128)
        A1 = Rr("A1", 512); A2 = Rr("A2", 512); Abf = Rr("Abf", 512, BF16); dl = Rr("dl", 16); wt1 = Rr("wt1", 16); wt2 = Rr("wt2", 16)
        rank = Rr("rank", 512); pstart = Rr("pstart", 32)
        d0f = Rr("d0f", 16); d1f = Rr("d1f", 16); d0i = Rr("d0i", 16, I32); d1i = Rr("d1i", 16, I32)
        v3 = lambda t, c: t[:].rearrange("p (i c) -> p i c", c=c)
        gl = lg3[:, :, 0:4]
        le4 = lg3[:, :, 4:36].rearrange("p i (g e) -> p i g e", g=4)
        k.op("dve", ["lgt"], ["gmax"], lambda e: e.tensor_reduce(gmax[:], gl, AX.X, ALU.max))
        k.op("dve", ["lgt", "gmax"], ["gsh"], lambda e: e.tensor_tensor(v3(gsh, 4), gl, gmax[:].unsqueeze(2).broadcast_to([128, 16, 4]), ALU.subtract))
        k.op("dve", ["gsh"], ["ohg"], lambda e: e.tensor_scalar(ohg[:], gsh[:], 0.0, None, ALU.is_ge))
        k.op("act", ["gsh"], ["gsh"], lambda e: e.activation(gsh[:], gsh[:], AF.Exp))
        k.op("dve", ["gsh"], ["gsum"], lambda e: e.tensor_reduce(gsum[:], v3(gsh, 4), AX.X, ALU.add))
        k.op("dve", ["gsum"], ["gtop"], lambda e: e.reciprocal(gtop[:], gsum[:]))
        k.op("dve", ["ohg"], ["ohg"], lambda e: e.tensor_scalar(ohg[:], ohg[:], 1e30, -1e30, ALU.mult, ALU.add))
        k.op("dve", ["lgt", "ohg"], ["msk"],
             lambda e: e.tensor_tensor(msk[:].rearrange("p (i g e) -> p i g e", i=16, g=4), le4,
                                       v3(ohg, 4).unsqueeze(3).broadcast_to([128, 16, 4, 8]), ALU.add))
        for i in range(NT):
            k.op("dve", ["msk"], ["m8e"], lambda e: e.max(v3(m8e, 8)[:, i, :], v3(msk, 32)[:, i, :]))
        k.op("dve", ["msk", "m8e"], ["A1"], lambda e: e.tensor_tensor(v3(A1, 32), v3(msk, 32), v3(m8e, 8)[:, :, 0:1].broadcast_to([128, 16, 32]), ALU.is_equal))
        k.op("dve", ["msk", "m8e"], ["A2"], lambda e: e.tensor_tensor(v3(A2, 32), v3(msk, 32), v3(m8e, 8)[:, :, 1:2].broadcast_to([128, 16, 32]), ALU.is_equal))
        k.op("dve", ["m8e"], ["dl"], lambda e: e.tensor_tensor(dl[:], v3(m8e, 8)[:, :, 0], v3(m8e, 8)[:, :, 1], ALU.subtract))
        k.op("act", ["dl"], ["wt1"], lambda e: e.activation(wt1[:], dl[:], AF.Sigmoid))
        k.op("dve", ["wt1", "gtop"], ["wt1"], lambda e: e.tensor_tensor(wt1[:], wt1[:], gtop[:], ALU.mult))
        k.op("dve", ["wt1", "gtop"], ["wt2"], lambda e: e.tensor_tensor(wt2[:], gtop[:], wt1[:], ALU.subtract))
        k.op("dve", ["A1", "A2"], ["Abf"], lambda e: e.tensor_tensor(Abf[:], A1[:], A2[:], ALU.add))
        Ab3 = v3(Abf, 32)
        for i in range(NT):
            bank = i % 2
            for j in range(i):
                k.op("pe", ["Abf", "s_ones_bf"], [pkey(bank)], lambda e: e.matmul(PB[bank][:, 0:32], ones_bf[:], Ab3[:, j, :], start=(j == 0), stop=False))
            k.op("pe", ["Abf", "s_Ustrict"], [pkey(bank)], lambda e: e.matmul(PB[bank][:, 0:32], Ust[:], Ab3[:, i, :], start=(i == 0), stop=True))
            k.op("act", [pkey(bank)], ["rank"], lambda e: e.copy(v3(rank, 32)[:, i, :], PB[bank][:, 0:32]))
        k.op("dve", ["s_bvals"], ["pstart"], lambda e: e.tensor_copy(pstart[:], bvals[:, 0:64:2]))
        k.op("dve", ["rank", "pstart"], ["rank"], lambda e: e.tensor_tensor(v3(rank, 32), v3(rank, 32), pstart[:].unsqueeze(1).broadcast_to([128, 16, 32]), ALU.add))
        k.op("dve", ["rank", "A1"], ["A1"], lambda e: e.tensor_tensor(A1[:], A1[:], rank[:], ALU.mult))
        k.op("dve", ["rank", "A2"], ["A2"], lambda e: e.tensor_tensor(A2[:], A2[:], rank[:], ALU.mult))
        k.op("dve", ["A1"], ["d0f"], lambda e: e.tensor_reduce(d0f[:], v3(A1, 32), AX.X, ALU.add))
        k.op("dve", ["A2"], ["d1f"], lambda e: e.tensor_reduce(d1f[:], v3(A2, 32), AX.X, ALU.add))
        k.op("dve", ["d0f"], ["d0i"], lambda e: e.tensor_copy(d0i[:], d0f[:]))
        k.op("dve", ["d1f"], ["d1i"], lambda e: e.tensor_copy(d1i[:], d1f[:]))
        if stage == "E2":
            dump(["d0f", "d1f", "wt1", "wt2"], d0f[:], 16)
            early(); return nc, k

        with scope():
            h2s = [sb("h2s%d" % i, [128, D], BF16) for i in range(2)]
            for i in range(NT):
                hk = "h2s%d" % (i % 2)
                k.dma("sp", ["h2d"], [hk], lambda e: e.dma_start(out=h2s[i % 2][:], in_=h2d[i * 128:(i + 1) * 128, :]))
                for di in (d0i, d1i):
                    k.dma("pool", [hk, "d0i", "d1i", "xs"] + ["xs_z%d" % b_ for b_ in range(NBLK)], ["xs"],
                          lambda e: e.indirect_dma_start(out=xs[:, :], out_offset=bass.IndirectOffsetOnAxis(ap=di[:, i:i + 1], axis=0),
                                                         in_=h2s[i % 2][:, :], in_offset=None))

        if stage == "E3":
            k.wait_all("sp", ["xs", "y", "h2d"]); dump(["d0f"], d0f[:], 16); early(); return nc, k

        with scope():
            xb = [sb("xb%d" % i, [128, D], BF16) for i in range(2)]
            xbT = [sb("xbT%d" % i, [128, 16 * 128], BF16) for i in range(2)]
            sg = sb("sg", [128, 512], F32)
            hmid = sb("hmid", [128, 512], BF16)
            hmT = sb("hmT", [128, 512], BF16); hmT3 = hmT[:].rearrange("p (c t) -> p c t", c=4)
            ysb = [sb("ysb%d" % i, [128, D], F32) for i in range(2)]
            def load_x(b):
                k.dma("sp", ["xs"], ["xb%d" % (b % 2)], lambda e: e.dma_start(out=xb[b % 2][:], in_=xs[b * 128:(b + 1) * 128, :]))
            YB = [4, 5, 6, 3]
            k.barrier()
            load_w(0)
            load_x(0)
            for b in range(NBLK):
                ex = b // 2
                if b % 2 == 0 and ex + 1 < NE:
                    load_w(ex + 1)
                if b + 1 < NBLK:
                    load_x(b + 1)
                (kg, wgt), (ku, wut), (kd, wdt) = wsets[ex % 2]
                xbk = "xb%d" % (b % 2)
                xbTk = "xbT%d" % (b % 2)
                xbT3 = xbT[b % 2][:].rearrange("p (c t) -> p c t", c=16)
                for half in range(2):
                    tr_to(xbk, xb[b % 2][:, half * 1024:(half + 1) * 1024], 8, 2 + half, xbTk, xbT3[:, half * 8:(half + 1) * 8, :])
                wg3 = wgt.rearrange("p (c f) -> p c f", c=16)
                wu3 = wut.rearrange("p (c f) -> p c f", c=16)
                wd3 = wdt.rearrange("p (c f) -> p c f", c=4)
                for c in range(16):
                    k.op("pe", [xbTk, kg], [pkey(0)], lambda e: e.matmul(PB[0][:, :], xbT3[:, c, :], wg3[:, c, :], start=(c == 0), stop=(c == 15)))
                for c in range(16):
                    k.op("pe", [xbTk, ku], [pkey(1)], lambda e: e.matmul(PB[1][:, :], xbT3[:, c, :], wu3[:, c, :], start=(c == 0), stop=(c == 15)))
                k.op("act", [pkey(0)], ["sg"], lambda e: e.activation(sg[:], PB[0][:, :], AF.Silu))
                k.op("dve", ["sg", pkey(1)], ["hmid"], lambda e: e.tensor_tensor(hmid[:], sg[:], PB[1][:, :], ALU.mult))
                tr_to("hmid", hmid, 4, 2, "hmT", hmT3)
                ysk = "ysb%d" % (b % 2)
                for nb in range(4):
                    for fc in range(4):
                        k.op("pe", ["hmT", kd], [pkey(YB[nb])],
                             lambda e: e.matmul(PB[YB[nb]][:, :], hmT3[:, fc, :], wd3[:, fc, nb * 512:(nb + 1) * 512], start=(fc == 0), stop=(fc == 3)))
                    if nb % 2 == 0:
                        k.op("act", [pkey(YB[nb])], [ysk], lambda e: e.copy(ysb[b % 2][:, nb * 512:(nb + 1) * 512], PB[YB[nb]][:, :]))
                    else:
                        k.op("dve", [pkey(YB[nb])], [ysk], lambda e: e.tensor_copy(ysb[b % 2][:, nb * 512:(nb + 1) * 512], PB[YB[nb]][:, :]))
                for hf in range(2):
                    k.dma("sp", [ysk], ["ysd%d" % hf], lambda e: e.dma_start(out=ysd[hf][b * 128:(b + 1) * 128, :], in_=ysb[b % 2][:, hf * 1024:(hf + 1) * 1024]))

        with scope():
            HW_ = D // 2
            xa = [sb("xa%d" % i, [128, HW_], F32) for i in range(2)]
            ga = [sb("ga%d" % i, [128, HW_], F32) for i in range(2)]
            gb = [sb("gb%d" % i, [128, HW_], F32) for i in range(2)]

            def e5_load(it):
                i, hf = it // 2, it % 2
                b = it % 2
                ts_ = slice(i * 128, (i + 1) * 128)
                cs2 = slice(hf * HW_, (hf + 1) * HW_)
                k.dma("sp", ["y"], ["xa%d" % b], lambda e: e.dma_start(out=xa[b][:], in_=y[ts_, cs2]))
                k.dma("pool", ["ysd%d" % hf, "d0i"], ["ga%d" % b, "gchainA"],
                      lambda e: e.indirect_dma_start(out=ga[b][:, :], out_offset=None, in_=ysd[hf][:, :],
                                                     in_offset=bass.IndirectOffsetOnAxis(ap=d0i[:, i:i + 1], axis=0)))
                k.dma("pool", ["ysd%d" % hf, "d1i"], ["gb%d" % b, "gchainB"],
                      lambda e: e.indirect_dma_start(out=gb[b][:, :], out_offset=None, in_=ysd[hf][:, :],
                                                     in_offset=bass.IndirectOffsetOnAxis(ap=d1i[:, i:i + 1], axis=0)))
            e5_load(0)
            for it in range(2 * NT):
                i, hf = it // 2, it % 2
                b = it % 2
                ts_ = slice(i * 128, (i + 1) * 128)
                cs2 = slice(hf * HW_, (hf + 1) * HW_)
                if it + 1 < 2 * NT:
                    e5_load(it + 1)
                k.op("dve", ["xa%d" % b, "ga%d" % b, "wt1"], ["xa%d" % b], lambda e: e.scalar_tensor_tensor(xa[b][:], ga[b][:], wt1[:, i:i + 1], xa[b][:], ALU.mult, ALU.add))
                k.op("dve", ["xa%d" % b, "gb%d" % b, "wt2"], ["xa%d" % b], lambda e: e.scalar_tensor_tensor(xa[b][:], gb[b][:], wt2[:, i:i + 1], xa[b][:], ALU.mult, ALU.add))
                k.dma("sp", ["xa%d" % b], ["y"], lambda e: e.dma_start(out=y[ts_, cs2], in_=xa[b][:]))
        k.wait_all("sp", ["y"])
        k.barrier()
        rt.__exit__(None, None, None)
    return nc, k


def _prep_inputs(inp):
    f = np.float32
    w_in = np.asarray(inp["w_in"][0], f)
    def blk(cols):
        w = w_in[:, cols]
        n = w.shape[1]
        return np.ascontiguousarray(w.reshape(16, 128, n).transpose(1, 0, 2)).reshape(128, 16 * n)
    r = np.arange
    wq = np.stack([blk(r(512 * g, 512 * g + 512)) for g in range(2)])
    wkv = np.stack([blk(np.concatenate([r(1024 + 128 * g, 1152 + 128 * g), r(1280 + 128 * g, 1408 + 128 * g),
                                        r(1536 + 128 * g, 1664 + 128 * g), r(1792 + 128 * g, 1920 + 128 * g)])) for g in range(2)])
    ww = np.stack([blk(np.concatenate([r(2048 + 128 * g, 2176 + 128 * g), r(2304 + 128 * g, 2432 + 128 * g),
                                       r(2560 + 12 * g, 2572 + 12 * g)])) for g in range(2)])
    wret = np.stack([blk(np.concatenate([r(2584 + 128 * h, 2712 + 128 * h), r(3608 + 128 * h, 3736 + 128 * h),
                                         r(4632 + 128 * h, 4760 + 128 * h), r(5656 + 128 * h, 5784 + 128 * h)])) for h in range(8)])
    shared = dict(
        zrow=np.zeros((128, D), ml_dtypes.bfloat16),
        g1=np.asarray(inp["norm1_g"], f).reshape(1, D), g2=np.asarray(inp["norm2_g"], f).reshape(1, D),
        wq=wq, wkv=wkv, ww=ww, wret=wret,
        w1=np.ascontiguousarray(np.asarray(inp["cmp_w1"][0], f).transpose(0, 2, 1, 3)).reshape(2, 128, 32 * 128),
        posT=np.ascontiguousarray(np.asarray(inp["cmp_pos"][0], f).transpose(0, 2, 1)),
        w2=np.asarray(inp["cmp_w2"][0], f),
        gq=np.asarray(inp["q_norm_g"], f).reshape(1, 128), gk=np.asarray(inp["k_norm_g"], f).reshape(1, 384),
        gng=np.asarray(inp["ret_gn_g"], f).reshape(1, 1024), gnb=np.asarray(inp["ret_gn_b"], f).reshape(1, 1024),
        wout=np.ascontiguousarray(np.asarray(inp["w_out"][0], f).reshape(16, 128, D).transpose(1, 0, 2)).reshape(128, 16 * D),
        wr=np.ascontiguousarray(np.concatenate([np.asarray(inp["w_router_group"][0], f), np.asarray(inp["w_router_expert"][0], f)], axis=1)
                                .reshape(16, 128, 36).transpose(1, 0, 2)).reshape(128, 16 * 36),
        br=np.concatenate([np.asarray(inp["b_router_group"], f).reshape(-1), np.asarray(inp["b_router_expert"], f).reshape(-1)]).reshape(1, 36),
        w_gate=np.ascontiguousarray(np.asarray(inp["w_exp_gate"][0], f).reshape(NE, 16, 128, DE).transpose(0, 2, 1, 3)).reshape(NE, 128, 16 * DE),
        w_up=np.ascontiguousarray(np.asarray(inp["w_exp_up"][0], f).reshape(NE, 16, 128, DE).transpose(0, 2, 1, 3)).reshape(NE, 128, 16 * DE),
        w_down=np.ascontiguousarray(np.asarray(inp["w_exp_down"][0], f).reshape(NE, 4, 128, D).transpose(0, 2, 1, 3)).reshape(NE, 128, 4 * D),
    )
    return shared


def kernel(**inp):
    consts = _consts()
    shared = _prep_inputs(inp)
    for n in CONST_DT:
        shared["c_" + n] = consts[n]
    nc, _ = build(consts, "full")
    xin = np.asarray(inp["x"], np.float32)
    in_maps = [dict(shared, x=np.ascontiguousarray(xin[b])) for b in range(8)]
    res = run_bass_kernel_spmd(nc, in_maps, core_ids=list(range(8)))
    return np.stack([np.asarray(r["y"], np.float32) for r in res.results], axis=0)
```

```python
import contextlib
import numpy as np
import ml_dtypes
import concourse.bass as bass
import concourse.mybir as mybir
from concourse.bass_utils import run_bass_kernel_spmd

F32 = mybir.dt.float32
BF16 = mybir.dt.bfloat16
I32 = mybir.dt.int32
AF = mybir.ActivationFunctionType
ALU = mybir.AluOpType
AX = mybir.AxisListType

S = 2048
D = 2048
NT = 16
HD = 128
NE = 32
DE = 512
NBLK = 64
NROWS = NBLK * 128
RMS_EPS = 1e-6
GN_EPS = 1e-5
BIG = 30000.0


class K:
    def __init__(self, nc, es):
        self.nc = nc
        self.es = es
        self.E = dict(pe=nc.tensor, act=nc.scalar, dve=nc.vector, pool=nc.gpsimd, sp=nc.sync)
        self.semobj = {}
        self.ccnt = {}
        for n in ("pe", "act", "dve", "pool"):
            self.semobj[n] = es.enter_context(nc.semaphore("c_" + n))
            self.ccnt[n] = 0
        self.dq = {}
        for q, n in (("sp", 20), ("pool", 10), ("act", 4)):
            names = []
            for i in range(n):
                nm = "d_%s%d" % (q, i)
                self.semobj[nm] = es.enter_context(nc.semaphore(nm))
                self.ccnt[nm] = 0
                names.append(nm)
            self.dq[q] = [names, 0]
        self.waited = {}
        self.lastw = {}
        self.readers = {}
        self.n_ins = 0

    def sb(self, name, shape, dt):
        return self.es.enter_context(self.nc.sbuf_tensor(name, list(shape), dt))

    def psum(self, name, shape, dt):
        return self.es.enter_context(self.nc.psum_tensor(name, list(shape), dt))

    def _wait(self, eng, tok):
        s, v = tok
        if eng == "pe" and s == "pe":
            return
        if self.waited.get((eng, s), 0) >= v:
            return
        self.E[eng].wait_ge(self.semobj[s], v)
        self.waited[(eng, s)] = v

    def _deps(self, reads, writes):
        toks = {}
        def add(t):
            if t is None:
                return
            if toks.get(t[0], 0) < t[1]:
                toks[t[0]] = t[1]
        for r in reads:
            add(self.lastw.get(r))
        for w in writes:
            add(self.lastw.get(w))
            for s, v in self.readers.get(w, {}).items():
                add((s, v))
        return list(toks.items())

    def _record(self, reads, writes, tok):
        for r in reads:
            d = self.readers.setdefault(r, {})
            if d.get(tok[0], 0) < tok[1]:
                d[tok[0]] = tok[1]
        for w in writes:
            self.lastw[w] = tok
            self.readers[w] = {}

    def op(self, eng, reads, writes, fn):
        for t in self._deps(reads, writes):
            self._wait(eng, t)
        ins = fn(self.E[eng])
        self.ccnt[eng] += 1
        ins.then_inc(self.semobj[eng], 1)
        tok = (eng, self.ccnt[eng])
        self._record(reads, writes, tok)
        self.n_ins += 1
        return tok

    def dma(self, q, reads, writes, fn):
        names, nxt = self.dq[q]
        nm = names[nxt]
        self.dq[q][1] = (nxt + 1) % len(names)
        if self.ccnt[nm] > 0:
            self._wait(q, (nm, self.ccnt[nm]))
        for t in self._deps(reads, writes):
            self._wait(q, t)
        ins = fn(self.E[q])
        self.ccnt[nm] += 16
        ins.then_inc(self.semobj[nm], 16)
        tok = (nm, self.ccnt[nm])
        self._record(reads, writes, tok)
        self.n_ins += 1
        return tok

    def barrier(self):
        for eng in ("pe", "act", "dve", "pool", "sp"):
            for s, v in self.ccnt.items():
                if v > 0:
                    if eng == "pe" and s == "pe":
                        continue
                    self._wait(eng, (s, v))

    def wait_all(self, eng, keys):
        for t in self._deps(keys, keys):
            self._wait(eng, t)


def _consts():
    c = {}
    bf = ml_dtypes.bfloat16
    c["ident"] = np.eye(128, dtype=np.float32).astype(bf)
    pos = np.arange(S)
    half = HD // 2
    inv_freq = 10000.0 ** (-np.arange(half, dtype=np.float64) / half)
    ang = pos[:, None].astype(np.float64) * inv_freq[None, :]
    def tm(a):
        return np.ascontiguousarray(a.reshape(NT, 128, -1).transpose(1, 0, 2)).astype(np.float32)
    c["cos"] = tm(np.cos(ang.astype(np.float32).astype(np.float64)))
    c["sin"] = tm(np.sin(ang.astype(np.float32).astype(np.float64)))
    c["nsin"] = -c["sin"]
    log_g = np.log1p(-np.exp2(-5.0 - np.arange(8, dtype=np.float64)))
    n = np.arange(128, dtype=np.float64)
    scale = HD ** -0.5
    c["xi"] = np.exp(log_g[None, :] * (n[:, None] + 1.0)).astype(np.float32)
    c["zs"] = (np.exp(log_g[None, :] * (127.0 - n[:, None])) * scale).astype(np.float32)
    diff = n[None, :] - n[:, None]
    dt = np.where(diff[:, None, :] >= 0, np.exp(log_g[None, :, None] * np.maximum(diff[:, None, :], 0.0)), 0.0) * scale
    c["DT"] = np.ascontiguousarray(dt).astype(np.float32)
    c["gC"] = [float(np.float32(np.exp(log_g[h] * 128.0))) for h in range(8)]
    ncmp = 127
    cstart = np.arange(ncmp) * 16
    cm = np.zeros((128, S), np.float32)
    cm[:ncmp] = ((cstart + 31)[:, None] <= pos[None, :])
    c["cmaskT"] = cm.astype(bf)
    sel_start = np.arange(32) * 64
    ovl = ((cstart[:, None] < (sel_start + 64)[None, :]) & ((cstart + 32)[:, None] > sel_start[None, :])).astype(np.float32)
    ri = np.zeros((128, 33), np.float32)
    ri[:ncmp, 0] = 1.0
    ri[:ncmp, 1:] = ovl
    c["rhs_imp"] = ri.astype(bf)
    k = np.arange(128)[:, None]
    cc = np.arange(1536)[None, :] - 512
    c["Mc"] = ((cc - k) >= 0).astype(np.float32).astype(bf)
    c["Mw"] = (((cc - k) >= 0) & ((cc - k) < 512)).astype(np.float32).astype(bf)
    c["Efull"] = (np.arange(S)[None, :] // 64 == np.arange(32)[:, None]).astype(np.float32).astype(bf)
    jb = np.arange(32)[None, :]
    cur = (pos // 64)[:, None]
    valid = sel_start[None, :] <= pos[:, None]
    forced = (jb == 0) | (jb == cur) | (jb == cur - 1)
    c["valid_nf"] = tm((valid & ~forced).astype(np.float32)).astype(bf)
    c["forcedbig"] = tm(np.where(valid, np.where(forced, 1e6, 0.0), -1e30)).astype(bf)
    c["Ustrict"] = (np.arange(128)[:, None] < np.arange(128)[None, :]).astype(np.float32).astype(bf)
    c["ones_bf"] = np.ones((128, 128), np.float32).astype(bf)
    c["bvals"] = np.tile((np.arange(NBLK, dtype=np.float32) * 128.0)[None, :], (128, 1))
    return c


CONST_DT = dict(ident=BF16, cos=F32, sin=F32, nsin=F32, xi=F32, zs=F32, DT=F32, cmaskT=BF16, rhs_imp=BF16,
                Mc=BF16, Mw=BF16, Efull=BF16, valid_nf=BF16, forcedbig=BF16, Ustrict=BF16, ones_bf=BF16, bvals=F32)


def build(consts, stage="full"):
    nc = bass.Bass("TRN2", target_bir_lowering=False)
    es = contextlib.ExitStack()
    dbg = {}
    with es:
        k = K(nc, es)

        k.inputs = []

        def din(name, shape, dt=F32):
            k.inputs.append(name)
            return nc.dram_tensor(name, list(shape), dt, kind="ExternalInput").ap()

        x = din("x", [S, D])
        zrow = din("zrow", [128, D], BF16)
        g1 = din("g1", [1, D])
        g2 = din("g2", [1, D])
        wq = din("wq", [2, 128, 16 * 512])
        wkv = din("wkv", [2, 128, 16 * 512])
        ww = din("ww", [2, 128, 16 * 268])
        wret = din("wret", [8, 128, 16 * 512])
        w1 = din("w1", [2, 128, 32 * 128])
        posT = din("posT", [2, 128, 32])
        w2 = din("w2", [2, 128, 128])
        gq = din("gq", [1, 128])
        gk = din("gk", [1, 3 * 128])
        gng = din("gng", [1, 1024])
        gnb = din("gnb", [1, 1024])
        wout = din("wout", [128, 16 * 2048])
        wr = din("wr", [128, 16 * 36])
        br = din("br", [1, 36])
        if stage in ("full", "E", "E4"):
            wg_d = din("w_gate", [NE, 128, 16 * DE])
            wu_d = din("w_up", [NE, 128, 16 * DE])
            wd_d = din("w_down", [NE, 128, 4 * D])
        cd = {n: din("c_" + n, list(consts[n].shape), CONST_DT[n]) for n in CONST_DT}
        y = nc.dram_tensor("y", [S, D], F32, kind="ExternalOutput").ap()
        h2d = nc.dram_tensor("h2d", [S, D], BF16, kind="Internal").ap()
        xs = nc.dram_tensor("xs", [NROWS, D], BF16, kind="Internal").ap()
        ysd = nc.dram_tensor("ysd", [NROWS, D], F32, kind="Internal").ap()
        if stage != "full":
            dbgo = nc.dram_tensor("dbg", [128, 16 * 2048], F32, kind="ExternalOutput").ap()

        scopes = [es]

        class scope:
            def __enter__(self_):
                st = contextlib.ExitStack()
                st.__enter__()
                scopes.append(st)
                return st
            def __exit__(self_, *a):
                k.barrier()
                st = scopes.pop()
                return st.__exit__(*a)

        uid = [0]

        def sb(name, shape, dt):
            uid[0] += 1
            return scopes[-1].enter_context(nc.sbuf_tensor("%s_u%d" % (name, uid[0]), list(shape), dt))

        def cload(n):
            shp = list(consts[n].shape)
            t = sb("s_" + n, [shp[0], int(np.prod(shp[1:]))], CONST_DT[n])
            src = cd[n]
            if len(shp) == 3:
                src = src.rearrange("p a b -> p (a b)")
            k.dma("sp", [], ["s_" + n], lambda e: e.dma_start(out=t[:], in_=src))
            return t

        def bload(name, src, w):
            t = sb(name, [128, w], F32)
            k.dma("sp", [], [name], lambda e: e.dma_start(out=t[:], in_=src.partition_broadcast(128)))
            return t

        def wload(name, t, src):
            k.dma("pool", [], [name], lambda e: e.dma_start(out=t, in_=src))

        ident = cload("ident")
        PB = [k.psum("pb%d" % i, [128, 512], F32) for i in range(8)]
        PBb = [p[:].bitcast(BF16) for p in PB]

        def pkey(i):
            return "pb%d" % i

        hT = sb("hT", [128, 16 * S], BF16)
        oT = sb("oTn", [128, 8 * S], BF16)
        hT3 = hT[:].rearrange("p (c t) -> p c t", c=16)
        oT3 = oT[:].rearrange("p (c t) -> p c t", c=8)
        small = sb("small", [128, 64], F32)

        def rstd_from(ssk, ss_ap, out_ap, outk, n, eps):
            k.op("act", [ssk], [outk], lambda e: e.activation(out_ap, ss_ap, AF.Sqrt, bias=float(eps), scale=1.0 / n))
            k.op("dve", [outk], [outk], lambda e: e.reciprocal(out_ap, out_ap))

        def early():
            while len(scopes) > 1:
                scopes.pop().__exit__(None, None, None)

        def dump(keys, src_ap, width):
            dt_ = sb("dbgt", [128, width], F32)
            k.op("act", keys, ["dbgt"], lambda e: e.copy(dt_[:], src_ap))
            k.dma("sp", ["dbgt"], ["dbgo"], lambda e: e.dma_start(out=dbgo[:, 0:width], in_=dt_[:]))
            k.wait_all("sp", ["dbgo"])

        with scope():
            g1b = bload("g1b", g1, D)
            sqs = sb("sqs", [128, 2048], BF16)
            xt = [sb("xt%d" % i, [128, D], F32) for i in range(2)]
            hb = [sb("hb%d" % i, [128, D], BF16) for i in range(2)]

            def a_front(i):
                b = i % 2
                k.dma("sp", [], ["xt%d" % b], lambda e: e.dma_start(out=xt[b][:], in_=x[i * 128:(i + 1) * 128, :]))
                k.op("act", ["xt%d" % b], ["sqs", "ss%d" % b], lambda e: e.activation(sqs[:], xt[b][:], AF.Square, accum_out=small[:, b:b + 1]))
                rstd_from("ss%d" % b, small[:, b:b + 1], small[:, 2 + b:3 + b], "rs%d" % b, D, RMS_EPS)
                k.op("dve", ["rs%d" % b, "xt%d" % b, "g1b"], ["hb%d" % b],
                     lambda e: e.scalar_tensor_tensor(hb[b][:], xt[b][:], small[:, 2 + b:3 + b], g1b[:], ALU.mult, ALU.mult))

            def a_back(i):
                b = i % 2
                for half in range(2):
                    pbv = PBb[2 * b + half]
                    for c in range(8):
                        cc = half * 8 + c
                        k.op("pe", ["hb%d" % b, "s_ident"], [pkey(2 * b + half)],
                             lambda e: e.transpose(pbv[:, c * 128:(c + 1) * 128], hb[b][:, cc * 128:(cc + 1) * 128], ident[:]))
                    k.op("act", [pkey(2 * b + half)], ["hT"],
                         lambda e: e.copy(hT3[:, half * 8:(half + 1) * 8, i * 128:(i + 1) * 128],
                                          pbv.rearrange("p (c t) -> p c t", c=8)))
            a_front(0)
            for i in range(NT):
                if i + 1 < NT:
                    a_front(i + 1)
                a_back(i)

        wbufs = {"wbA": sb("wbA", [128, 16 * 512], BF16)}
        wloaded = {}

        def wprefetch(bk, src, ncols, tag):
            wload(bk, wbufs[bk][:, 0:16 * ncols], src)
            wloaded[bk] = tag
        IPB = [0, 1, 5, 6]

        def inproj(bk, tag, src, ncols, post_a, post_b):
            if wloaded.get(bk) != tag:
                wprefetch(bk, src, ncols, tag)
            w3 = wbufs[bk][:, 0:16 * ncols].rearrange("p (c n) -> p c n", c=16)
            pend = None
            for i in range(NT):
                bank = IPB[i % 4]
                for c in range(16):
                    k.op("pe", ["hT", bk], [pkey(bank)],
                         lambda e: e.matmul(PB[bank][:, 0:ncols], hT3[:, c, i * 128:(i + 1) * 128], w3[:, c, :],
                                            start=(c == 0), stop=(c == 15)))
                if pend is not None:
                    post_b(pend)
                post_a(i, bank)
                pend = i
            post_b(pend)

        def tr_to(src_key, src_ap, n128, tbank, dst_key, dst_ap):
            for j in range(n128):
                k.op("pe", [src_key, "s_ident"], [pkey(tbank)],
                     lambda e: e.transpose(PBb[tbank][:, j * 128:(j + 1) * 128], src_ap[:, j * 128:(j + 1) * 128], ident[:]))
            k.op("act", [pkey(tbank)], [dst_key],
                 lambda e: e.copy(dst_ap, PBb[tbank][:, 0:n128 * 128].rearrange("p (c t) -> p c t", c=n128)))

        def pipeline(blocks):
            n = len(blocks)
            if n == 0:
                return
            slot = lambda i: i % 3
            deferred = []
            for j in range(min(2, n)):
                blocks[j][0](slot(j))
            for i in range(n):
                if i + 2 < n:
                    blocks[i + 2][0](slot(i + 2))
                for d in deferred:
                    d()
                deferred = []
                blocks[i][1](slot(i))
                blocks[i][2](slot(i))
                if blocks[i][3] is not None:
                    blocks[i][3]()
                if blocks[i][4] is not None:
                    deferred.append(blocks[i][4])
            for d in deferred:
                d()

        with scope():
            wbufs["wbB"] = sb("wbB", [128, 16 * 512], BF16)
            gqb = bload("gqb", gq, 128)
            gkb = bload("gkb", gk, 384)
            k.op("dve", ["gqb"], ["gqb"], lambda e: e.tensor_scalar(gqb[:], gqb[:], float(HD ** -0.5), None, ALU.mult))
            sqs = sb("sqsB", [128, 512], F32)
            qn = [sb("qn%d" % i, [128, 512], BF16) for i in range(2)]
            accb = [sb("accb%d" % i, [128, 512], BF16) for i in range(2)]
            qT = sb("qT", [128, 4 * S], BF16); qT3 = qT[:].rearrange("p (h t) -> p h t", h=4)
            ksT = sb("ksT", [128, S], BF16)
            kwT = sb("kwT", [128, S], BF16)
            vs1 = sb("vs1", [128, NT * 129], BF16); vs13 = vs1[:].rearrange("p (i c) -> p i c", i=NT)
            vw1 = sb("vw1", [128, NT * 129], BF16); vw13 = vw1[:].rearrange("p (i c) -> p i c", i=NT)
            vc1 = sb("vc1", [128, 129], BF16)
            gates = sb("gates", [128, NT * 12], F32); gates3 = gates[:].rearrange("p (i c) -> p i c", i=NT)
            kcnT = sb("kcnT", [128, 128], BF16)
            k.op("pool", [], ["vs1"], lambda e: e.memset(vs1[:], 1.0))
            k.op("pool", [], ["vw1"], lambda e: e.memset(vw1[:], 1.0))
            k.op("pool", [], ["vc1"], lambda e: e.memset(vc1[:], 1.0))

            def norm_heads(bank, c0, nh, gtab, outt, outk):
                P = PB[bank]
                k.op("act", [pkey(bank)], ["sqsB"], lambda e: e.activation(sqs[:, 0:nh * 128], P[:, c0:c0 + nh * 128], AF.Square))
                k.op("dve", ["sqsB"], ["ssB"], lambda e: e.tensor_reduce(small[:, 8:8 + nh], sqs[:, 0:nh * 128].rearrange("p (h d) -> p h d", h=nh), AX.X, ALU.add))
                rstd_from("ssB", small[:, 8:8 + nh], small[:, 16:16 + nh], "rsB", HD, RMS_EPS)
                for h in range(nh):
                    k.op("dve", ["rsB", pkey(bank)], [outk],
                         lambda e: e.scalar_tensor_tensor(outt[:, h * 128:(h + 1) * 128], P[:, c0 + h * 128:c0 + (h + 1) * 128],
                                                          small[:, 16 + h:17 + h], gtab, ALU.mult, ALU.mult))

            for g in range(2):
              sc1 = scope(); sc1.__enter__()
              if True:
                cvT = sb("cvT", [128, 2 * S], BF16); cvT3 = cvT[:].rearrange("p (h t) -> p h t", h=2)
                w1s = sb("w1s", [128, 2 * 32 * 128], BF16)
                wload("w1s", w1s[:].rearrange("p (i r) -> p i r", i=2), w1.rearrange("i p r -> p i r"))
                w1v = w1s[:].rearrange("p (i l e) -> p i l e", i=2, l=32)
                w2s = sb("w2s", [128, 2 * 128], BF16)
                wload("w2s", w2s[:].rearrange("p (i r) -> p i r", i=2), w2.rearrange("i p r -> p i r"))
                posTs = sb("posTs", [128, 2 * 32], BF16)
                wload("posTs", posTs[:].rearrange("p (i r) -> p i r", i=2), posT.rearrange("i p r -> p i r"))
                hidT = sb("hidT", [128, 2 * 128], BF16)
                kcn = sb("kcn", [128, 128], BF16)
                cbias = sb("cbias", [128, 2], F32)
                k.op("pool", [], ["kcn"], lambda e: e.memset(kcn[:], 0.0))
                k.op("pool", [], ["hidT"], lambda e: e.memset(hidT[:], 0.0))

                def q_a(i, bank):
                    norm_heads(bank, 0, 4, gqb[:], qn[i % 2], "qn%d" % (i % 2))

                def q_b(i):
                    tr_to("qn%d" % (i % 2), qn[i % 2], 4, 2, "qT", qT3[:, :, i * 128:(i + 1) * 128])
                bq, bkv = ("wbA", "wbB") if g == 0 else ("wbB", "wbA")
                if g == 0:
                    wprefetch(bq, wq[g], 512, "q0")
                wprefetch(bkv, wkv[g], 512, "kv%d" % g)
                inproj(bq, "q%d" % g, wq[g], 512, q_a, q_b)
                wprefetch(bq, ww[g], 268, "w%d" % g)

                def kv_a(i, bank):
                    P = PB[bank]
                    k.op("act", [pkey(bank)], ["qn%d" % (i % 2)], lambda e: e.copy(qn[i % 2][:, 0:256], P[:, 0:256]))
                    norm_heads(bank, 256, 1, gkb[:, 128:256], accb[i % 2], "accb%d" % (i % 2))
                    k.op("act", [pkey(bank)], ["vs1"], lambda e: e.copy(vs13[:, i, 0:128], P[:, 384:512]))

                def kv_b(i):
                    tr_to("qn%d" % (i % 2), qn[i % 2], 2, 2, "cvT", cvT3[:, :, i * 128:(i + 1) * 128])
                    tr_to("accb%d" % (i % 2), accb[i % 2], 1, 3, "ksT", ksT[:, i * 128:(i + 1) * 128].rearrange("p (c t) -> p c t", c=1))
                inproj(bkv, "kv%d" % g, wkv[g], 512, kv_a, kv_b)
                if g == 0:
                    wprefetch(bkv, wq[1], 512, "q1")

                def w_a(i, bank):
                    P = PB[bank]
                    norm_heads(bank, 0, 1, gkb[:, 256:384], accb[i % 2], "accb%d" % (i % 2))
                    k.op("act", [pkey(bank)], ["vw1"], lambda e: e.copy(vw13[:, i, 0:128], P[:, 128:256]))
                    k.op("act", [pkey(bank)], ["gates"], lambda e: e.activation(gates3[:, i, :], P[:, 256:268], AF.Sigmoid))

                def w_b(i):
                    tr_to("accb%d" % (i % 2), accb[i % 2], 1, 3, "kwT", kwT[:, i * 128:(i + 1) * 128].rearrange("p (c t) -> p c t", c=1))
                inproj(bq, "w%d" % g, ww[g], 268, w_a, w_b)
                if g == 1:
                    wprefetch("wbA", wret[0], 512, "r0")

                for ci in range(2):
                    src = cvT3[:, ci, :]
                    for l in range(32):
                        k.op("pe", ["w1s", "posTs"], [pkey(4)],
                             lambda e: e.matmul(PB[4][:, 0:1], w1v[:, ci, l, :], posTs[:, ci * 32 + l:ci * 32 + l + 1], start=(l == 0), stop=(l == 31)))
                    k.op("act", [pkey(4)], ["cbias"], lambda e: e.copy(cbias[:, ci:ci + 1], PB[4][:, 0:1]))
                    for l in range(32):
                        k.op("pe", ["w1s", "cvT"], [pkey(5)],
                             lambda e: e.matmul(PB[5][:, 0:127], w1v[:, ci, l, :], src[:, l:l + 16 * 126 + 1:16], start=(l == 0), stop=(l == 31)))
                    k.op("act", [pkey(5), "cbias"], ["hidT"],
                         lambda e: e.activation(hidT[:, ci * 128:ci * 128 + 127], PB[5][:, 0:127], AF.Silu, bias=cbias[:, ci:ci + 1]))
                    k.op("pe", ["hidT", "w2s"], [pkey(4)],
                         lambda e: e.matmul(PB[4][0:127, 0:128], hidT[:, ci * 128:ci * 128 + 127], w2s[:, ci * 128:(ci + 1) * 128], start=True, stop=True))
                    if ci == 0:
                        P = PB[4]
                        k.op("act", [pkey(4)], ["sqsB"], lambda e: e.activation(sqs[0:127, 0:128], P[0:127, 0:128], AF.Square, accum_out=small[0:127, 24:25]))
                        rstd_from("sqsB", small[0:127, 24:25], small[0:127, 25:26], "rsC", HD, RMS_EPS)
                        k.op("dve", ["rsC", pkey(4)], ["kcn"],
                             lambda e: e.scalar_tensor_tensor(kcn[0:127, :], P[0:127, 0:128], small[0:127, 25:26], gkb[0:127, 0:128], ALU.mult, ALU.mult))
                        tr_to("kcn", kcn, 1, 3, "kcnT", kcnT[:].rearrange("p (c t) -> p c t", c=1))
                    else:
                        k.op("act", [pkey(4)], ["vc1"], lambda e: e.copy(vc1[0:127, 0:128], PB[4][0:127, 0:128]))
              sc1.__exit__(None, None, None)
              sc2 = scope(); sc2.__enter__()
              if True:
                cmaskT = cload("cmaskT"); rhs_imp = cload("rhs_imp"); Mc = cload("Mc"); Mw = cload("Mw")
                Efull = cload("Efull"); valid_nf = cload("valid_nf"); forcedbig = cload("forcedbig")
                imp = sb("imp", [128, NT * 32], F32); imp3 = imp[:].rearrange("p (i c) -> p i c", i=NT)
                score = sb("score", [128, NT * 32], F32); score3 = score[:].rearrange("p (i c) -> p i c", i=NT)
                selneg = sb("selneg", [128, NT * 32], BF16); selneg3 = selneg[:].rearrange("p (i c) -> p i c", i=NT)
                selnegT = sb("selnegT", [32, S], BF16)
                m8 = sb("m8", [128, NT * 8], F32); m83 = m8[:].rearrange("p (i c) -> p i c", i=NT)
                Et = [sb("Et%d" % i, [128, 512], BF16) for i in range(3)]
                acc = sb("acc", [128, 512], F32)
                accq = [sb("accq%d" % i, [128, 512], BF16) for i in range(2)]
                Osb = [sb("Osb%d" % i, [128, 4 * 129], F32) for i in range(2)]
                rrp = [sb("rrp%d" % i, [128, 4], F32) for i in range(2)]
                rr1 = [sb("rr1_%d" % i, [128, 4], F32) for i in range(2)]

                def S_cmp(hh, qb):
                    def f(sl):
                        k.op("pe", ["kcnT", "qT"], [pkey(sl)],
                             lambda e: e.matmul(PB[sl][:, :], kcnT[:], qT3[:, hh, qb * 512:(qb + 1) * 512], start=True, stop=True))
                    return f

                def post_cmp(qb):
                    def f(sl):
                        k.op("act", [pkey(sl)], ["Et%d" % sl], lambda e: e.activation(Et[sl][:], PB[sl][:, :], AF.Exp))
                        k.op("dve", ["Et%d" % sl, "s_cmaskT"], ["Et%d" % sl],
                             lambda e: e.tensor_tensor(Et[sl][:], Et[sl][:], cmaskT[:, qb * 512:(qb + 1) * 512], ALU.mult))
                    return f

                blocks = []
                cnt_ = [0]
                for hh in range(4):
                    for qb in range(4):
                        def mk(hh=hh, qb=qb, idx=cnt_[0]):
                            rb = 5 + idx % 2
                            R3 = PB[rb][:, 0:4 * 64].rearrange("p (s c) -> p s c", s=4)[:, :, 0:33]
                            r1 = rr1[idx % 2]

                            def pvf(sl):
                                for st in range(4):
                                    k.op("pe", ["Et%d" % sl, "s_rhs_imp"], [pkey(rb)],
                                         lambda e: e.matmul(R3[:, st, :], Et[sl][:, st * 128:(st + 1) * 128], rhs_imp[:], start=True, stop=True))

                            def tail():
                                kr = "rr1_%d" % (idx % 2)
                                k.op("dve", [pkey(rb)], [kr], lambda e: e.tensor_scalar(r1[:, 0:4], R3[:, :, 0], 1e-30, None, ALU.max))
                                k.op("dve", [kr], [kr], lambda e: e.reciprocal(r1[:, 0:4], r1[:, 0:4]))
                                for st in range(4):
                                    ti = qb * 4 + st
                                    if hh == 0:
                                        k.op("dve", [kr, pkey(rb)], ["imp"],
                                             lambda e: e.tensor_scalar(imp3[:, ti, :], R3[:, st, 1:33], r1[:, st:st + 1], None, ALU.mult))
                                    else:
                                        k.op("dve", [kr, pkey(rb), "imp"], ["imp"],
                                             lambda e: e.scalar_tensor_tensor(imp3[:, ti, :], R3[:, st, 1:33], r1[:, st:st + 1], imp3[:, ti, :], ALU.mult, ALU.add))
                            return (S_cmp(hh, qb), post_cmp(qb), pvf, tail, None)
                        blocks.append(mk())
                        cnt_[0] += 1
                pipeline(blocks)

                k.op("dve", ["imp", "s_valid_nf"], ["score"], lambda e: e.tensor_tensor(score[:], imp[:], valid_nf[:], ALU.mult))
                k.op("dve", ["score", "s_forcedbig"], ["score"], lambda e: e.tensor_tensor(score[:], score[:], forcedbig[:], ALU.add))
                for ti in range(NT):
                    k.op("dve", ["score"], ["m8"], lambda e: e.max(m83[:, ti, :], score3[:, ti, :]))
                k.op("dve", ["score", "m8"], ["score"],
                     lambda e: e.tensor_tensor(score3, score3, m83[:, :, 7:8].broadcast_to([128, NT, 32]), ALU.is_ge))
                k.op("dve", ["score"], ["selneg"], lambda e: e.tensor_scalar(selneg[:], score[:], BIG, -BIG, ALU.mult, ALU.add))
                for half in range(2):
                    for j in range(8):
                        ti = half * 8 + j
                        k.op("pe", ["selneg", "s_ident"], [pkey(3)],
                             lambda e: e.transpose(PBb[3][0:32, j * 128:(j + 1) * 128], selneg3[:, ti, :], ident[:]))
                    k.op("act", [pkey(3)], ["selnegT"], lambda e: e.copy(selnegT[:, half * 1024:(half + 1) * 1024], PBb[3][0:32, :]))

                fbn = [0]

                def finish_branch(qb, col, first):
                    j = fbn[0] % 2
                    fbn[0] += 1
                    ok_ = "Osb%d" % j
                    O_ = Osb[j][:].rearrange("p (s c) -> p s c", s=4)
                    for st in range(4):
                        k.op("dve", [pkey(4 + st)], [ok_], lambda e: e.tensor_copy(O_[:, st, :], PB[4 + st][:, 0:129]))
                    rk = "rrp%d" % j
                    k.op("dve", [ok_], [rk], lambda e: e.tensor_scalar(rrp[j][:, 0:4], O_[:, :, 128], 1e-30, None, ALU.max))
                    k.op("dve", [rk], [rk], lambda e: e.reciprocal(rrp[j][:, 0:4], rrp[j][:, 0:4]))
                    k.op("pool", [rk, "gates"], [rk], lambda e: e.tensor_tensor(rrp[j][:, 0:4], gates3[:, qb * 4:qb * 4 + 4, col], rrp[j][:, 0:4], ALU.mult))
                    rb_ = rrp[j][:, 0:4].unsqueeze(2).broadcast_to([128, 4, 128])
                    acc3 = acc[:].rearrange("p (s c) -> p s c", s=4)
                    if first:
                        k.op("pool", [rk, ok_], ["acc"], lambda e: e.tensor_tensor(acc3, O_[:, :, 0:128], rb_, ALU.mult))
                    else:
                        k.op("pool", [rk, ok_], [ok_], lambda e: e.tensor_tensor(O_[:, :, 0:128], O_[:, :, 0:128], rb_, ALU.mult))
                        k.op("pool", [ok_, "acc"], ["acc"], lambda e: e.tensor_tensor(acc3, acc3, O_[:, :, 0:128], ALU.add))

                def pv(sl, vkey, vap, first_of, last_of, sts):
                    for st in sts:
                        k.op("pe", ["Et%d" % sl, vkey], [pkey(4 + st)],
                             lambda e: e.matmul(PB[4 + st][:, 0:129], Et[sl][:, st * 128:(st + 1) * 128], vap, start=first_of(st), stop=last_of(st)))

                blocks = []
                it_ = [0]
                for hh in range(4):
                    for qb in range(4):
                        def mk_c(hh=hh, qb=qb):
                            def pvf(sl):
                                pv(sl, "vc1", vc1[:], lambda st: True, lambda st: True, range(4))
                            return (S_cmp(hh, qb), post_cmp(qb), pvf, lambda: finish_branch(qb, hh * 3 + 0, True), None)
                        blocks.append(mk_c())
                        nkb = 4 * qb + 4
                        for kb in range(nkb):
                            def mk_s(hh=hh, qb=qb, kb=kb, nkb=nkb):
                                def sf(sl):
                                    k.op("pe", ["ksT", "qT"], [pkey(sl)],
                                         lambda e: e.matmul(PB[sl][:, :], ksT[:, kb * 128:(kb + 1) * 128], qT3[:, hh, qb * 512:(qb + 1) * 512], start=True, stop=False))
                                    k.op("pe", ["s_Efull", "selnegT"], [pkey(sl)],
                                         lambda e: e.matmul(PB[sl][:, :], Efull[0:32, kb * 128:(kb + 1) * 128], selnegT[:, qb * 512:(qb + 1) * 512], start=False, stop=True))

                                def pf(sl):
                                    k.op("act", [pkey(sl)], ["Et%d" % sl], lambda e: e.activation(Et[sl][:], PB[sl][:, :], AF.Exp))
                                    o = kb * 128 - qb * 512
                                    if o >= 0:
                                        k.op("dve", ["Et%d" % sl, "s_Mc"], ["Et%d" % sl],
                                             lambda e: e.tensor_tensor(Et[sl][:], Et[sl][:], Mc[:, 512 - o:1024 - o], ALU.mult))

                                def pvf(sl):
                                    sts = [st for st in range(4) if kb * 128 <= qb * 512 + st * 128 + 127]
                                    pv(sl, "vs1", vs13[:, kb, :], lambda st: kb == 0, lambda st: kb == 4 * qb + st, sts)
                                tail = (lambda: finish_branch(qb, hh * 3 + 1, False)) if kb == nkb - 1 else None
                                return (sf, pf, pvf, tail, None)
                            blocks.append(mk_s())
                        kb0 = max(0, 4 * qb - 4)
                        kbl = 4 * qb + 3
                        for kb in range(kb0, kbl + 1):
                            def mk_w(hh=hh, qb=qb, kb=kb, kbl=kbl, itn=it_[0]):
                                def sf(sl):
                                    k.op("pe", ["kwT", "qT"], [pkey(sl)],
                                         lambda e: e.matmul(PB[sl][:, :], kwT[:, kb * 128:(kb + 1) * 128], qT3[:, hh, qb * 512:(qb + 1) * 512], start=True, stop=True))

                                def pf(sl):
                                    k.op("act", [pkey(sl)], ["Et%d" % sl], lambda e: e.activation(Et[sl][:], PB[sl][:, :], AF.Exp))
                                    o = kb * 128 - qb * 512
                                    k.op("dve", ["Et%d" % sl, "s_Mw"], ["Et%d" % sl],
                                         lambda e: e.tensor_tensor(Et[sl][:], Et[sl][:], Mw[:, 512 - o:1024 - o], ALU.mult))

                                def pvf(sl):
                                    sts = [st for st in range(4) if 4 * qb + st - 4 <= kb <= 4 * qb + st]
                                    pv(sl, "vw1", vw13[:, kb, :], lambda st: kb == max(0, 4 * qb + st - 4), lambda st: kb == 4 * qb + st, sts)
                                tail = None
                                tail_pe = None
                                if kb == kbl:
                                    aq = accq[itn % 2]
                                    kq = "accq%d" % (itn % 2)

                                    def tail():
                                        finish_branch(qb, hh * 3 + 2, False)
                                        k.op("act", ["acc"], [kq], lambda e: e.copy(aq[:], acc[:]))

                                    def tail_pe():
                                        tr_to(kq, aq, 4, 3, "oT", oT3[:, 4 * g + hh, qb * 512:(qb + 1) * 512].rearrange("p (c t) -> p c t", c=4))
                                return (sf, pf, pvf, tail, tail_pe)
                            blocks.append(mk_w())
                        it_[0] += 1
                pipeline(blocks)
              sc2.__exit__(None, None, None)
        if stage == "C":
            dump(["oT"], oT[:, 0:2 * S], 2 * S)
            return nc, k

        oTr = sb("oTr", [128, 8 * S], BF16)
        oTr3 = oTr[:].rearrange("p (c t) -> p c t", c=8)
        with scope():
            cos = cload("cos"); sin = cload("sin"); xi = cload("xi"); zs = cload("zs")
            cos3 = cos[:].rearrange("p (i c) -> p i c", i=NT)
            sin3 = sin[:].rearrange("p (i c) -> p i c", i=NT)
            DTh = sb("DTh", [128, 128], F32)
            gngh = sb("gngh", [128, 128], F32)
            gnbh = sb("gnbh", [128, 128], F32)
            rot32 = sb("rot32", [128, 256], F32)
            ta = sb("ta", [128, 128], F32)
            tb = sb("tb", [128, 128], F32)
            rotb = [sb("rotb%d" % i, [128, 256], BF16) for i in range(2)]
            qkT = sb("qkT", [128, 2 * S], BF16); qkT3 = qkT[:].rearrange("p (a t) -> p a t", a=2)
            kz = sb("kz", [128, NT * 128], BF16); kz3 = kz[:].rearrange("p (i c) -> p i c", i=NT)
            rvb = sb("rvb", [128, NT * 128], BF16); rvb3 = rvb[:].rearrange("p (i c) -> p i c", i=NT)
            rgs = sb("rgs", [128, NT * 128], BF16); rgs3 = rgs[:].rearrange("p (i c) -> p i c", i=NT)
            At16 = sb("At16", [128, NT * 128], BF16); At3 = At16[:].rearrange("p (i c) -> p i c", i=NT)
            o32a = sb("o32a", [128, NT * 128], F32); o3 = o32a[:].rearrange("p (i c) -> p i c", i=NT)
            Rball = sb("Rball", [128, NT * 128], BF16); Rb3 = Rball[:].rearrange("p (i c) -> p i c", i=NT)
            ob16a = sb("ob16a", [128, NT * 128], BF16)
            R32 = [sb("R32_%d" % i, [128, 128], F32) for i in range(2)]
            stt_ = sb("stt", [128, 96], F32)
            for h in range(8):
                k.dma("sp", [], ["DTh"], lambda e: e.dma_start(out=DTh[:], in_=cd["DT"][:, h, :]))
                k.dma("sp", [], ["gngh"], lambda e: e.dma_start(out=gngh[:], in_=gng[:, h * 128:(h + 1) * 128].partition_broadcast(128)))
                k.dma("sp", [], ["gnbh"], lambda e: e.dma_start(out=gnbh[:], in_=gnb[:, h * 128:(h + 1) * 128].partition_broadcast(128)))

                def r_a(i, bank):
                    P = PB[bank]
                    P4 = P[:, 0:256].rearrange("p (a b c) -> p a b c", a=2, b=2)
                    t1 = P4[:, :, 0, :]
                    t2 = P4[:, :, 1, :]
                    r4 = rot32[:].rearrange("p (a b c) -> p a b c", a=2, b=2)
                    ta3 = ta[:].rearrange("p (a c) -> p a c", a=2)
                    tb3 = tb[:].rearrange("p (a c) -> p a c", a=2)
                    cb = cos3[:, i, :].unsqueeze(1).broadcast_to([128, 2, 64])
                    sbb = sin3[:, i, :].unsqueeze(1).broadcast_to([128, 2, 64])
                    k.op("dve", [pkey(bank), "s_cos"], ["ta"], lambda e: e.tensor_tensor(ta3, t1, cb, ALU.mult))
                    k.op("dve", [pkey(bank), "s_sin"], ["tb"], lambda e: e.tensor_tensor(tb3, t2, sbb, ALU.mult))
                    k.op("dve", ["ta", "tb"], ["rot32"], lambda e: e.tensor_tensor(r4[:, :, 0, :], ta3, tb3, ALU.subtract))
                    k.op("dve", [pkey(bank), "s_sin"], ["ta"], lambda e: e.tensor_tensor(ta3, t1, sbb, ALU.mult))
                    k.op("dve", [pkey(bank), "s_cos"], ["tb"], lambda e: e.tensor_tensor(tb3, t2, cb, ALU.mult))
                    k.op("dve", ["ta", "tb"], ["rot32"], lambda e: e.tensor_tensor(r4[:, :, 1, :], ta3, tb3, ALU.add))
                    k.op("act", ["rot32"], ["rotb%d" % (i % 2)], lambda e: e.copy(rotb[i % 2][:], rot32[:]))
                    k.op("pool", ["rot32", "s_zs"], ["kz"], lambda e: e.tensor_scalar(kz3[:, i, :], rot32[:, 128:256], zs[:, h:h + 1], None, ALU.mult))
                    k.op("act", [pkey(bank)], ["rvb"], lambda e: e.copy(rvb3[:, i, :], P[:, 256:384]))
                    k.op("act", [pkey(bank)], ["rgs"], lambda e: e.activation(rgs3[:, i, :], P[:, 384:512], AF.Silu))

                def r_b(i):
                    tr_to("rotb%d" % (i % 2), rotb[i % 2], 2, 2, "qkT", qkT3[:, :, i * 128:(i + 1) * 128])
                inproj("wbA", "r%d" % h, wret[h], 512, r_a, r_b)
                if h == 0:
                    for b in range(NBLK):
                        k.dma("sp", [], ["xs_z%d" % b], lambda e: e.dma_start(out=xs[b * 128:(b + 1) * 128, :], in_=zrow[:, :]))
                if h + 1 < 8:
                    wprefetch("wbA", wret[h + 1], 512, "r%d" % (h + 1))
                else:
                    for c in range(16):
                        wload("hT", hT3[:, c, :], wout[:, c * D:(c + 1) * D])

                cs_ = lambda c: slice(c * 128, (c + 1) * 128)
                AB = [3, 4]
                OB = [5, 6]
                DTb = DTh[:].unsqueeze(1).broadcast_to([128, 4, 128])

                def emitA(gq):
                    ba = AB[gq % 2]
                    for j in range(4):
                        c = 4 * gq + j
                        k.op("pe", ["qkT"], [pkey(ba)], lambda e: e.matmul(PB[ba][:, j * 128:(j + 1) * 128], qkT3[:, 1, cs_(c)], qkT3[:, 0, cs_(c)], start=True, stop=True))
                    k.op("dve", [pkey(ba), "DTh"], ["At16"],
                         lambda e: e.tensor_tensor(At3[:, 4 * gq:4 * gq + 4, :], PB[ba][:, :].rearrange("p (j n) -> p j n", j=4), DTb, ALU.mult))
                emitA(0)
                for gq in range(4):
                    if gq + 1 < 4:
                        emitA(gq + 1)
                    bo = OB[gq % 2]
                    for j in range(4):
                        c = 4 * gq + j
                        k.op("pe", ["At16", "rvb"], [pkey(bo)], lambda e: e.matmul(PB[bo][:, j * 128:(j + 1) * 128], At3[:, c, :], rvb3[:, c, :], start=True, stop=True))
                    k.op("act", [pkey(bo)], ["o32a"], lambda e: e.copy(o32a[:, gq * 512:(gq + 1) * 512], PB[bo][:, :]))
                for gq in range(4):
                    bd = AB[gq % 2]
                    cl = [c for c in range(4 * gq, 4 * gq + 4) if c < NT - 1]
                    for c in cl:
                        j = c - 4 * gq
                        k.op("pe", ["kz", "rvb"], [pkey(bd)], lambda e: e.matmul(PB[bd][:, j * 128:(j + 1) * 128], kz3[:, c, :], rvb3[:, c, :], start=True, stop=True))
                    for c in cl:
                        j = c - 4 * gq
                        if c == 0:
                            k.op("dve", [pkey(bd)], ["R32_0"], lambda e: e.tensor_copy(R32[0][:], PB[bd][:, 0:128]))
                        else:
                            k.op("dve", [pkey(bd), "R32_%d" % ((c - 1) % 2)], ["R32_%d" % (c % 2)],
                                 lambda e: e.scalar_tensor_tensor(R32[c % 2][:], R32[(c - 1) % 2][:], float(consts["gC"][h]), PB[bd][:, j * 128:(j + 1) * 128], ALU.mult, ALU.add))
                        k.op("act", ["R32_%d" % (c % 2)], ["Rball"], lambda e: e.copy(Rb3[:, c + 1, :], R32[c % 2][:]))
                for gq in range(4):
                    bo = OB[gq % 2]
                    lo = 1 if gq == 0 else 0
                    for j in range(lo, 4):
                        c = 4 * gq + j
                        k.op("pe", ["qkT", "Rball"], [pkey(bo)], lambda e: e.matmul(PB[bo][:, j * 128:(j + 1) * 128], qkT3[:, 0, cs_(c)], Rb3[:, c, :], start=True, stop=True))
                    k.op("dve", [pkey(bo), "o32a", "s_xi"], ["o32a"],
                         lambda e: e.scalar_tensor_tensor(o32a[:, gq * 512 + lo * 128:(gq + 1) * 512], PB[bo][:, lo * 128:512], xi[:, h:h + 1],
                                                          o32a[:, gq * 512 + lo * 128:(gq + 1) * 512], ALU.mult, ALU.add))
                k.op("dve", ["o32a"], ["gsum"], lambda e: e.tensor_reduce(stt_[:, 0:16], o3, AX.X, ALU.add))
                for c in range(NT):
                    k.op("act", ["o32a"], ["ta", "gsq"], lambda e: e.activation(ta[:], o3[:, c, :], AF.Square, accum_out=stt_[:, 16 + c:17 + c]))
                k.op("dve", ["gsum"], ["gsum"], lambda e: e.tensor_scalar(stt_[:, 0:16], stt_[:, 0:16], 1.0 / 128, None, ALU.mult))
                k.op("dve", ["gsum"], ["gmsq"], lambda e: e.tensor_tensor(stt_[:, 32:48], stt_[:, 0:16], stt_[:, 0:16], ALU.mult))
                k.op("dve", ["gsq", "gmsq"], ["gvar"], lambda e: e.scalar_tensor_tensor(stt_[:, 48:64], stt_[:, 16:32], 1.0 / 128, stt_[:, 32:48], ALU.mult, ALU.subtract))
                k.op("act", ["gvar"], ["grs"], lambda e: e.activation(stt_[:, 64:80], stt_[:, 48:64], AF.Sqrt, bias=float(GN_EPS), scale=1.0))
                k.op("dve", ["grs"], ["grs"], lambda e: e.reciprocal(stt_[:, 64:80], stt_[:, 64:80]))
                k.op("dve", ["o32a", "gsum"], ["o32a"], lambda e: e.tensor_tensor(o3, o3, stt_[:, 0:16].unsqueeze(2).broadcast_to([128, NT, 128]), ALU.subtract))
                k.op("dve", ["o32a", "grs"], ["o32a"], lambda e: e.tensor_tensor(o3, o3, stt_[:, 64:80].unsqueeze(2).broadcast_to([128, NT, 128]), ALU.mult))
                k.op("dve", ["o32a", "gngh"], ["o32a"], lambda e: e.tensor_tensor(o3, o3, gngh[:].unsqueeze(1).broadcast_to([128, NT, 128]), ALU.mult))
                k.op("dve", ["o32a", "gnbh"], ["o32a"], lambda e: e.tensor_tensor(o3, o3, gnbh[:].unsqueeze(1).broadcast_to([128, NT, 128]), ALU.add))
                k.op("dve", ["o32a", "rgs"], ["ob16a"], lambda e: e.tensor_tensor(ob16a[:], o32a[:], rgs[:], ALU.mult))
                for half in range(2):
                    tr_to("ob16a", ob16a[:, half * 1024:(half + 1) * 1024], 8, 2 + half, "oTr",
                          oTr3[:, h, half * 1024:(half + 1) * 1024].rearrange("p (c t) -> p c t", c=8))
        if stage == "D":
            dump(["oTr"], oTr[:, 0:2 * S], 2 * S)
            return nc, k

        wo3 = hT3
        lgt = sb("lgt", [128, NT * 36], F32)
        lg3 = lgt[:].rearrange("p (i c) -> p i c", i=NT)
        with scope():
            g2b = bload("g2b", g2, D)
            brb = bload("brb", br, 36)
            wrs = sb("wrs", [128, 16 * 36], BF16)
            wload("wrs", wrs[:], wr)
            wrs3 = wrs[:].rearrange("p (c n) -> p c n", c=16)
            xt2 = [sb("xt2_%d" % i, [128, D], F32) for i in range(2)]
            x1t = [sb("x1t_%d" % i, [128, D], F32) for i in range(2)]
            sq2 = sb("sq2", [128, D], BF16)
            h2t = [sb("h2t_%d" % i, [128, D], BF16) for i in range(2)]
            h2T = sb("h2T", [128, 16 * 128], BF16)
            h2T3 = h2T[:].rearrange("p (c t) -> p c t", c=16)

            def e1_front(i):
                b = i % 2
                ts_ = slice(i * 128, (i + 1) * 128)
                k.dma("sp", [], ["xt2_%d" % b], lambda e: e.dma_start(out=xt2[b][:], in_=x[ts_, :]))
                for nb in range(4):
                    for c in range(16):
                        src = oT3[:, c, ts_] if c < 8 else oTr3[:, c - 8, ts_]
                        k.op("pe", ["oT", "oTr", "hT"], [pkey(nb)],
                             lambda e: e.matmul(PB[nb][:, :], src, wo3[:, c, nb * 512:(nb + 1) * 512], start=(c == 0), stop=(c == 15)))
                    k.op("dve", [pkey(nb), "xt2_%d" % b], ["x1t_%d" % b],
                         lambda e: e.tensor_tensor(x1t[b][:, nb * 512:(nb + 1) * 512], PB[nb][:, :], xt2[b][:, nb * 512:(nb + 1) * 512], ALU.add))
                k.dma("sp", ["x1t_%d" % b], ["y"], lambda e: e.dma_start(out=y[ts_, :], in_=x1t[b][:]))
                k.op("act", ["x1t_%d" % b], ["sq2", "ss2_%d" % b], lambda e: e.activation(sq2[:], x1t[b][:], AF.Square, accum_out=small[:, 32 + b:33 + b]))
                rstd_from("ss2_%d" % b, small[:, 32 + b:33 + b], small[:, 34 + b:35 + b], "rs2_%d" % b, D, RMS_EPS)
                k.op("dve", ["rs2_%d" % b, "x1t_%d" % b, "g2b"], ["h2t_%d" % b],
                     lambda e: e.scalar_tensor_tensor(h2t[b][:], x1t[b][:], small[:, 34 + b:35 + b], g2b[:], ALU.mult, ALU.mult))
                k.dma("sp", ["h2t_%d" % b], ["h2d"], lambda e: e.dma_start(out=h2d[ts_, :], in_=h2t[b][:]))

            def e1_back(i):
                b = i % 2
                for half in range(2):
                    tr_to("h2t_%d" % b, h2t[b][:, half * 1024:(half + 1) * 1024], 8, 4 + half, "h2T", h2T3[:, half * 8:(half + 1) * 8, :])
                for c in range(16):
                    k.op("pe", ["h2T", "wrs"], [pkey(6)],
                         lambda e: e.matmul(PB[6][:, 0:36], h2T3[:, c, :], wrs3[:, c, :], start=(c == 0), stop=(c == 15)))
                k.op("dve", [pkey(6), "brb"], ["lgt"], lambda e: e.tensor_tensor(lg3[:, i, :], PB[6][:, 0:36], brb[:], ALU.add))
            e1_front(0)
            for i in range(NT):
                if i + 1 < NT:
                    e1_front(i + 1)
                e1_back(i)
        if stage == "E1":
            dump(["lgt"], lgt[:], NT * 36)
            k.wait_all("sp", ["y", "h2d"])
            return nc, k

        wsets = [
            (("wgA", hT[:, 0:8192]), ("wuA", hT[:, 8192:16384]), ("wdA", hT[:, 16384:24576])),
            (("wgB", hT[:, 24576:32768]), ("wuB", oT[:, 0:8192]), ("wdB", oT[:, 8192:16384])),
        ]

        def load_w(ex, extra=()):
            (kg, wgt), (ku, wut), (kd, wdt) = wsets[ex % 2]
            k.dma("pool", [], [kg] + list(extra), lambda e: e.dma_start(out=wgt, in_=wg_d[ex]))
            k.dma("pool", [], [ku] + list(extra), lambda e: e.dma_start(out=wut, in_=wu_d[ex]))
            k.dma("pool", [], [kd] + list(extra), lambda e: e.dma_start(out=wdt, in_=wd_d[ex]))

        rt = scope(); rt.__enter__()
        ones_bf = cload("ones_bf"); Ust = cload("Ustrict"); bvals = cload("bvals")

        def Rr(name, w, dt=F32):
            return sb(name, [128, w], dt)
        gmax = Rr("gmax", 16); gsh = Rr("gsh", 64); gsum = Rr("gsum", 16); gtop = Rr("gtop", 16)
        ohg = Rr("ohg", 64); msk = Rr("msk", 512); m8e = Rr("m8e", 128)
        A1 = Rr("A1", 512); A2 = Rr("A2", 512); Abf = Rr("Abf", 512, BF16); dl = Rr("dl", 16); wt1 = Rr("wt1", 16); wt2 = Rr("wt2", 16)
        rank = Rr("rank", 512); pstart = Rr("pstart", 32)
        d0f = Rr("d0f", 16); d1f = Rr("d1f", 16); d0i = Rr("d0i", 16, I32); d1i = Rr("d1i", 16, I32)
        v3 = lambda t, c: t[:].rearrange("p (i c) -> p i c", c=c)
        gl = lg3[:, :, 0:4]
        le4 = lg3[:, :, 4:36].rearrange("p i (g e) -> p i g e", g=4)
        k.op("dve", ["lgt"], ["gmax"], lambda e: e.tensor_reduce(gmax[:], gl, AX.X, ALU.max))
        k.op("dve", ["lgt", "gmax"], ["gsh"], lambda e: e.tensor_tensor(v3(gsh, 4), gl, gmax[:].unsqueeze(2).broadcast_to([128, 16, 4]), ALU.subtract))
        k.op("dve", ["gsh"], ["ohg"], lambda e: e.tensor_scalar(ohg[:], gsh[:], 0.0, None, ALU.is_ge))
        k.op("act", ["gsh"], ["gsh"], lambda e: e.activation(gsh[:], gsh[:], AF.Exp))
        k.op("dve", ["gsh"], ["gsum"], lambda e: e.tensor_reduce(gsum[:], v3(gsh, 4), AX.X, ALU.add))
        k.op("dve", ["gsum"], ["gtop"], lambda e: e.reciprocal(gtop[:], gsum[:]))
        k.op("dve", ["ohg"], ["ohg"], lambda e: e.tensor_scalar(ohg[:], ohg[:], 1e30, -1e30, ALU.mult, ALU.add))
        k.op("dve", ["lgt", "ohg"], ["msk"],
             lambda e: e.tensor_tensor(msk[:].rearrange("p (i g e) -> p i g e", i=16, g=4), le4,
                                       v3(ohg, 4).unsqueeze(3).broadcast_to([128, 16, 4, 8]), ALU.add))
        for i in range(NT):
            k.op("dve", ["msk"], ["m8e"], lambda e: e.max(v3(m8e, 8)[:, i, :], v3(msk, 32)[:, i, :]))
        k.op("dve", ["msk", "m8e"], ["A1"], lambda e: e.tensor_tensor(v3(A1, 32), v3(msk, 32), v3(m8e, 8)[:, :, 0:1].broadcast_to([128, 16, 32]), ALU.is_equal))
        k.op("dve", ["msk", "m8e"], ["A2"], lambda e: e.tensor_tensor(v3(A2, 32), v3(msk, 32), v3(m8e, 8)[:, :, 1:2].broadcast_to([128, 16, 32]), ALU.is_equal))
        k.op("dve", ["m8e"], ["dl"], lambda e: e.tensor_tensor(dl[:], v3(m8e, 8)[:, :, 0], v3(m8e, 8)[:, :, 1], ALU.subtract))
        k.op("act", ["dl"], ["wt1"], lambda e: e.activation(wt1[:], dl[:], AF.Sigmoid))
        k.op("dve", ["wt1", "gtop"], ["wt1"], lambda e: e.tensor_tensor(wt1[:], wt1[:], gtop[:], ALU.mult))
        k.op("dve", ["wt1", "gtop"], ["wt2"], lambda e: e.tensor_tensor(wt2[:], gtop[:], wt1[:], ALU.subtract))
        k.op("dve", ["A1", "A2"], ["Abf"], lambda e: e.tensor_tensor(Abf[:], A1[:], A2[:], ALU.add))
        Ab3 = v3(Abf, 32)
        for i in range(NT):
            bank = i % 2
            for j in range(i):
                k.op("pe", ["Abf", "s_ones_bf"], [pkey(bank)], lambda e: e.matmul(PB[bank][:, 0:32], ones_bf[:], Ab3[:, j, :], start=(j == 0), stop=False))
            k.op("pe", ["Abf", "s_Ustrict"], [pkey(bank)], lambda e: e.matmul(PB[bank][:, 0:32], Ust[:], Ab3[:, i, :], start=(i == 0), stop=True))
            k.op("act", [pkey(bank)], ["rank"], lambda e: e.copy(v3(rank, 32)[:, i, :], PB[bank][:, 0:32]))
        k.op("dve", ["s_bvals"], ["pstart"], lambda e: e.tensor_copy(pstart[:], bvals[:, 0:64:2]))
        k.op("dve", ["rank", "pstart"], ["rank"], lambda e: e.tensor_tensor(v3(rank, 32), v3(rank, 32), pstart[:].unsqueeze(1).broadcast_to([128, 16, 32]), ALU.add))
        k.op("dve", ["rank", "A1"], ["A1"], lambda e: e.tensor_tensor(A1[:], A1[:], rank[:], ALU.mult))
        k.op("dve", ["rank", "A2"], ["A2"], lambda e: e.tensor_tensor(A2[:], A2[:], rank[:], ALU.mult))
        k.op("dve", ["A1"], ["d0f"], lambda e: e.tensor_reduce(d0f[:], v3(A1, 32), AX.X, ALU.add))
        k.op("dve", ["A2"], ["d1f"], lambda e: e.tensor_reduce(d1f[:], v3(A2, 32), AX.X, ALU.add))
        k.op("dve", ["d0f"], ["d0i"], lambda e: e.tensor_copy(d0i[:], d0f[:]))
        k.op("dve", ["d1f"], ["d1i"], lambda e: e.tensor_copy(d1i[:], d1f[:]))
        if stage == "E2":
            dump(["d0f", "d1f", "wt1", "wt2"], d0f[:], 16)
            early(); return nc, k

        with scope():
            h2s = [sb("h2s%d" % i, [128, D], BF16) for i in range(2)]
            for i in range(NT):
                hk = "h2s%d" % (i % 2)
                k.dma("sp", ["h2d"], [hk], lambda e: e.dma_start(out=h2s[i % 2][:], in_=h2d[i * 128:(i + 1) * 128, :]))
                for di in (d0i, d1i):
                    k.dma("pool", [hk, "d0i", "d1i", "xs"] + ["xs_z%d" % b_ for b_ in range(NBLK)], ["xs"],
                          lambda e: e.indirect_dma_start(out=xs[:, :], out_offset=bass.IndirectOffsetOnAxis(ap=di[:, i:i + 1], axis=0),
                                                         in_=h2s[i % 2][:, :], in_offset=None))

        if stage == "E3":
            k.wait_all("sp", ["xs", "y", "h2d"]); dump(["d0f"], d0f[:], 16); early(); return nc, k

        with scope():
            xb = [sb("xb%d" % i, [128, D], BF16) for i in range(2)]
            xbT = [sb("xbT%d" % i, [128, 16 * 128], BF16) for i in range(2)]
            sg = sb("sg", [128, 512], F32)
            hmid = sb("hmid", [128, 512], BF16)
            hmT = sb("hmT", [128, 512], BF16); hmT3 = hmT[:].rearrange("p (c t) -> p c t", c=4)
            ysb = [sb("ysb%d" % i, [128, D], F32) for i in range(2)]
            def load_x(b):
                k.dma("sp", ["xs"], ["xb%d" % (b % 2)], lambda e: e.dma_start(out=xb[b % 2][:], in_=xs[b * 128:(b + 1) * 128, :]))
            YB = [4, 5, 6, 3]
            k.barrier()
            load_w(0)
            load_x(0)
            for b in range(NBLK):
                ex = b // 2
                if b % 2 == 0 and ex + 1 < NE:
                    load_w(ex + 1)
                if b + 1 < NBLK:
                    load_x(b + 1)
                (kg, wgt), (ku, wut), (kd, wdt) = wsets[ex % 2]
                xbk = "xb%d" % (b % 2)
                xbTk = "xbT%d" % (b % 2)
                xbT3 = xbT[b % 2][:].rearrange("p (c t) -> p c t", c=16)
                for half in range(2):
                    tr_to(xbk, xb[b % 2][:, half * 1024:(half + 1) * 1024], 8, 2 + half, xbTk, xbT3[:, half * 8:(half + 1) * 8, :])
                wg3 = wgt.rearrange("p (c f) -> p c f", c=16)
                wu3 = wut.rearrange("p (c f) -> p c f", c=16)
                wd3 = wdt.rearrange("p (c f) -> p c f", c=4)
                for c in range(16):
                    k.op("pe", [xbTk, kg], [pkey(0)], lambda e: e.matmul(PB[0][:, :], xbT3[:, c, :], wg3[:, c, :], start=(c == 0), stop=(c == 15)))
                for c in range(16):
                    k.op("pe", [xbTk, ku], [pkey(1)], lambda e: e.matmul(PB[1][:, :], xbT3[:, c, :], wu3[:, c, :], start=(c == 0), stop=(c == 15)))
                k.op("act", [pkey(0)], ["sg"], lambda e: e.activation(sg[:], PB[0][:, :], AF.Silu))
                k.op("dve", ["sg", pkey(1)], ["hmid"], lambda e: e.tensor_tensor(hmid[:], sg[:], PB[1][:, :], ALU.mult))
                tr_to("hmid", hmid, 4, 2, "hmT", hmT3)
                ysk = "ysb%d" % (b % 2)
                for nb in range(4):
                    for fc in range(4):
                        k.op("pe", ["hmT", kd], [pkey(YB[nb])],
                             lambda e: e.matmul(PB[YB[nb]][:, :], hmT3[:, fc, :], wd3[:, fc, nb * 512:(nb + 1) * 512], start=(fc == 0), stop=(fc == 3)))
                    if nb % 2 == 0:
                        k.op("act", [pkey(YB[nb])], [ysk], lambda e: e.copy(ysb[b % 2][:, nb * 512:(nb + 1) * 512], PB[YB[nb]][:, :]))
                    else:
                        k.op("dve", [pkey(YB[nb])], [ysk], lambda e: e.tensor_copy(ysb[b % 2][:, nb * 512:(nb + 1) * 512], PB[YB[nb]][:, :]))
                k.dma("sp", [ysk], ["ysd"], lambda e: e.dma_start(out=ysd[b * 128:(b + 1) * 128, :], in_=ysb[b % 2][:]))

        with scope():
            xa = sb("xa", [128, D], F32)
            ga = sb("ga", [128, D], F32)
            gb = sb("gb", [128, D], F32)
            for i in range(NT):
                ts_ = slice(i * 128, (i + 1) * 128)
                k.dma("sp", ["y"], ["xa"], lambda e: e.dma_start(out=xa[:], in_=y[ts_, :]))
                k.dma("pool", ["ysd", "d0i"], ["ga"],
                      lambda e: e.indirect_dma_start(out=ga[:, :], out_offset=None, in_=ysd[:, :],
                                                     in_offset=bass.IndirectOffsetOnAxis(ap=d0i[:, i:i + 1], axis=0)))
                k.dma("pool", ["ysd", "d1i"], ["gb"],
                      lambda e: e.indirect_dma_start(out=gb[:, :], out_offset=None, in_=ysd[:, :],
                                                     in_offset=bass.IndirectOffsetOnAxis(ap=d1i[:, i:i + 1], axis=0)))
                k.op("dve", ["xa", "ga", "wt1"], ["xa"], lambda e: e.scalar_tensor_tensor(xa[:], ga[:], wt1[:, i:i + 1], xa[:], ALU.mult, ALU.add))
                k.op("dve", ["xa", "gb", "wt2"], ["xa"], lambda e: e.scalar_tensor_tensor(xa[:], gb[:], wt2[:, i:i + 1], xa[:], ALU.mult, ALU.add))
                k.dma("sp", ["xa"], ["y"], lambda e: e.dma_start(out=y[ts_, :], in_=xa[:]))
        k.wait_all("sp", ["y"])
        k.barrier()
        rt.__exit__(None, None, None)
    return nc, k


def _prep_inputs(inp):
    f = np.float32
    w_in = np.asarray(inp["w_in"][0], f)
    def blk(cols):
        w = w_in[:, cols]
        n = w.shape[1]
        return np.ascontiguousarray(w.reshape(16, 128, n).transpose(1, 0, 2)).reshape(128, 16 * n)
    r = np.arange
    wq = np.stack([blk(r(512 * g, 512 * g + 512)) for g in range(2)])
    wkv = np.stack([blk(np.concatenate([r(1024 + 128 * g, 1152 + 128 * g), r(1280 + 128 * g, 1408 + 128 * g),
                                        r(1536 + 128 * g, 1664 + 128 * g), r(1792 + 128 * g, 1920 + 128 * g)])) for g in range(2)])
    ww = np.stack([blk(np.concatenate([r(2048 + 128 * g, 2176 + 128 * g), r(2304 + 128 * g, 2432 + 128 * g),
                                       r(2560 + 12 * g, 2572 + 12 * g)])) for g in range(2)])
    wret = np.stack([blk(np.concatenate([r(2584 + 128 * h, 2712 + 128 * h), r(3608 + 128 * h, 3736 + 128 * h),
                                         r(4632 + 128 * h, 4760 + 128 * h), r(5656 + 128 * h, 5784 + 128 * h)])) for h in range(8)])
    shared = dict(
        zrow=np.zeros((128, D), ml_dtypes.bfloat16),
        g1=np.asarray(inp["norm1_g"], f).reshape(1, D), g2=np.asarray(inp["norm2_g"], f).reshape(1, D),
        wq=wq, wkv=wkv, ww=ww, wret=wret,
        w1=np.ascontiguousarray(np.asarray(inp["cmp_w1"][0], f).transpose(0, 2, 1, 3)).reshape(2, 128, 32 * 128),
        posT=np.ascontiguousarray(np.asarray(inp["cmp_pos"][0], f).transpose(0, 2, 1)),
        w2=np.asarray(inp["cmp_w2"][0], f),
        gq=np.asarray(inp["q_norm_g"], f).reshape(1, 128), gk=np.asarray(inp["k_norm_g"], f).reshape(1, 384),
        gng=np.asarray(inp["ret_gn_g"], f).reshape(1, 1024), gnb=np.asarray(inp["ret_gn_b"], f).reshape(1, 1024),
        wout=np.ascontiguousarray(np.asarray(inp["w_out"][0], f).reshape(16, 128, D).transpose(1, 0, 2)).reshape(128, 16 * D),
        wr=np.ascontiguousarray(np.concatenate([np.asarray(inp["w_router_group"][0], f), np.asarray(inp["w_router_expert"][0], f)], axis=1)
                                .reshape(16, 128, 36).transpose(1, 0, 2)).reshape(128, 16 * 36),
        br=np.concatenate([np.asarray(inp["b_router_group"], f).reshape(-1), np.asarray(inp["b_router_expert"], f).reshape(-1)]).reshape(1, 36),
        w_gate=np.ascontiguousarray(np.asarray(inp["w_exp_gate"][0], f).reshape(NE, 16, 128, DE).transpose(0, 2, 1, 3)).reshape(NE, 128, 16 * DE),
        w_up=np.ascontiguousarray(np.asarray(inp["w_exp_up"][0], f).reshape(NE, 16, 128, DE).transpose(0, 2, 1, 3)).reshape(NE, 128, 16 * DE),
        w_down=np.ascontiguousarray(np.asarray(inp["w_exp_down"][0], f).reshape(NE, 4, 128, D).transpose(0, 2, 1, 3)).reshape(NE, 128, 4 * D),
    )
    return shared


def kernel(**inp):
    consts = _consts()
    shared = _prep_inputs(inp)
    for n in CONST_DT:
        shared["c_" + n] = consts[n]
    nc, _ = build(consts, "full")
    xin = np.asarray(inp["x"], np.float32)
    in_maps = [dict(shared, x=np.ascontiguousarray(xin[b])) for b in range(8)]
    res = run_bass_kernel_spmd(nc, in_maps, core_ids=list(range(8)))
    return np.stack([np.asarray(r["y"], np.float32) for r in res.results], axis=0)
```

```python
import contextlib
import numpy as np
import ml_dtypes
import concourse.bass as bass
import concourse.mybir as mybir
from concourse.bass_utils import run_bass_kernel_spmd

F32 = mybir.dt.float32
BF16 = mybir.dt.bfloat16
I32 = mybir.dt.int32
AF = mybir.ActivationFunctionType
ALU = mybir.AluOpType
AX = mybir.AxisListType

S = 2048
D = 2048
NT = 16
HD = 128
NE = 32
DE = 512
NBLK = 64
NROWS = NBLK * 128
RMS_EPS = 1e-6
GN_EPS = 1e-5
BIG = 30000.0


class K:
    def __init__(self, nc, es):
        self.nc = nc
        self.es = es
        self.E = dict(pe=nc.tensor, act=nc.scalar, dve=nc.vector, pool=nc.gpsimd, sp=nc.sync)
        self.semobj = {}
        self.ccnt = {}
        for n in ("pe", "act", "dve", "pool"):
            self.semobj[n] = es.enter_context(nc.semaphore("c_" + n))
            self.ccnt[n] = 0
        self.dq = {}
        for q, n in (("sp", 20), ("pool", 10), ("act", 4)):
            names = []
            for i in range(n):
                nm = "d_%s%d" % (q, i)
                self.semobj[nm] = es.enter_context(nc.semaphore(nm))
                self.ccnt[nm] = 0
                names.append(nm)
            self.dq[q] = [names, 0]
        self.waited = {}
        self.lastw = {}
        self.readers = {}
        self.n_ins = 0

    def sb(self, name, shape, dt):
        return self.es.enter_context(self.nc.sbuf_tensor(name, list(shape), dt))

    def psum(self, name, shape, dt):
        return self.es.enter_context(self.nc.psum_tensor(name, list(shape), dt))

    def _wait(self, eng, tok):
        s, v = tok
        if eng == "pe" and s == "pe":
            return
        if self.waited.get((eng, s), 0) >= v:
            return
        self.E[eng].wait_ge(self.semobj[s], v)
        self.waited[(eng, s)] = v

    def _deps(self, reads, writes):
        toks = {}
        def add(t):
            if t is None:
                return
            if toks.get(t[0], 0) < t[1]:
                toks[t[0]] = t[1]
        for r in reads:
            add(self.lastw.get(r))
        for w in writes:
            add(self.lastw.get(w))
            for s, v in self.readers.get(w, {}).items():
                add((s, v))
        return list(toks.items())

    def _record(self, reads, writes, tok):
        for r in reads:
            d = self.readers.setdefault(r, {})
            if d.get(tok[0], 0) < tok[1]:
                d[tok[0]] = tok[1]
        for w in writes:
            self.lastw[w] = tok
            self.readers[w] = {}

    def op(self, eng, reads, writes, fn):
        for t in self._deps(reads, writes):
            self._wait(eng, t)
        ins = fn(self.E[eng])
        self.ccnt[eng] += 1
        ins.then_inc(self.semobj[eng], 1)
        tok = (eng, self.ccnt[eng])
        self._record(reads, writes, tok)
        self.n_ins += 1
        return tok

    def dma(self, q, reads, writes, fn):
        names, nxt = self.dq[q]
        nm = names[nxt]
        self.dq[q][1] = (nxt + 1) % len(names)
        if self.ccnt[nm] > 0:
            self._wait(q, (nm, self.ccnt[nm]))
        for t in self._deps(reads, writes):
            self._wait(q, t)
        ins = fn(self.E[q])
        self.ccnt[nm] += 16
        ins.then_inc(self.semobj[nm], 16)
        tok = (nm, self.ccnt[nm])
        self._record(reads, writes, tok)
        self.n_ins += 1
        return tok

    def barrier(self):
        for eng in ("pe", "act", "dve", "pool", "sp"):
            for s, v in self.ccnt.items():
                if v > 0:
                    if eng == "pe" and s == "pe":
                        continue
                    self._wait(eng, (s, v))

    def wait_all(self, eng, keys):
        for t in self._deps(keys, keys):
            self._wait(eng, t)


def _consts():
    c = {}
    bf = ml_dtypes.bfloat16
    c["ident"] = np.eye(128, dtype=np.float32).astype(bf)
    pos = np.arange(S)
    half = HD // 2
    inv_freq = 10000.0 ** (-np.arange(half, dtype=np.float64) / half)
    ang = pos[:, None].astype(np.float64) * inv_freq[None, :]
    def tm(a):
        return np.ascontiguousarray(a.reshape(NT, 128, -1).transpose(1, 0, 2)).astype(np.float32)
    c["cos"] = tm(np.cos(ang.astype(np.float32).astype(np.float64)))
    c["sin"] = tm(np.sin(ang.astype(np.float32).astype(np.float64)))
    c["nsin"] = -c["sin"]
    log_g = np.log1p(-np.exp2(-5.0 - np.arange(8, dtype=np.float64)))
    n = np.arange(128, dtype=np.float64)
    scale = HD ** -0.5
    c["xi"] = np.exp(log_g[None, :] * (n[:, None] + 1.0)).astype(np.float32)
    c["zs"] = (np.exp(log_g[None, :] * (127.0 - n[:, None])) * scale).astype(np.float32)
    diff = n[None, :] - n[:, None]
    dt = np.where(diff[:, None, :] >= 0, np.exp(log_g[None, :, None] * np.maximum(diff[:, None, :], 0.0)), 0.0) * scale
    c["DT"] = np.ascontiguousarray(dt).astype(np.float32)
    c["gC"] = [float(np.float32(np.exp(log_g[h] * 128.0))) for h in range(8)]
    ncmp = 127
    cstart = np.arange(ncmp) * 16
    cm = np.zeros((128, S), np.float32)
    cm[:ncmp] = ((cstart + 31)[:, None] <= pos[None, :])
    c["cmaskT"] = cm.astype(bf)
    sel_start = np.arange(32) * 64
    ovl = ((cstart[:, None] < (sel_start + 64)[None, :]) & ((cstart + 32)[:, None] > sel_start[None, :])).astype(np.float32)
    ri = np.zeros((128, 33), np.float32)
    ri[:ncmp, 0] = 1.0
    ri[:ncmp, 1:] = ovl
    c["rhs_imp"] = ri.astype(bf)
    k = np.arange(128)[:, None]
    cc = np.arange(1536)[None, :] - 512
    c["Mc"] = ((cc - k) >= 0).astype(np.float32).astype(bf)
    c["Mw"] = (((cc - k) >= 0) & ((cc - k) < 512)).astype(np.float32).astype(bf)
    c["Efull"] = (np.arange(S)[None, :] // 64 == np.arange(32)[:, None]).astype(np.float32).astype(bf)
    jb = np.arange(32)[None, :]
    cur = (pos // 64)[:, None]
    valid = sel_start[None, :] <= pos[:, None]
    forced = (jb == 0) | (jb == cur) | (jb == cur - 1)
    c["valid_nf"] = tm((valid & ~forced).astype(np.float32)).astype(bf)
    c["forcedbig"] = tm(np.where(valid, np.where(forced, 1e6, 0.0), -1e30)).astype(bf)
    c["Ustrict"] = (np.arange(128)[:, None] < np.arange(128)[None, :]).astype(np.float32).astype(bf)
    c["ones_bf"] = np.ones((128, 128), np.float32).astype(bf)
    c["bvals"] = np.tile((np.arange(NBLK, dtype=np.float32) * 128.0)[None, :], (128, 1))
    return c


CONST_DT = dict(ident=BF16, cos=F32, sin=F32, nsin=F32, xi=F32, zs=F32, DT=F32, cmaskT=BF16, rhs_imp=BF16,
                Mc=BF16, Mw=BF16, Efull=BF16, valid_nf=BF16, forcedbig=BF16, Ustrict=BF16, ones_bf=BF16, bvals=F32)


def build(consts, stage="full"):
    nc = bass.Bass("TRN2", target_bir_lowering=False)
    es = contextlib.ExitStack()
    dbg = {}
    with es:
        k = K(nc, es)

        k.inputs = []

        def din(name, shape, dt=F32):
            k.inputs.append(name)
            return nc.dram_tensor(name, list(shape), dt, kind="ExternalInput").ap()

        x = din("x", [S, D])
        zrow = din("zrow", [128, D], BF16)
        g1 = din("g1", [1, D])
        g2 = din("g2", [1, D])
        wq = din("wq", [2, 128, 16 * 512])
        wkv = din("wkv", [2, 128, 16 * 512])
        ww = din("ww", [2, 128, 16 * 268])
        wret = din("wret", [8, 128, 16 * 512])
        w1 = din("w1", [2, 128, 32 * 128])
        posT = din("posT", [2, 128, 32])
        w2 = din("w2", [2, 128, 128])
        gq = din("gq", [1, 128])
        gk = din("gk", [1, 3 * 128])
        gng = din("gng", [1, 1024])
        gnb = din("gnb", [1, 1024])
        wout = din("wout", [128, 16 * 2048])
        wr = din("wr", [128, 16 * 36])
        br = din("br", [1, 36])
        if stage in ("full", "E", "E4"):
            wg_d = din("w_gate", [NE, 128, 16 * DE])
            wu_d = din("w_up", [NE, 128, 16 * DE])
            wd_d = din("w_down", [NE, 128, 4 * D])
        cd = {n: din("c_" + n, list(consts[n].shape), CONST_DT[n]) for n in CONST_DT}
        y = nc.dram_tensor("y", [S, D], F32, kind="ExternalOutput").ap()
        h2d = nc.dram_tensor("h2d", [S, D], BF16, kind="Internal").ap()
        xs = nc.dram_tensor("xs", [NROWS, D], BF16, kind="Internal").ap()
        ysd = nc.dram_tensor("ysd", [NROWS, D], F32, kind="Internal").ap()
        if stage != "full":
            dbgo = nc.dram_tensor("dbg", [128, 16 * 2048], F32, kind="ExternalOutput").ap()

        scopes = [es]

        class scope:
            def __enter__(self_):
                st = contextlib.ExitStack()
                st.__enter__()
                scopes.append(st)
                return st
            def __exit__(self_, *a):
                k.barrier()
                st = scopes.pop()
                return st.__exit__(*a)

        uid = [0]

        def sb(name, shape, dt):
            uid[0] += 1
            return scopes[-1].enter_context(nc.sbuf_tensor("%s_u%d" % (name, uid[0]), list(shape), dt))

        def cload(n):
            shp = list(consts[n].shape)
            t = sb("s_" + n, [shp[0], int(np.prod(shp[1:]))], CONST_DT[n])
            src = cd[n]
            if len(shp) == 3:
                src = src.rearrange("p a b -> p (a b)")
            k.dma("sp", [], ["s_" + n], lambda e: e.dma_start(out=t[:], in_=src))
            return t

        def bload(name, src, w):
            t = sb(name, [128, w], F32)
            k.dma("sp", [], [name], lambda e: e.dma_start(out=t[:], in_=src.partition_broadcast(128)))
            return t

        def wload(name, t, src):
            k.dma("pool", [], [name], lambda e: e.dma_start(out=t, in_=src))

        ident = cload("ident")
        PB = [k.psum("pb%d" % i, [128, 512], F32) for i in range(8)]
        PBb = [p[:].bitcast(BF16) for p in PB]

        def pkey(i):
            return "pb%d" % i

        hT = sb("hT", [128, 16 * S], BF16)
        oT = sb("oTn", [128, 8 * S], BF16)
        hT3 = hT[:].rearrange("p (c t) -> p c t", c=16)
        oT3 = oT[:].rearrange("p (c t) -> p c t", c=8)
        small = sb("small", [128, 64], F32)

        def rstd_from(ssk, ss_ap, out_ap, outk, n, eps):
            k.op("act", [ssk], [outk], lambda e: e.activation(out_ap, ss_ap, AF.Sqrt, bias=float(eps), scale=1.0 / n))
            k.op("dve", [outk], [outk], lambda e: e.reciprocal(out_ap, out_ap))

        def early():
            while len(scopes) > 1:
                scopes.pop().__exit__(None, None, None)

        def dump(keys, src_ap, width):
            dt_ = sb("dbgt", [128, width], F32)
            k.op("act", keys, ["dbgt"], lambda e: e.copy(dt_[:], src_ap))
            k.dma("sp", ["dbgt"], ["dbgo"], lambda e: e.dma_start(out=dbgo[:, 0:width], in_=dt_[:]))
            k.wait_all("sp", ["dbgo"])

        with scope():
            g1b = bload("g1b", g1, D)
            sqs = sb("sqs", [128, 2048], BF16)
            xt = [sb("xt%d" % i, [128, D], F32) for i in range(2)]
            hb = [sb("hb%d" % i, [128, D], BF16) for i in range(2)]

            def a_front(i):
                b = i % 2
                k.dma("sp", [], ["xt%d" % b], lambda e: e.dma_start(out=xt[b][:], in_=x[i * 128:(i + 1) * 128, :]))
                k.op("act", ["xt%d" % b], ["sqs", "ss%d" % b], lambda e: e.activation(sqs[:], xt[b][:], AF.Square, accum_out=small[:, b:b + 1]))
                rstd_from("ss%d" % b, small[:, b:b + 1], small[:, 2 + b:3 + b], "rs%d" % b, D, RMS_EPS)
                k.op("dve", ["rs%d" % b, "xt%d" % b, "g1b"], ["hb%d" % b],
                     lambda e: e.scalar_tensor_tensor(hb[b][:], xt[b][:], small[:, 2 + b:3 + b], g1b[:], ALU.mult, ALU.mult))

            def a_back(i):
                b = i % 2
                for half in range(2):
                    pbv = PBb[2 * b + half]
                    for c in range(8):
                        cc = half * 8 + c
                        k.op("pe", ["hb%d" % b, "s_ident"], [pkey(2 * b + half)],
                             lambda e: e.transpose(pbv[:, c * 128:(c + 1) * 128], hb[b][:, cc * 128:(cc + 1) * 128], ident[:]))
                    k.op("act", [pkey(2 * b + half)], ["hT"],
                         lambda e: e.copy(hT3[:, half * 8:(half + 1) * 8, i * 128:(i + 1) * 128],
                                          pbv.rearrange("p (c t) -> p c t", c=8)))
            a_front(0)
            for i in range(NT):
                if i + 1 < NT:
                    a_front(i + 1)
                a_back(i)

        wbufs = {"wbA": sb("wbA", [128, 16 * 512], BF16)}
        wloaded = {}

        def wprefetch(bk, src, ncols, tag):
            wload(bk, wbufs[bk][:, 0:16 * ncols], src)
            wloaded[bk] = tag
        IPB = [0, 1, 5, 6]

        def inproj(bk, tag, src, ncols, post_a, post_b):
            if wloaded.get(bk) != tag:
                wprefetch(bk, src, ncols, tag)
            w3 = wbufs[bk][:, 0:16 * ncols].rearrange("p (c n) -> p c n", c=16)
            pend = None
            for i in range(NT):
                bank = IPB[i % 4]
                for c in range(16):
                    k.op("pe", ["hT", bk], [pkey(bank)],
                         lambda e: e.matmul(PB[bank][:, 0:ncols], hT3[:, c, i * 128:(i + 1) * 128], w3[:, c, :],
                                            start=(c == 0), stop=(c == 15)))
                if pend is not None:
                    post_b(pend)
                post_a(i, bank)
                pend = i
            post_b(pend)

        def tr_to(src_key, src_ap, n128, tbank, dst_key, dst_ap):
            for j in range(n128):
                k.op("pe", [src_key, "s_ident"], [pkey(tbank)],
                     lambda e: e.transpose(PBb[tbank][:, j * 128:(j + 1) * 128], src_ap[:, j * 128:(j + 1) * 128], ident[:]))
            k.op("act", [pkey(tbank)], [dst_key],
                 lambda e: e.copy(dst_ap, PBb[tbank][:, 0:n128 * 128].rearrange("p (c t) -> p c t", c=n128)))

        def pipeline(blocks):
            n = len(blocks)
            if n == 0:
                return
            slot = lambda i: i % 3
            deferred = []
            for j in range(min(2, n)):
                blocks[j][0](slot(j))
            for i in range(n):
                if i + 2 < n:
                    blocks[i + 2][0](slot(i + 2))
                for d in deferred:
                    d()
                deferred = []
                blocks[i][1](slot(i))
                blocks[i][2](slot(i))
                if blocks[i][3] is not None:
                    blocks[i][3]()
                if blocks[i][4] is not None:
                    deferred.append(blocks[i][4])
            for d in deferred:
                d()

        with scope():
            wbufs["wbB"] = sb("wbB", [128, 16 * 512], BF16)
            gqb = bload("gqb", gq, 128)
            gkb = bload("gkb", gk, 384)
            k.op("dve", ["gqb"], ["gqb"], lambda e: e.tensor_scalar(gqb[:], gqb[:], float(HD ** -0.5), None, ALU.mult))
            sqs = sb("sqsB", [128, 512], F32)
            qn = [sb("qn%d" % i, [128, 512], BF16) for i in range(2)]
            accb = [sb("accb%d" % i, [128, 512], BF16) for i in range(2)]
            qT = sb("qT", [128, 4 * S], BF16); qT3 = qT[:].rearrange("p (h t) -> p h t", h=4)
            ksT = sb("ksT", [128, S], BF16)
            kwT = sb("kwT", [128, S], BF16)
            vs1 = sb("vs1", [128, NT * 129], BF16); vs13 = vs1[:].rearrange("p (i c) -> p i c", i=NT)
            vw1 = sb("vw1", [128, NT * 129], BF16); vw13 = vw1[:].rearrange("p (i c) -> p i c", i=NT)
            vc1 = sb("vc1", [128, 129], BF16)
            gates = sb("gates", [128, NT * 12], F32); gates3 = gates[:].rearrange("p (i c) -> p i c", i=NT)
            kcnT = sb("kcnT", [128, 128], BF16)
            k.op("pool", [], ["vs1"], lambda e: e.memset(vs1[:], 1.0))
            k.op("pool", [], ["vw1"], lambda e: e.memset(vw1[:], 1.0))
            k.op("pool", [], ["vc1"], lambda e: e.memset(vc1[:], 1.0))

            def norm_heads(bank, c0, nh, gtab, outt, outk):
                P = PB[bank]
                k.op("act", [pkey(bank)], ["sqsB"], lambda e: e.activation(sqs[:, 0:nh * 128], P[:, c0:c0 + nh * 128], AF.Square))
                k.op("dve", ["sqsB"], ["ssB"], lambda e: e.tensor_reduce(small[:, 8:8 + nh], sqs[:, 0:nh * 128].rearrange("p (h d) -> p h d", h=nh), AX.X, ALU.add))
                rstd_from("ssB", small[:, 8:8 + nh], small[:, 16:16 + nh], "rsB", HD, RMS_EPS)
                for h in range(nh):
                    k.op("dve", ["rsB", pkey(bank)], [outk],
                         lambda e: e.scalar_tensor_tensor(outt[:, h * 128:(h + 1) * 128], P[:, c0 + h * 128:c0 + (h + 1) * 128],
                                                          small[:, 16 + h:17 + h], gtab, ALU.mult, ALU.mult))

            for g in range(2):
              sc1 = scope(); sc1.__enter__()
              if True:
                cvT = sb("cvT", [128, 2 * S], BF16); cvT3 = cvT[:].rearrange("p (h t) -> p h t", h=2)
                w1s = sb("w1s", [128, 2 * 32 * 128], BF16)
                wload("w1s", w1s[:].rearrange("p (i r) -> p i r", i=2), w1.rearrange("i p r -> p i r"))
                w1v = w1s[:].rearrange("p (i l e) -> p i l e", i=2, l=32)
                w2s = sb("w2s", [128, 2 * 128], BF16)
                wload("w2s", w2s[:].rearrange("p (i r) -> p i r", i=2), w2.rearrange("i p r -> p i r"))
                posTs = sb("posTs", [128, 2 * 32], BF16)
                wload("posTs", posTs[:].rearrange("p (i r) -> p i r", i=2), posT.rearrange("i p r -> p i r"))
                hidT = sb("hidT", [128, 2 * 128], BF16)
                kcn = sb("kcn", [128, 128], BF16)
                cbias = sb("cbias", [128, 2], F32)
                k.op("pool", [], ["kcn"], lambda e: e.memset(kcn[:], 0.0))
                k.op("pool", [], ["hidT"], lambda e: e.memset(hidT[:], 0.0))

                def q_a(i, bank):
                    norm_heads(bank, 0, 4, gqb[:], qn[i % 2], "qn%d" % (i % 2))

                def q_b(i):
                    tr_to("qn%d" % (i % 2), qn[i % 2], 4, 2, "qT", qT3[:, :, i * 128:(i + 1) * 128])
                bq, bkv = ("wbA", "wbB") if g == 0 else ("wbB", "wbA")
                if g == 0:
                    wprefetch(bq, wq[g], 512, "q0")
                wprefetch(bkv, wkv[g], 512, "kv%d" % g)
                inproj(bq, "q%d" % g, wq[g], 512, q_a, q_b)
                wprefetch(bq, ww[g], 268, "w%d" % g)

                def kv_a(i, bank):
                    P = PB[bank]
                    k.op("act", [pkey(bank)], ["qn%d" % (i % 2)], lambda e: e.copy(qn[i % 2][:, 0:256], P[:, 0:256]))
                    norm_heads(bank, 256, 1, gkb[:, 128:256], accb[i % 2], "accb%d" % (i % 2))
                    k.op("act", [pkey(bank)], ["vs1"], lambda e: e.copy(vs13[:, i, 0:128], P[:, 384:512]))

                def kv_b(i):
                    tr_to("qn%d" % (i % 2), qn[i % 2], 2, 2, "cvT", cvT3[:, :, i * 128:(i + 1) * 128])
                    tr_to("accb%d" % (i % 2), accb[i % 2], 1, 3, "ksT", ksT[:, i * 128:(i + 1) * 128].rearrange("p (c t) -> p c t", c=1))
                inproj(bkv, "kv%d" % g, wkv[g], 512, kv_a, kv_b)
                if g == 0:
                    wprefetch(bkv, wq[1], 512, "q1")

                def w_a(i, bank):
                    P = PB[bank]
                    norm_heads(bank, 0, 1, gkb[:, 256:384], accb[i % 2], "accb%d" % (i % 2))
                    k.op("act", [pkey(bank)], ["vw1"], lambda e: e.copy(vw13[:, i, 0:128], P[:, 128:256]))
                    k.op("act", [pkey(bank)], ["gates"], lambda e: e.activation(gates3[:, i, :], P[:, 256:268], AF.Sigmoid))

                def w_b(i):
                    tr_to("accb%d" % (i % 2), accb[i % 2], 1, 3, "kwT", kwT[:, i * 128:(i + 1) * 128].rearrange("p (c t) -> p c t", c=1))
                inproj(bq, "w%d" % g, ww[g], 268, w_a, w_b)
                if g == 1:
                    wprefetch("wbA", wret[0], 512, "r0")

                for ci in range(2):
                    src = cvT3[:, ci, :]
                    for l in range(32):
                        k.op("pe", ["w1s", "posTs"], [pkey(4)],
                             lambda e: e.matmul(PB[4][:, 0:1], w1v[:, ci, l, :], posTs[:, ci * 32 + l:ci * 32 + l + 1], start=(l == 0), stop=(l == 31)))
                    k.op("act", [pkey(4)], ["cbias"], lambda e: e.copy(cbias[:, ci:ci + 1], PB[4][:, 0:1]))
                    for l in range(32):
                        k.op("pe", ["w1s", "cvT"], [pkey(5)],
                             lambda e: e.matmul(PB[5][:, 0:127], w1v[:, ci, l, :], src[:, l:l + 16 * 126 + 1:16], start=(l == 0), stop=(l == 31)))
                    k.op("act", [pkey(5), "cbias"], ["hidT"],
                         lambda e: e.activation(hidT[:, ci * 128:ci * 128 + 127], PB[5][:, 0:127], AF.Silu, bias=cbias[:, ci:ci + 1]))
                    k.op("pe", ["hidT", "w2s"], [pkey(4)],
                         lambda e: e.matmul(PB[4][0:127, 0:128], hidT[:, ci * 128:ci * 128 + 127], w2s[:, ci * 128:(ci + 1) * 128], start=True, stop=True))
                    if ci == 0:
                        P = PB[4]
                        k.op("act", [pkey(4)], ["sqsB"], lambda e: e.activation(sqs[0:127, 0:128], P[0:127, 0:128], AF.Square, accum_out=small[0:127, 24:25]))
                        rstd_from("sqsB", small[0:127, 24:25], small[0:127, 25:26], "rsC", HD, RMS_EPS)
                        k.op("dve", ["rsC", pkey(4)], ["kcn"],
                             lambda e: e.scalar_tensor_tensor(kcn[0:127, :], P[0:127, 0:128], small[0:127, 25:26], gkb[0:127, 0:128], ALU.mult, ALU.mult))
                        tr_to("kcn", kcn, 1, 3, "kcnT", kcnT[:].rearrange("p (c t) -> p c t", c=1))
                    else:
                        k.op("act", [pkey(4)], ["vc1"], lambda e: e.copy(vc1[0:127, 0:128], PB[4][0:127, 0:128]))
              sc1.__exit__(None, None, None)
              sc2 = scope(); sc2.__enter__()
              if True:
                cmaskT = cload("cmaskT"); rhs_imp = cload("rhs_imp"); Mc = cload("Mc"); Mw = cload("Mw")
                Efull = cload("Efull"); valid_nf = cload("valid_nf"); forcedbig = cload("forcedbig")
                imp = sb("imp", [128, NT * 32], F32); imp3 = imp[:].rearrange("p (i c) -> p i c", i=NT)
                score = sb("score", [128, NT * 32], F32); score3 = score[:].rearrange("p (i c) -> p i c", i=NT)
                selneg = sb("selneg", [128, NT * 32], BF16); selneg3 = selneg[:].rearrange("p (i c) -> p i c", i=NT)
                selnegT = sb("selnegT", [32, S], BF16)
                m8 = sb("m8", [128, NT * 8], F32); m83 = m8[:].rearrange("p (i c) -> p i c", i=NT)
                Et = [sb("Et%d" % i, [128, 512], BF16) for i in range(3)]
                acc = sb("acc", [128, 512], F32)
                accq = [sb("accq%d" % i, [128, 512], BF16) for i in range(2)]
                Osb = [sb("Osb%d" % i, [128, 4 * 129], F32) for i in range(2)]
                rrp = [sb("rrp%d" % i, [128, 4], F32) for i in range(2)]
                rr1 = [sb("rr1_%d" % i, [128, 4], F32) for i in range(2)]

                def S_cmp(hh, qb):
                    def f(sl):
                        k.op("pe", ["kcnT", "qT"], [pkey(sl)],
                             lambda e: e.matmul(PB[sl][:, :], kcnT[:], qT3[:, hh, qb * 512:(qb + 1) * 512], start=True, stop=True))
                    return f

                def post_cmp(qb):
                    def f(sl):
                        k.op("act", [pkey(sl)], ["Et%d" % sl], lambda e: e.activation(Et[sl][:], PB[sl][:, :], AF.Exp))
                        k.op("dve", ["Et%d" % sl, "s_cmaskT"], ["Et%d" % sl],
                             lambda e: e.tensor_tensor(Et[sl][:], Et[sl][:], cmaskT[:, qb * 512:(qb + 1) * 512], ALU.mult))
                    return f

                blocks = []
                cnt_ = [0]
                for hh in range(4):
                    for qb in range(4):
                        def mk(hh=hh, qb=qb, idx=cnt_[0]):
                            rb = 5 + idx % 2
                            R3 = PB[rb][:, 0:4 * 64].rearrange("p (s c) -> p s c", s=4)[:, :, 0:33]
                            r1 = rr1[idx % 2]

                            def pvf(sl):
                                for st in range(4):
                                    k.op("pe", ["Et%d" % sl, "s_rhs_imp"], [pkey(rb)],
                                         lambda e: e.matmul(R3[:, st, :], Et[sl][:, st * 128:(st + 1) * 128], rhs_imp[:], start=True, stop=True))

                            def tail():
                                kr = "rr1_%d" % (idx % 2)
                                k.op("dve", [pkey(rb)], [kr], lambda e: e.tensor_scalar(r1[:, 0:4], R3[:, :, 0], 1e-30, None, ALU.max))
                                k.op("dve", [kr], [kr], lambda e: e.reciprocal(r1[:, 0:4], r1[:, 0:4]))
                                for st in range(4):
                                    ti = qb * 4 + st
                                    if hh == 0:
                                        k.op("dve", [kr, pkey(rb)], ["imp"],
                                             lambda e: e.tensor_scalar(imp3[:, ti, :], R3[:, st, 1:33], r1[:, st:st + 1], None, ALU.mult))
                                    else:
                                        k.op("dve", [kr, pkey(rb), "imp"], ["imp"],
                                             lambda e: e.scalar_tensor_tensor(imp3[:, ti, :], R3[:, st, 1:33], r1[:, st:st + 1], imp3[:, ti, :], ALU.mult, ALU.add))
                            return (S_cmp(hh, qb), post_cmp(qb), pvf, tail, None)
                        blocks.append(mk())
                        cnt_[0] += 1
                pipeline(blocks)

                k.op("dve", ["imp", "s_valid_nf"], ["score"], lambda e: e.tensor_tensor(score[:], imp[:], valid_nf[:], ALU.mult))
                k.op("dve", ["score", "s_forcedbig"], ["score"], lambda e: e.tensor_tensor(score[:], score[:], forcedbig[:], ALU.add))
                for ti in range(NT):
                    k.op("dve", ["score"], ["m8"], lambda e: e.max(m83[:, ti, :], score3[:, ti, :]))
                k.op("dve", ["score", "m8"], ["score"],
                     lambda e: e.tensor_tensor(score3, score3, m83[:, :, 7:8].broadcast_to([128, NT, 32]), ALU.is_ge))
                k.op("dve", ["score"], ["selneg"], lambda e: e.tensor_scalar(selneg[:], score[:], BIG, -BIG, ALU.mult, ALU.add))
                for half in range(2):
                    for j in range(8):
                        ti = half * 8 + j
                        k.op("pe", ["selneg", "s_ident"], [pkey(3)],
                             lambda e: e.transpose(PBb[3][0:32, j * 128:(j + 1) * 128], selneg3[:, ti, :], ident[:]))
                    k.op("act", [pkey(3)], ["selnegT"], lambda e: e.copy(selnegT[:, half * 1024:(half + 1) * 1024], PBb[3][0:32, :]))

                fbn = [0]

                def finish_branch(qb, col, first):
                    j = fbn[0] % 2
                    fbn[0] += 1
                    ok_ = "Osb%d" % j
                    O_ = Osb[j][:].rearrange("p (s c) -> p s c", s=4)
                    for st in range(4):
                        k.op("dve", [pkey(4 + st)], [ok_], lambda e: e.tensor_copy(O_[:, st, :], PB[4 + st][:, 0:129]))
                    rk = "rrp%d" % j
                    k.op("dve", [ok_], [rk], lambda e: e.tensor_scalar(rrp[j][:, 0:4], O_[:, :, 128], 1e-30, None, ALU.max))
                    k.op("dve", [rk], [rk], lambda e: e.reciprocal(rrp[j][:, 0:4], rrp[j][:, 0:4]))
                    k.op("pool", [rk, "gates"], [rk], lambda e: e.tensor_tensor(rrp[j][:, 0:4], gates3[:, qb * 4:qb * 4 + 4, col], rrp[j][:, 0:4], ALU.mult))
                    rb_ = rrp[j][:, 0:4].unsqueeze(2).broadcast_to([128, 4, 128])
                    acc3 = acc[:].rearrange("p (s c) -> p s c", s=4)
                    if first:
                        k.op("pool", [rk, ok_], ["acc"], lambda e: e.tensor_tensor(acc3, O_[:, :, 0:128], rb_, ALU.mult))
                    else:
                        k.op("pool", [rk, ok_], [ok_], lambda e: e.tensor_tensor(O_[:, :, 0:128], O_[:, :, 0:128], rb_, ALU.mult))
                        k.op("pool", [ok_, "acc"], ["acc"], lambda e: e.tensor_tensor(acc3, acc3, O_[:, :, 0:128], ALU.add))

                def pv(sl, vkey, vap, first_of, last_of, sts):
                    for st in sts:
                        k.op("pe", ["Et%d" % sl, vkey], [pkey(4 + st)],
                             lambda e: e.matmul(PB[4 + st][:, 0:129], Et[sl][:, st * 128:(st + 1) * 128], vap, start=first_of(st), stop=last_of(st)))

                blocks = []
                it_ = [0]
                for hh in range(4):
                    for qb in range(4):
                        def mk_c(hh=hh, qb=qb):
                            def pvf(sl):
                                pv(sl, "vc1", vc1[:], lambda st: True, lambda st: True, range(4))
                            return (S_cmp(hh, qb), post_cmp(qb), pvf, lambda: finish_branch(qb, hh * 3 + 0, True), None)
                        blocks.append(mk_c())
                        nkb = 4 * qb + 4
                        for kb in range(nkb):
                            def mk_s(hh=hh, qb=qb, kb=kb, nkb=nkb):
                                def sf(sl):
                                    k.op("pe", ["ksT", "qT"], [pkey(sl)],
                                         lambda e: e.matmul(PB[sl][:, :], ksT[:, kb * 128:(kb + 1) * 128], qT3[:, hh, qb * 512:(qb + 1) * 512], start=True, stop=False))
                                    k.op("pe", ["s_Efull", "selnegT"], [pkey(sl)],
                                         lambda e: e.matmul(PB[sl][:, :], Efull[0:32, kb * 128:(kb + 1) * 128], selnegT[:, qb * 512:(qb + 1) * 512], start=False, stop=True))

                                def pf(sl):
                                    k.op("act", [pkey(sl)], ["Et%d" % sl], lambda e: e.activation(Et[sl][:], PB[sl][:, :], AF.Exp))
                                    o = kb * 128 - qb * 512
                                    if o >= 0:
                                        k.op("dve", ["Et%d" % sl, "s_Mc"], ["Et%d" % sl],
                                             lambda e: e.tensor_tensor(Et[sl][:], Et[sl][:], Mc[:, 512 - o:1024 - o], ALU.mult))

                                def pvf(sl):
                                    sts = [st for st in range(4) if kb * 128 <= qb * 512 + st * 128 + 127]
                                    pv(sl, "vs1", vs13[:, kb, :], lambda st: kb == 0, lambda st: kb == 4 * qb + st, sts)
                                tail = (lambda: finish_branch(qb, hh * 3 + 1, False)) if kb == nkb - 1 else None
                                return (sf, pf, pvf, tail, None)
                            blocks.append(mk_s())
                        kb0 = max(0, 4 * qb - 4)
                        kbl = 4 * qb + 3
                        for kb in range(kb0, kbl + 1):
                            def mk_w(hh=hh, qb=qb, kb=kb, kbl=kbl, itn=it_[0]):
                                def sf(sl):
                                    k.op("pe", ["kwT", "qT"], [pkey(sl)],
                                         lambda e: e.matmul(PB[sl][:, :], kwT[:, kb * 128:(kb + 1) * 128], qT3[:, hh, qb * 512:(qb + 1) * 512], start=True, stop=True))

                                def pf(sl):
                                    k.op("act", [pkey(sl)], ["Et%d" % sl], lambda e: e.activation(Et[sl][:], PB[sl][:, :], AF.Exp))
                                    o = kb * 128 - qb * 512
                                    k.op("dve", ["Et%d" % sl, "s_Mw"], ["Et%d" % sl],
                                         lambda e: e.tensor_tensor(Et[sl][:], Et[sl][:], Mw[:, 512 - o:1024 - o], ALU.mult))

                                def pvf(sl):
                                    sts = [st for st in range(4) if 4 * qb + st - 4 <= kb <= 4 * qb + st]
                                    pv(sl, "vw1", vw13[:, kb, :], lambda st: kb == max(0, 4 * qb + st - 4), lambda st: kb == 4 * qb + st, sts)
                                tail = None
                                tail_pe = None
                                if kb == kbl:
                                    aq = accq[itn % 2]
                                    kq = "accq%d" % (itn % 2)

                                    def tail():
                                        finish_branch(qb, hh * 3 + 2, False)
                                        k.op("act", ["acc"], [kq], lambda e: e.copy(aq[:], acc[:]))

                                    def tail_pe():
                                        tr_to(kq, aq, 4, 3, "oT", oT3[:, 4 * g + hh, qb * 512:(qb + 1) * 512].rearrange("p (c t) -> p c t", c=4))
                                return (sf, pf, pvf, tail, tail_pe)
                            blocks.append(mk_w())
                        it_[0] += 1
                pipeline(blocks)
              sc2.__exit__(None, None, None)
        if stage == "C":
            dump(["oT"], oT[:, 0:2 * S], 2 * S)
            return nc, k

        oTr = sb("oTr", [128, 8 * S], BF16)
        oTr3 = oTr[:].rearrange("p (c t) -> p c t", c=8)
        with scope():
            cos = cload("cos"); sin = cload("sin"); xi = cload("xi"); zs = cload("zs")
            cos3 = cos[:].rearrange("p (i c) -> p i c", i=NT)
            sin3 = sin[:].rearrange("p (i c) -> p i c", i=NT)
            DTh = sb("DTh", [128, 128], F32)
            gngh = sb("gngh", [128, 128], F32)
            gnbh = sb("gnbh", [128, 128], F32)
            rot32 = sb("rot32", [128, 256], F32)
            ta = sb("ta", [128, 128], F32)
            tb = sb("tb", [128, 128], F32)
            rotb = [sb("rotb%d" % i, [128, 256], BF16) for i in range(2)]
            qkT = sb("qkT", [128, 2 * S], BF16); qkT3 = qkT[:].rearrange("p (a t) -> p a t", a=2)
            kz = sb("kz", [128, NT * 128], BF16); kz3 = kz[:].rearrange("p (i c) -> p i c", i=NT)
            rvb = sb("rvb", [128, NT * 128], BF16); rvb3 = rvb[:].rearrange("p (i c) -> p i c", i=NT)
            rgs = sb("rgs", [128, NT * 128], BF16); rgs3 = rgs[:].rearrange("p (i c) -> p i c", i=NT)
            At16 = sb("At16", [128, NT * 128], BF16); At3 = At16[:].rearrange("p (i c) -> p i c", i=NT)
            o32a = sb("o32a", [128, NT * 128], F32); o3 = o32a[:].rearrange("p (i c) -> p i c", i=NT)
            Rball = sb("Rball", [128, NT * 128], BF16); Rb3 = Rball[:].rearrange("p (i c) -> p i c", i=NT)
            ob16a = sb("ob16a", [128, NT * 128], BF16)
            R32 = [sb("R32_%d" % i, [128, 128], F32) for i in range(2)]
            stt_ = sb("stt", [128, 96], F32)
            for h in range(8):
                k.dma("sp", [], ["DTh"], lambda e: e.dma_start(out=DTh[:], in_=cd["DT"][:, h, :]))
                k.dma("sp", [], ["gngh"], lambda e: e.dma_start(out=gngh[:], in_=gng[:, h * 128:(h + 1) * 128].partition_broadcast(128)))
                k.dma("sp", [], ["gnbh"], lambda e: e.dma_start(out=gnbh[:], in_=gnb[:, h * 128:(h + 1) * 128].partition_broadcast(128)))

                def r_a(i, bank):
                    P = PB[bank]
                    P4 = P[:, 0:256].rearrange("p (a b c) -> p a b c", a=2, b=2)
                    t1 = P4[:, :, 0, :]
                    t2 = P4[:, :, 1, :]
                    r4 = rot32[:].rearrange("p (a b c) -> p a b c", a=2, b=2)
                    ta3 = ta[:].rearrange("p (a c) -> p a c", a=2)
                    tb3 = tb[:].rearrange("p (a c) -> p a c", a=2)
                    cb = cos3[:, i, :].unsqueeze(1).broadcast_to([128, 2, 64])
                    sbb = sin3[:, i, :].unsqueeze(1).broadcast_to([128, 2, 64])
                    k.op("dve", [pkey(bank), "s_cos"], ["ta"], lambda e: e.tensor_tensor(ta3, t1, cb, ALU.mult))
                    k.op("dve", [pkey(bank), "s_sin"], ["tb"], lambda e: e.tensor_tensor(tb3, t2, sbb, ALU.mult))
                    k.op("dve", ["ta", "tb"], ["rot32"], lambda e: e.tensor_tensor(r4[:, :, 0, :], ta3, tb3, ALU.subtract))
                    k.op("dve", [pkey(bank), "s_sin"], ["ta"], lambda e: e.tensor_tensor(ta3, t1, sbb, ALU.mult))
                    k.op("dve", [pkey(bank), "s_cos"], ["tb"], lambda e: e.tensor_tensor(tb3, t2, cb, ALU.mult))
                    k.op("dve", ["ta", "tb"], ["rot32"], lambda e: e.tensor_tensor(r4[:, :, 1, :], ta3, tb3, ALU.add))
                    k.op("act", ["rot32"], ["rotb%d" % (i % 2)], lambda e: e.copy(rotb[i % 2][:], rot32[:]))
                    k.op("pool", ["rot32", "s_zs"], ["kz"], lambda e: e.tensor_scalar(kz3[:, i, :], rot32[:, 128:256], zs[:, h:h + 1], None, ALU.mult))
                    k.op("act", [pkey(bank)], ["rvb"], lambda e: e.copy(rvb3[:, i, :], P[:, 256:384]))
                    k.op("act", [pkey(bank)], ["rgs"], lambda e: e.activation(rgs3[:, i, :], P[:, 384:512], AF.Silu))

                def r_b(i):
                    tr_to("rotb%d" % (i % 2), rotb[i % 2], 2, 2, "qkT", qkT3[:, :, i * 128:(i + 1) * 128])
                inproj("wbA", "r%d" % h, wret[h], 512, r_a, r_b)
                if h == 0:
                    for b in range(NBLK):
                        k.dma("sp", [], ["xs_z%d" % b], lambda e: e.dma_start(out=xs[b * 128:(b + 1) * 128, :], in_=zrow[:, :]))
                if h + 1 < 8:
                    wprefetch("wbA", wret[h + 1], 512, "r%d" % (h + 1))
                else:
                    for c in range(16):
                        wload("hT", hT3[:, c, :], wout[:, c * D:(c + 1) * D])

                cs_ = lambda c: slice(c * 128, (c + 1) * 128)
                AB = [3, 4]
                OB = [5, 6]
                DTb = DTh[:].unsqueeze(1).broadcast_to([128, 4, 128])

                def emitA(gq):
                    ba = AB[gq % 2]
                    for j in range(4):
                        c = 4 * gq + j
                        k.op("pe", ["qkT"], [pkey(ba)], lambda e: e.matmul(PB[ba][:, j * 128:(j + 1) * 128], qkT3[:, 1, cs_(c)], qkT3[:, 0, cs_(c)], start=True, stop=True))
                    k.op("dve", [pkey(ba), "DTh"], ["At16"],
                         lambda e: e.tensor_tensor(At3[:, 4 * gq:4 * gq + 4, :], PB[ba][:, :].rearrange("p (j n) -> p j n", j=4), DTb, ALU.mult))
                emitA(0)
                for gq in range(4):
                    if gq + 1 < 4:
                        emitA(gq + 1)
                    bo = OB[gq % 2]
                    for j in range(4):
                        c = 4 * gq + j
                        k.op("pe", ["At16", "rvb"], [pkey(bo)], lambda e: e.matmul(PB[bo][:, j * 128:(j + 1) * 128], At3[:, c, :], rvb3[:, c, :], start=True, stop=True))
                    k.op("act", [pkey(bo)], ["o32a"], lambda e: e.copy(o32a[:, gq * 512:(gq + 1) * 512], PB[bo][:, :]))
                for gq in range(4):
                    bd = AB[gq % 2]
                    cl = [c for c in range(4 * gq, 4 * gq + 4) if c < NT - 1]
                    for c in cl:
                        j = c - 4 * gq
                        k.op("pe", ["kz", "rvb"], [pkey(bd)], lambda e: e.matmul(PB[bd][:, j * 128:(j + 1) * 128], kz3[:, c, :], rvb3[:, c, :], start=True, stop=True))
                    for c in cl:
                        j = c - 4 * gq
                        if c == 0:
                            k.op("dve", [pkey(bd)], ["R32_0"], lambda e: e.tensor_copy(R32[0][:], PB[bd][:, 0:128]))
                        else:
                            k.op("dve", [pkey(bd), "R32_%d" % ((c - 1) % 2)], ["R32_%d" % (c % 2)],
                                 lambda e: e.scalar_tensor_tensor(R32[c % 2][:], R32[(c - 1) % 2][:], float(consts["gC"][h]), PB[bd][:, j * 128:(j + 1) * 128], ALU.mult, ALU.add))
                        k.op("act", ["R32_%d" % (c % 2)], ["Rball"], lambda e: e.copy(Rb3[:, c + 1, :], R32[c % 2][:]))
                for gq in range(4):
                    bo = OB[gq % 2]
                    lo = 1 if gq == 0 else 0
                    for j in range(lo, 4):
                        c = 4 * gq + j
                        k.op("pe", ["qkT", "Rball"], [pkey(bo)], lambda e: e.matmul(PB[bo][:, j * 128:(j + 1) * 128], qkT3[:, 0, cs_(c)], Rb3[:, c, :], start=True, stop=True))
                    k.op("dve", [pkey(bo), "o32a", "s_xi"], ["o32a"],
                         lambda e: e.scalar_tensor_tensor(o32a[:, gq * 512 + lo * 128:(gq + 1) * 512], PB[bo][:, lo * 128:512], xi[:, h:h + 1],
                                                          o32a[:, gq * 512 + lo * 128:(gq + 1) * 512], ALU.mult, ALU.add))
                k.op("dve", ["o32a"], ["gsum"], lambda e: e.tensor_reduce(stt_[:, 0:16], o3, AX.X, ALU.add))
                for c in range(NT):
                    k.op("act", ["o32a"], ["ta", "gsq"], lambda e: e.activation(ta[:], o3[:, c, :], AF.Square, accum_out=stt_[:, 16 + c:17 + c]))
                k.op("dve", ["gsum"], ["gsum"], lambda e: e.tensor_scalar(stt_[:, 0:16], stt_[:, 0:16], 1.0 / 128, None, ALU.mult))
                k.op("dve", ["gsum"], ["gmsq"], lambda e: e.tensor_tensor(stt_[:, 32:48], stt_[:, 0:16], stt_[:, 0:16], ALU.mult))
                k.op("dve", ["gsq", "gmsq"], ["gvar"], lambda e: e.scalar_tensor_tensor(stt_[:, 48:64], stt_[:, 16:32], 1.0 / 128, stt_[:, 32:48], ALU.mult, ALU.subtract))
                k.op("act", ["gvar"], ["grs"], lambda e: e.activation(stt_[:, 64:80], stt_[:, 48:64], AF.Sqrt, bias=float(GN_EPS), scale=1.0))
                k.op("dve", ["grs"], ["grs"], lambda e: e.reciprocal(stt_[:, 64:80], stt_[:, 64:80]))
                k.op("dve", ["o32a", "gsum"], ["o32a"], lambda e: e.tensor_tensor(o3, o3, stt_[:, 0:16].unsqueeze(2).broadcast_to([128, NT, 128]), ALU.subtract))
                k.op("dve", ["o32a", "grs"], ["o32a"], lambda e: e.tensor_tensor(o3, o3, stt_[:, 64:80].unsqueeze(2).broadcast_to([128, NT, 128]), ALU.mult))
                k.op("dve", ["o32a", "gngh"], ["o32a"], lambda e: e.tensor_tensor(o3, o3, gngh[:].unsqueeze(1).broadcast_to([128, NT, 128]), ALU.mult))
                k.op("dve", ["o32a", "gnbh"], ["o32a"], lambda e: e.tensor_tensor(o3, o3, gnbh[:].unsqueeze(1).broadcast_to([128, NT, 128]), ALU.add))
                k.op("dve", ["o32a", "rgs"], ["ob16a"], lambda e: e.tensor_tensor(ob16a[:], o32a[:], rgs[:], ALU.mult))
                for half in range(2):
                    tr_to("ob16a", ob16a[:, half * 1024:(half + 1) * 1024], 8, 2 + half, "oTr",
                          oTr3[:, h, half * 1024:(half + 1) * 1024].rearrange("p (c t) -> p c t", c=8))
        if stage == "D":
            dump(["oTr"], oTr[:, 0:2 * S], 2 * S)
            return nc, k

        wo3 = hT3
        lgt = sb("lgt", [128, NT * 36], F32)
        lg3 = lgt[:].rearrange("p (i c) -> p i c", i=NT)
        with scope():
            g2b = bload("g2b", g2, D)
            brb = bload("brb", br, 36)
            wrs = sb("wrs", [128, 16 * 36], BF16)
            wload("wrs", wrs[:], wr)
            wrs3 = wrs[:].rearrange("p (c n) -> p c n", c=16)
            xt2 = [sb("xt2_%d" % i, [128, D], F32) for i in range(2)]
            x1t = [sb("x1t_%d" % i, [128, D], F32) for i in range(2)]
            sq2 = sb("sq2", [128, D], BF16)
            h2t = [sb("h2t_%d" % i, [128, D], BF16) for i in range(2)]
            h2T = sb("h2T", [128, 16 * 128], BF16)
            h2T3 = h2T[:].rearrange("p (c t) -> p c t", c=16)

            def e1_front(i):
                b = i % 2
                ts_ = slice(i * 128, (i + 1) * 128)
                k.dma("sp", [], ["xt2_%d" % b], lambda e: e.dma_start(out=xt2[b][:], in_=x[ts_, :]))
                for nb in range(4):
                    for c in range(16):
                        src = oT3[:, c, ts_] if c < 8 else oTr3[:, c - 8, ts_]
                        k.op("pe", ["oT", "oTr", "hT"], [pkey(nb)],
                             lambda e: e.matmul(PB[nb][:, :], src, wo3[:, c, nb * 512:(nb + 1) * 512], start=(c == 0), stop=(c == 15)))
                    k.op("dve", [pkey(nb), "xt2_%d" % b], ["x1t_%d" % b],
                         lambda e: e.tensor_tensor(x1t[b][:, nb * 512:(nb + 1) * 512], PB[nb][:, :], xt2[b][:, nb * 512:(nb + 1) * 512], ALU.add))
                k.dma("sp", ["x1t_%d" % b], ["y"], lambda e: e.dma_start(out=y[ts_, :], in_=x1t[b][:]))
                k.op("act", ["x1t_%d" % b], ["sq2", "ss2_%d" % b], lambda e: e.activation(sq2[:], x1t[b][:], AF.Square, accum_out=small[:, 32 + b:33 + b]))
                rstd_from("ss2_%d" % b, small[:, 32 + b:33 + b], small[:, 34 + b:35 + b], "rs2_%d" % b, D, RMS_EPS)
                k.op("dve", ["rs2_%d" % b, "x1t_%d" % b, "g2b"], ["h2t_%d" % b],
                     lambda e: e.scalar_tensor_tensor(h2t[b][:], x1t[b][:], small[:, 34 + b:35 + b], g2b[:], ALU.mult, ALU.mult))
                k.dma("sp", ["h2t_%d" % b], ["h2d"], lambda e: e.dma_start(out=h2d[ts_, :], in_=h2t[b][:]))

            def e1_back(i):
                b = i % 2
                for half in range(2):
                    tr_to("h2t_%d" % b, h2t[b][:, half * 1024:(half + 1) * 1024], 8, 4 + half, "h2T", h2T3[:, half * 8:(half + 1) * 8, :])
                for c in range(16):
                    k.op("pe", ["h2T", "wrs"], [pkey(6)],
                         lambda e: e.matmul(PB[6][:, 0:36], h2T3[:, c, :], wrs3[:, c, :], start=(c == 0), stop=(c == 15)))
                k.op("dve", [pkey(6), "brb"], ["lgt"], lambda e: e.tensor_tensor(lg3[:, i, :], PB[6][:, 0:36], brb[:], ALU.add))
            e1_front(0)
            for i in range(NT):
                if i + 1 < NT:
                    e1_front(i + 1)
                e1_back(i)
        if stage == "E1":
            dump(["lgt"], lgt[:], NT * 36)
            k.wait_all("sp", ["y", "h2d"])
            return nc, k

        wsets = [
            (("wgA", hT[:, 0:8192]), ("wuA", hT[:, 8192:16384]), ("wdA", hT[:, 16384:24576])),
            (("wgB", hT[:, 24576:32768]), ("wuB", oT[:, 0:8192]), ("wdB", oT[:, 8192:16384])),
        ]

        def load_w(ex, extra=()):
            (kg, wgt), (ku, wut), (kd, wdt) = wsets[ex % 2]
            k.dma("pool", [], [kg] + list(extra), lambda e: e.dma_start(out=wgt, in_=wg_d[ex]))
            k.dma("pool", [], [ku] + list(extra), lambda e: e.dma_start(out=wut, in_=wu_d[ex]))
            k.dma("pool", [], [kd] + list(extra), lambda e: e.dma_start(out=wdt, in_=wd_d[ex]))

        if stage == "full":
            load_w(0, ("hT", "oT"))
            load_w(1, ("hT", "oT"))

        rt = scope(); rt.__enter__()
        ones_bf = cload("ones_bf"); Ust = cload("Ustrict"); bvals = cload("bvals")

        def Rr(name, w, dt=F32):
            return sb(name, [128, w], dt)
        gmax = Rr("gmax", 16); gsh = Rr("gsh", 64); gsum = Rr("gsum", 16); gtop = Rr("gtop", 16)
        ohg = Rr("ohg", 64); msk = Rr("msk", 512); m8e = Rr("m8e", 128)
        A1 = Rr("A1", 512); A2 = Rr("A2", 512); Abf = Rr("Abf", 512, BF16); dl = Rr("dl", 16); wt1 = Rr("wt1", 16); wt2 = Rr("wt2", 16)
        rank = Rr("rank", 512); pstart = Rr("pstart", 32)
        d0f = Rr("d0f", 16); d1f = Rr("d1f", 16); d0i = Rr("d0i", 16, I32); d1i = Rr("d1i", 16, I32)
        v3 = lambda t, c: t[:].rearrange("p (i c) -> p i c", c=c)
        gl = lg3[:, :, 0:4]
        le4 = lg3[:, :, 4:36].rearrange("p i (g e) -> p i g e", g=4)
        k.op("dve", ["lgt"], ["gmax"], lambda e: e.tensor_reduce(gmax[:], gl, AX.X, ALU.max))
        k.op("dve", ["lgt", "gmax"], ["gsh"], lambda e: e.tensor_tensor(v3(gsh, 4), gl, gmax[:].unsqueeze(2).broadcast_to([128, 16, 4]), ALU.subtract))
        k.op("dve", ["gsh"], ["ohg"], lambda e: e.tensor_scalar(ohg[:], gsh[:], 0.0, None, ALU.is_ge))
        k.op("act", ["gsh"], ["gsh"], lambda e: e.activation(gsh[:], gsh[:], AF.Exp))
        k.op("dve", ["gsh"], ["gsum"], lambda e: e.tensor_reduce(gsum[:], v3(gsh, 4), AX.X, ALU.add))
        k.op("dve", ["gsum"], ["gtop"], lambda e: e.reciprocal(gtop[:], gsum[:]))
        k.op("dve", ["ohg"], ["ohg"], lambda e: e.tensor_scalar(ohg[:], ohg[:], 1e30, -1e30, ALU.mult, ALU.add))
        k.op("dve", ["lgt", "ohg"], ["msk"],
             lambda e: e.tensor_tensor(msk[:].rearrange("p (i g e) -> p i g e", i=16, g=4), le4,
                                       v3(ohg, 4).unsqueeze(3).broadcast_to([128, 16, 4, 8]), ALU.add))
        for i in range(NT):
            k.op("dve", ["msk"], ["m8e"], lambda e: e.max(v3(m8e, 8)[:, i, :], v3(msk, 32)[:, i, :]))
        k.op("dve", ["msk", "m8e"], ["A1"], lambda e: e.tensor_tensor(v3(A1, 32), v3(msk, 32), v3(m8e, 8)[:, :, 0:1].broadcast_to([128, 16, 32]), ALU.is_equal))
        k.op("dve", ["msk", "m8e"], ["A2"], lambda e: e.tensor_tensor(v3(A2, 32), v3(msk, 32), v3(m8e, 8)[:, :, 1:2].broadcast_to([128, 16, 32]), ALU.is_equal))
        k.op("dve", ["m8e"], ["dl"], lambda e: e.tensor_tensor(dl[:], v3(m8e, 8)[:, :, 0], v3(m8e, 8)[:, :, 1], ALU.subtract))
        k.op("act", ["dl"], ["wt1"], lambda e: e.activation(wt1[:], dl[:], AF.Sigmoid))
        k.op("dve", ["wt1", "gtop"], ["wt1"], lambda e: e.tensor_tensor(wt1[:], wt1[:], gtop[:], ALU.mult))
        k.op("dve", ["wt1", "gtop"], ["wt2"], lambda e: e.tensor_tensor(wt2[:], gtop[:], wt1[:], ALU.subtract))
        k.op("dve", ["A1", "A2"], ["Abf"], lambda e: e.tensor_tensor(Abf[:], A1[:], A2[:], ALU.add))
        Ab3 = v3(Abf, 32)
        for i in range(NT):
            bank = i % 2
            for j in range(i):
                k.op("pe", ["Abf", "s_ones_bf"], [pkey(bank)], lambda e: e.matmul(PB[bank][:, 0:32], ones_bf[:], Ab3[:, j, :], start=(j == 0), stop=False))
            k.op("pe", ["Abf", "s_Ustrict"], [pkey(bank)], lambda e: e.matmul(PB[bank][:, 0:32], Ust[:], Ab3[:, i, :], start=(i == 0), stop=True))
            k.op("act", [pkey(bank)], ["rank"], lambda e: e.copy(v3(rank, 32)[:, i, :], PB[bank][:, 0:32]))
        k.op("dve", ["s_bvals"], ["pstart"], lambda e: e.tensor_copy(pstart[:], bvals[:, 0:64:2]))
        k.op("dve", ["rank", "pstart"], ["rank"], lambda e: e.tensor_tensor(v3(rank, 32), v3(rank, 32), pstart[:].unsqueeze(1).broadcast_to([128, 16, 32]), ALU.add))
        k.op("dve", ["rank", "A1"], ["A1"], lambda e: e.tensor_tensor(A1[:], A1[:], rank[:], ALU.mult))
        k.op("dve", ["rank", "A2"], ["A2"], lambda e: e.tensor_tensor(A2[:], A2[:], rank[:], ALU.mult))
        k.op("dve", ["A1"], ["d0f"], lambda e: e.tensor_reduce(d0f[:], v3(A1, 32), AX.X, ALU.add))
        k.op("dve", ["A2"], ["d1f"], lambda e: e.tensor_reduce(d1f[:], v3(A2, 32), AX.X, ALU.add))
        k.op("dve", ["d0f"], ["d0i"], lambda e: e.tensor_copy(d0i[:], d0f[:]))
        k.op("dve", ["d1f"], ["d1i"], lambda e: e.tensor_copy(d1i[:], d1f[:]))
        if stage == "E2":
            dump(["d0f", "d1f", "wt1", "wt2"], d0f[:], 16)
            early(); return nc, k

        with scope():
            h2s = [sb("h2s%d" % i, [128, D], BF16) for i in range(2)]
            for i in range(NT):
                hk = "h2s%d" % (i % 2)
                k.dma("sp", ["h2d"], [hk], lambda e: e.dma_start(out=h2s[i % 2][:], in_=h2d[i * 128:(i + 1) * 128, :]))
                for di in (d0i, d1i):
                    k.dma("pool", [hk, "d0i", "d1i", "xs"] + ["xs_z%d" % b_ for b_ in range(NBLK)], ["xs"],
                          lambda e: e.indirect_dma_start(out=xs[:, :], out_offset=bass.IndirectOffsetOnAxis(ap=di[:, i:i + 1], axis=0),
                                                         in_=h2s[i % 2][:, :], in_offset=None))

        if stage == "E3":
            k.wait_all("sp", ["xs", "y", "h2d"]); dump(["d0f"], d0f[:], 16); early(); return nc, k

        with scope():
            xb = [sb("xb%d" % i, [128, D], BF16) for i in range(2)]
            xbT = [sb("xbT%d" % i, [128, 16 * 128], BF16) for i in range(2)]
            sg = sb("sg", [128, 512], F32)
            hmid = sb("hmid", [128, 512], BF16)
            hmT = sb("hmT", [128, 512], BF16); hmT3 = hmT[:].rearrange("p (c t) -> p c t", c=4)
            ysb = [sb("ysb%d" % i, [128, D], F32) for i in range(2)]
            def load_x(b):
                k.dma("sp", ["xs"], ["xb%d" % (b % 2)], lambda e: e.dma_start(out=xb[b % 2][:], in_=xs[b * 128:(b + 1) * 128, :]))
            YB = [4, 5, 6, 3]
            k.barrier()
            load_x(0)
            for b in range(NBLK):
                ex = b // 2
                if b % 2 == 0 and 2 <= ex + 1 < NE:
                    load_w(ex + 1)
                if b + 1 < NBLK:
                    load_x(b + 1)
                (kg, wgt), (ku, wut), (kd, wdt) = wsets[ex % 2]
                xbk = "xb%d" % (b % 2)
                xbTk = "xbT%d" % (b % 2)
                xbT3 = xbT[b % 2][:].rearrange("p (c t) -> p c t", c=16)
                for half in range(2):
                    tr_to(xbk, xb[b % 2][:, half * 1024:(half + 1) * 1024], 8, 2 + half, xbTk, xbT3[:, half * 8:(half + 1) * 8, :])
                wg3 = wgt.rearrange("p (c f) -> p c f", c=16)
                wu3 = wut.rearrange("p (c f) -> p c f", c=16)
                wd3 = wdt.rearrange("p (c f) -> p c f", c=4)
                for c in range(16):
                    k.op("pe", [xbTk, kg], [pkey(0)], lambda e: e.matmul(PB[0][:, :], xbT3[:, c, :], wg3[:, c, :], start=(c == 0), stop=(c == 15)))
                for c in range(16):
                    k.op("pe", [xbTk, ku], [pkey(1)], lambda e: e.matmul(PB[1][:, :], xbT3[:, c, :], wu3[:, c, :], start=(c == 0), stop=(c == 15)))
                k.op("act", [pkey(0)], ["sg"], lambda e: e.activation(sg[:], PB[0][:, :], AF.Silu))
                k.op("dve", ["sg", pkey(1)], ["hmid"], lambda e: e.tensor_tensor(hmid[:], sg[:], PB[1][:, :], ALU.mult))
                tr_to("hmid", hmid, 4, 2, "hmT", hmT3)
                ysk = "ysb%d" % (b % 2)
                for nb in range(4):
                    for fc in range(4):
                        k.op("pe", ["hmT", kd], [pkey(YB[nb])],
                             lambda e: e.matmul(PB[YB[nb]][:, :], hmT3[:, fc, :], wd3[:, fc, nb * 512:(nb + 1) * 512], start=(fc == 0), stop=(fc == 3)))
                    if nb % 2 == 0:
                        k.op("act", [pkey(YB[nb])], [ysk], lambda e: e.copy(ysb[b % 2][:, nb * 512:(nb + 1) * 512], PB[YB[nb]][:, :]))
                    else:
                        k.op("dve", [pkey(YB[nb])], [ysk], lambda e: e.tensor_copy(ysb[b % 2][:, nb * 512:(nb + 1) * 512], PB[YB[nb]][:, :]))
                k.dma("sp", [ysk], ["ysd"], lambda e: e.dma_start(out=ysd[b * 128:(b + 1) * 128, :], in_=ysb[b % 2][:]))

        with scope():
            xa = sb("xa", [128, D], F32)
            ga = sb("ga", [128, D], F32)
            gb = sb("gb", [128, D], F32)
            for i in range(NT):
                ts_ = slice(i * 128, (i + 1) * 128)
                k.dma("sp", ["y"], ["xa"], lambda e: e.dma_start(out=xa[:], in_=y[ts_, :]))
                k.dma("pool", ["ysd", "d0i"], ["ga"],
                      lambda e: e.indirect_dma_start(out=ga[:, :], out_offset=None, in_=ysd[:, :],
                                                     in_offset=bass.IndirectOffsetOnAxis(ap=d0i[:, i:i + 1], axis=0)))
                k.dma("pool", ["ysd", "d1i"], ["gb"],
                      lambda e: e.indirect_dma_start(out=gb[:, :], out_offset=None, in_=ysd[:, :],
                                                     in_offset=bass.IndirectOffsetOnAxis(ap=d1i[:, i:i + 1], axis=0)))
                k.op("dve", ["xa", "ga", "wt1"], ["xa"], lambda e: e.scalar_tensor_tensor(xa[:], ga[:], wt1[:, i:i + 1], xa[:], ALU.mult, ALU.add))
                k.op("dve", ["xa", "gb", "wt2"], ["xa"], lambda e: e.scalar_tensor_tensor(xa[:], gb[:], wt2[:, i:i + 1], xa[:], ALU.mult, ALU.add))
                k.dma("sp", ["xa"], ["y"], lambda e: e.dma_start(out=y[ts_, :], in_=xa[:]))
        k.wait_all("sp", ["y"])
        k.barrier()
        rt.__exit__(None, None, None)
    return nc, k


def _prep_inputs(inp):
    f = np.float32
    w_in = np.asarray(inp["w_in"][0], f)
    def blk(cols):
        w = w_in[:, cols]
        n = w.shape[1]
        return np.ascontiguousarray(w.reshape(16, 128, n).transpose(1, 0, 2)).reshape(128, 16 * n)
    r = np.arange
    wq = np.stack([blk(r(512 * g, 512 * g + 512)) for g in range(2)])
    wkv = np.stack([blk(np.concatenate([r(1024 + 128 * g, 1152 + 128 * g), r(1280 + 128 * g, 1408 + 128 * g),
                                        r(1536 + 128 * g, 1664 + 128 * g), r(1792 + 128 * g, 1920 + 128 * g)])) for g in range(2)])
    ww = np.stack([blk(np.concatenate([r(2048 + 128 * g, 2176 + 128 * g), r(2304 + 128 * g, 2432 + 128 * g),
                                       r(2560 + 12 * g, 2572 + 12 * g)])) for g in range(2)])
    wret = np.stack([blk(np.concatenate([r(2584 + 128 * h, 2712 + 128 * h), r(3608 + 128 * h, 3736 + 128 * h),
                                         r(4632 + 128 * h, 4760 + 128 * h), r(5656 + 128 * h, 5784 + 128 * h)])) for h in range(8)])
    shared = dict(
        zrow=np.zeros((128, D), ml_dtypes.bfloat16),
        g1=np.asarray(inp["norm1_g"], f).reshape(1, D), g2=np.asarray(inp["norm2_g"], f).reshape(1, D),
        wq=wq, wkv=wkv, ww=ww, wret=wret,
        w1=np.ascontiguousarray(np.asarray(inp["cmp_w1"][0], f).transpose(0, 2, 1, 3)).reshape(2, 128, 32 * 128),
        posT=np.ascontiguousarray(np.asarray(inp["cmp_pos"][0], f).transpose(0, 2, 1)),
        w2=np.asarray(inp["cmp_w2"][0], f),
        gq=np.asarray(inp["q_norm_g"], f).reshape(1, 128), gk=np.asarray(inp["k_norm_g"], f).reshape(1, 384),
        gng=np.asarray(inp["ret_gn_g"], f).reshape(1, 1024), gnb=np.asarray(inp["ret_gn_b"], f).reshape(1, 1024),
        wout=np.ascontiguousarray(np.asarray(inp["w_out"][0], f).reshape(16, 128, D).transpose(1, 0, 2)).reshape(128, 16 * D),
        wr=np.ascontiguousarray(np.concatenate([np.asarray(inp["w_router_group"][0], f), np.asarray(inp["w_router_expert"][0], f)], axis=1)
                                .reshape(16, 128, 36).transpose(1, 0, 2)).reshape(128, 16 * 36),
        br=np.concatenate([np.asarray(inp["b_router_group"], f).reshape(-1), np.asarray(inp["b_router_expert"], f).reshape(-1)]).reshape(1, 36),
        w_gate=np.ascontiguousarray(np.asarray(inp["w_exp_gate"][0], f).reshape(NE, 16, 128, DE).transpose(0, 2, 1, 3)).reshape(NE, 128, 16 * DE),
        w_up=np.ascontiguousarray(np.asarray(inp["w_exp_up"][0], f).reshape(NE, 16, 128, DE).transpose(0, 2, 1, 3)).reshape(NE, 128, 16 * DE),
        w_down=np.ascontiguousarray(np.asarray(inp["w_exp_down"][0], f).reshape(NE, 4, 128, D).transpose(0, 2, 1, 3)).reshape(NE, 128, 4 * D),
    )
    return shared


def kernel(**inp):
    consts = _consts()
    shared = _prep_inputs(inp)
    for n in CONST_DT:
        shared["c_" + n] = consts[n]
    nc, _ = build(consts, "full")
    xin = np.asarray(inp["x"], np.float32)
    in_maps = [dict(shared, x=np.ascontiguousarray(xin[b])) for b in range(8)]
    res = run_bass_kernel_spmd(nc, in_maps, core_ids=list(range(8)))
    return np.stack([np.asarray(r["y"], np.float32) for r in res.results], axis=0)
```
